# Optimizing a Trainium2 kernel written in Bass

```python
import math
import jax, jax.numpy as jnp
from jax import lax
import numpy as np

D_MODEL = 1024
BATCH = 2
SEQ = 16384
DEPTH = 2

CTX_LEN = 256
GRID_W = 64
N_MIXERS = 2
DA_HEADS = 8
DA_HEAD_DIM = 64
DA_V_DIM = 2 * DA_HEAD_DIM
DA_QK_WIDTH = DA_HEADS * 2 * DA_HEAD_DIM
DA_V_WIDTH = DA_HEADS * DA_V_DIM
Q_BLOCK = 128
ROPE_BASE = 10000.0
ROPE_FREQS = DA_HEAD_DIM // 4
GM_CHUNK = 128
GM_HALF = 2 * D_MODEL
GM_GROUPS = 8
GM_GROUP_DIM = GM_HALF // GM_GROUPS
FFN_DIM = 2816
N_EXPERTS = 8
TOP_K = 2
EXPERT_DIM = 3584
EPS = 1e-6
N_ATTN_LAYERS = (DEPTH + 1) // 2
N_GMLP_LAYERS = DEPTH // 2

kernel_name = 'hybrid_diffattn_chunkgmlp_moe_dit'


def rmsnorm(x, w):
    xf = x.astype(jnp.float32)
    y = xf * lax.rsqrt(jnp.mean(xf * xf, axis=-1, keepdims=True) + EPS)
    return (y * w).astype(x.dtype)


def layernorm(x, w, b):
    xf = x.astype(jnp.float32)
    mu = jnp.mean(xf, axis=-1, keepdims=True)
    xc = xf - mu
    y = xc * lax.rsqrt(jnp.mean(xc * xc, axis=-1, keepdims=True) + EPS)
    return (y * w + b).astype(x.dtype)


def ada(cond, w_mod, b_mod):
    m = jax.nn.silu(cond) @ w_mod + b_mod
    return jnp.split(m, 6, axis=-1)


def modulate(h, shift, scale):
    return h * (1.0 + scale) + shift


def rope_axis(x, ang):
    x1, x2 = jnp.split(x, 2, axis=-1)
    cs = jnp.cos(ang)[None, :, None, None, :]
    sn = jnp.sin(ang)[None, :, None, None, :]
    return jnp.concatenate([x1 * cs - x2 * sn, x1 * sn + x2 * cs], axis=-1).astype(x.dtype)


def rope2d(x, ang_row, ang_col):
    xr, xcl = jnp.split(x, 2, axis=-1)
    return jnp.concatenate([rope_axis(xr, ang_row), rope_axis(xcl, ang_col)], axis=-1)


def diff_attention(h, hc, w_qkv, w_o, lq1, lk1, lq2, lk2, subln_w, lam_init, ang_row, ang_col, ctx_out):
    B, S, _ = h.shape
    L = hc.shape[1]
    qkv = h @ w_qkv
    q = qkv[..., :DA_QK_WIDTH].reshape(B, S, DA_HEADS, 2, DA_HEAD_DIM)
    k = qkv[..., DA_QK_WIDTH:2 * DA_QK_WIDTH].reshape(B, S, DA_HEADS, 2, DA_HEAD_DIM)
    v = qkv[..., 2 * DA_QK_WIDTH:].reshape(B, S, DA_HEADS, DA_V_DIM)
    q = rope2d(q, ang_row, ang_col)
    k = rope2d(k, ang_row, ang_col)
    if ctx_out:
        qkv_c = hc @ w_qkv
        qc = qkv_c[..., :DA_QK_WIDTH].reshape(B, L, DA_HEADS, 2, DA_HEAD_DIM)
        kv_c = qkv_c[..., DA_QK_WIDTH:]
    else:
        kv_c = hc @ w_qkv[:, DA_QK_WIDTH:]
    kc = kv_c[..., :DA_QK_WIDTH].reshape(B, L, DA_HEADS, 2, DA_HEAD_DIM)
    vc = kv_c[..., DA_QK_WIDTH:].reshape(B, L, DA_HEADS, DA_V_DIM)
    lam = (jnp.exp(jnp.sum(lq1.astype(jnp.float32) * lk1.astype(jnp.float32)))
           - jnp.exp(jnp.sum(lq2.astype(jnp.float32) * lk2.astype(jnp.float32))) + lam_init)
    scale = DA_HEAD_DIM ** -0.5

    def attend(qb, keys, vals):
        s = jnp.einsum('bqhcd,bkhcd->bhcqk', qb, keys).astype(jnp.float32) * scale
        p = jax.nn.softmax(s, axis=-1)
        a = p[:, :, 0] - lam * p[:, :, 1]
        return jnp.einsum('bhqk,bkhv->bqhv', a.astype(vals.dtype), vals)

    k_all = jnp.concatenate([kc, k], axis=1)
    v_all = jnp.concatenate([vc, v], axis=1)
    n_blk = S // Q_BLOCK
    qb = q.reshape(B, n_blk, Q_BLOCK, DA_HEADS, 2, DA_HEAD_DIM).swapaxes(0, 1)
    o = lax.map(lambda blk: attend(blk, k_all, v_all), qb)
    o = o.swapaxes(0, 1).reshape(B, S, DA_HEADS, DA_V_DIM)

    def finish(out):
        out = rmsnorm(out, subln_w) * (1.0 - lam_init)
        return out.reshape(out.shape[0], out.shape[1], DA_V_WIDTH) @ w_o

    y = finish(o)
    yc = finish(attend(qc, kc, vc)) if ctx_out else None
    return y, yc


def chunk_gmlp(h, w_in, vn_w, vn_b, w_s, b_s, w_out):
    B, L, _ = h.shape
    z = jax.nn.gelu(h @ w_in, approximate=False)
    u, v = z[..., :GM_HALF], z[..., GM_HALF:]
    v = layernorm(v, vn_w, vn_b)
    v = v.reshape(B, L // GM_CHUNK, GM_CHUNK, GM_GROUPS, GM_GROUP_DIM)
    s = jnp.einsum('gpq,bnqgc->bnpgc', w_s, v) + b_s.T[:, :, None]
    return (u * s.reshape(B, L, GM_HALF)) @ w_out


def swiglu(h, wg, wu, wd):
    return (jax.nn.silu(h @ wg) * (h @ wu)) @ wd


def moe_ffn(h, w_router, wg, wu, wd):
    logits = (h @ w_router).astype(jnp.float32)
    top_v, top_i = lax.top_k(logits, TOP_K)
    gates = jax.nn.softmax(top_v, axis=-1)
    y = jnp.zeros_like(h)
    for e in range(N_EXPERTS):
        g_e = jnp.sum(jnp.where(top_i == e, gates, 0.0), axis=-1, keepdims=True).astype(h.dtype)
        y = y + g_e * swiglu(h, wg[e], wu[e], wd[e])
    return y


def setup_inputs(seed: int = 0) -> dict:
    key = jax.random.key(seed)
    ks = jax.random.split(key, 32)
    f32 = jnp.float32
    D = D_MODEL

    def nrm(k, shape, s):
        return jax.random.normal(k, shape, f32) * s

    def gain(k, shape):
        return 1.0 + 0.02 * jax.random.normal(k, shape, f32)

    return {
        'x': nrm(ks[0], (BATCH, SEQ, D), 1.0),
        'c': nrm(ks[1], (BATCH, D), 1.0),
        'ctx': nrm(ks[2], (BATCH, CTX_LEN, D), 1.0),
        'c_ctx': nrm(ks[3], (D,), 1.0),
        'w_mod': nrm(ks[4], (DEPTH, D, 6 * D), 0.5 * D ** -0.5),
        'b_mod': nrm(ks[5], (DEPTH, 6 * D), 0.02),
        'norm1_w': gain(ks[6], (DEPTH, D)),
        'norm2_w': gain(ks[7], (DEPTH, D)),
        'final_norm_w': gain(ks[8], (D,)),
        'da_w_qkv': nrm(ks[9], (N_ATTN_LAYERS, D, 2 * DA_QK_WIDTH + DA_V_WIDTH), D ** -0.5),
        'da_w_o': nrm(ks[10], (N_ATTN_LAYERS, DA_V_WIDTH, D), DA_V_WIDTH ** -0.5),
        'da_lambda_q1': nrm(ks[11], (N_ATTN_LAYERS, DA_HEAD_DIM), 0.1),
        'da_lambda_k1': nrm(ks[12], (N_ATTN_LAYERS, DA_HEAD_DIM), 0.1),
        'da_lambda_q2': nrm(ks[13], (N_ATTN_LAYERS, DA_HEAD_DIM), 0.1),
        'da_lambda_k2': nrm(ks[14], (N_ATTN_LAYERS, DA_HEAD_DIM), 0.1),
        'da_subln_w': gain(ks[15], (N_ATTN_LAYERS, DA_V_DIM)),
        'gm_w_in': nrm(ks[16], (N_GMLP_LAYERS, D, 2 * GM_HALF), D ** -0.5),
        'gm_vnorm_w': gain(ks[17], (N_GMLP_LAYERS, GM_HALF)),
        'gm_vnorm_b': nrm(ks[18], (N_GMLP_LAYERS, GM_HALF), 0.02),
        'gm_w_s': nrm(ks[19], (N_GMLP_LAYERS, GM_GROUPS, GM_CHUNK, GM_CHUNK), GM_CHUNK ** -0.5),
        'gm_b_s': gain(ks[20], (N_GMLP_LAYERS, GM_GROUPS, GM_CHUNK)),
        'gm_w_out': nrm(ks[21], (N_GMLP_LAYERS, GM_HALF, D), GM_HALF ** -0.5),
        'ffn_w_gate': nrm(ks[22], (N_ATTN_LAYERS, D, FFN_DIM), D ** -0.5),
        'ffn_w_up': nrm(ks[23], (N_ATTN_LAYERS, D, FFN_DIM), D ** -0.5),
        'ffn_w_down': nrm(ks[24], (N_ATTN_LAYERS, FFN_DIM, D), FFN_DIM ** -0.5),
        'moe_w_router': nrm(ks[25], (N_GMLP_LAYERS, D, N_EXPERTS), D ** -0.5),
        'moe_w_gate': nrm(ks[26], (N_GMLP_LAYERS, N_EXPERTS, D, EXPERT_DIM), D ** -0.5),
        'moe_w_up': nrm(ks[27], (N_GMLP_LAYERS, N_EXPERTS, D, EXPERT_DIM), D ** -0.5),
        'moe_w_down': nrm(ks[28], (N_GMLP_LAYERS, N_EXPERTS, EXPERT_DIM, D), EXPERT_DIM ** -0.5),
    }


def reference(x, c, ctx, c_ctx, w_mod, b_mod, norm1_w, norm2_w, final_norm_w,
              da_w_qkv, da_w_o, da_lambda_q1, da_lambda_k1, da_lambda_q2, da_lambda_k2, da_subln_w,
              gm_w_in, gm_vnorm_w, gm_vnorm_b, gm_w_s, gm_b_s, gm_w_out,
              ffn_w_gate, ffn_w_up, ffn_w_down,
              moe_w_router, moe_w_gate, moe_w_up, moe_w_down):
    n_tok = x.shape[1]
    rows = n_tok // GRID_W
    row = jnp.broadcast_to(jnp.arange(rows, dtype=jnp.float32)[:, None], (rows, GRID_W)).reshape(-1)
    col = jnp.broadcast_to(jnp.arange(GRID_W, dtype=jnp.float32)[None, :], (rows, GRID_W)).reshape(-1)
    inv_freq = ROPE_BASE ** (-jnp.arange(ROPE_FREQS, dtype=jnp.float32) / ROPE_FREQS)
    ang_row = row[:, None] * inv_freq
    ang_col = col[:, None] * inv_freq

    xc = ctx
    for i in range(DEPTH):
        j = i // 2
        is_attn = (i % N_MIXERS == 0)
        ctx_out = any(l % N_MIXERS == 0 for l in range(i + 1, DEPTH))
        sh1, sc1, g1, sh2, sc2, g2 = ada(c[:, None, :], w_mod[i], b_mod[i])
        csh1, csc1, cg1, csh2, csc2, cg2 = ada(c_ctx, w_mod[i], b_mod[i])
        h = modulate(rmsnorm(x, norm1_w[i]), sh1, sc1)
        if is_attn or ctx_out:
            hc = modulate(rmsnorm(xc, norm1_w[i]), csh1, csc1)
        if is_attn:
            lam_init = 0.8 - 0.6 * math.exp(-0.3 * i)
            y, yc = diff_attention(h, hc, da_w_qkv[j], da_w_o[j], da_lambda_q1[j], da_lambda_k1[j],
                                   da_lambda_q2[j], da_lambda_k2[j], da_subln_w[j], lam_init,
                                   ang_row, ang_col, ctx_out)
        else:
            gm = (gm_w_in[j], gm_vnorm_w[j], gm_vnorm_b[j], gm_w_s[j], gm_b_s[j], gm_w_out[j])
            y = chunk_gmlp(h, *gm)
            yc = chunk_gmlp(hc, *gm) if ctx_out else None
        x = x + g1 * y
        if ctx_out:
            xc = xc + cg1 * yc

        if i % 2 == 0:
            ffn = lambda t: swiglu(t, ffn_w_gate[j], ffn_w_up[j], ffn_w_down[j])
        else:
            ffn = lambda t: moe_ffn(t, moe_w_router[j], moe_w_gate[j], moe_w_up[j], moe_w_down[j])
        h = modulate(rmsnorm(x, norm2_w[i]), sh2, sc2)
        x = x + g2 * ffn(h)
        if ctx_out:
            hc = modulate(rmsnorm(xc, norm2_w[i]), csh2, csc2)
            xc = xc + cg2 * ffn(hc)
    return rmsnorm(x, final_norm_w)
```

```python
import math
from contextlib import ExitStack

import numpy as np
import concourse.bass as bass
import concourse.mybir as mybir
from concourse.bass_utils import run_bass_kernel_spmd

F32 = mybir.dt.float32
BF16 = mybir.dt.bfloat16
AF = mybir.ActivationFunctionType
ALU = mybir.AluOpType
AX = mybir.AxisListType

D = 1024
SEQ = 16384
CTX = 256
NKEY = SEQ + CTX
NKT = NKEY // 128
TOK = 4096
H = 8
FFN = 2816
EXP_D = 3584
NEXP = 8
EPS = 1e-6
VW = 130
LAM_INIT0 = 0.8 - 0.6 * math.exp(-0.3 * 0)


class Ev:
    __slots__ = ("eng", "needed", "value", "key")

    def __init__(self, eng, key=None):
        self.eng = eng
        self.needed = False
        self.value = None
        self.key = key


class Prog:
    ENGS = ("pe", "act", "dve", "pool", "sp")

    def __init__(self, nc, name):
        self.nc = nc
        self.name = name
        self.ops = {e: [] for e in self.ENGS}
        self.lastw = {}
        self.readers = {}
        self.dma_cnt = {}

    def _deps(self, reads, writes, dma_key=None):
        deps = []
        for k in reads:
            e = self.lastw.get(k)
            if e is not None:
                deps.append(e)
        for k in writes:
            e = self.lastw.get(k)
            if e is not None:
                deps.append(e)
            deps.extend(self.readers.get(k, {}).values())
        out = []
        for d in deps:
            if d.eng == "dma":
                if dma_key is not None and d.key == dma_key:
                    continue
                if d.value != self.dma_cnt[d.key]:
                    d2 = Ev("dma", d.key)
                    d2.value = self.dma_cnt[d.key]
                    d = d2
            d.needed = True
            out.append(d)
        return out

    def _commit(self, ev, reads, writes):
        rk = ev.key if ev.eng == "dma" else ev.eng
        for k in reads:
            self.readers.setdefault(k, {})[rk] = ev
        for k in writes:
            self.lastw[k] = ev
            self.readers[k] = {}

    def op(self, eng, fn, reads=(), writes=(), extra=()):
        deps = self._deps(reads, writes)
        for d in extra:
            if d is not None:
                d.needed = True
                deps.append(d)
        ev = Ev(eng)
        self.ops[eng].append((fn, deps, ev))
        self._commit(ev, reads, writes)
        return ev

    def dma(self, q, out, in_, key, reads=(), writes=(), **kw):
        deps = self._deps(reads, writes, dma_key=key)
        ev = Ev("dma", key)
        self.dma_cnt[key] = self.dma_cnt.get(key, 0) + 16
        ev.value = self.dma_cnt[key]
        ev.needed = True
        self.ops[q].append((lambda e: e.dma_start(out=out, in_=in_, **kw), deps, ev))
        self._commit(ev, reads, writes)
        return ev

    def dma_g(self, q, out, in_, gs, **kw):
        ev = Ev("gdma", gs["sem"])
        gs["count"] += 16
        self.ops[q].append((lambda e: e.dma_start(out=out, in_=in_, **kw), [], ev))
        return ev

    def flush(self):
        nc = self.nc
        with ExitStack() as st:
            sems = {e: st.enter_context(nc.semaphore(f"{self.name}_s_{e}")) for e in ("pe", "act", "dve", "pool")}
            dsems = {}
            for i, k in enumerate(self.dma_cnt):
                dsems[k] = st.enter_context(nc.semaphore(f"{self.name}_d{i}"))
            for e in self.ENGS:
                c = 0
                for (_, _, ev) in self.ops[e]:
                    if ev.eng not in ("dma", "gdma") and ev.needed:
                        c += 1
                        ev.value = c
            block = st.enter_context(nc.Block())

            def run(e, eo):
                waited = {}
                for (fn, deps, ev) in self.ops[e]:
                    for d in deps:
                        if d.eng == "dma":
                            sk = ("d", d.key)
                            s = dsems[d.key]
                        else:
                            if d.eng == "pe" and e == "pe":
                                continue
                            sk = ("e", d.eng)
                            s = sems[d.eng]
                        if waited.get(sk, 0) >= d.value:
                            continue
                        eo.wait_ge(s, d.value)
                        waited[sk] = d.value
                    ins = fn(eo)
                    if ev.eng == "dma":
                        ins.then_inc(dsems[ev.key], 16)
                    elif ev.eng == "gdma":
                        ins.then_inc(ev.key, 16)
                    elif ev.needed:
                        ins.then_inc(sems[e], 1)
                if e == "sp":
                    for k, v in self.dma_cnt.items():
                        if waited.get(("d", k), 0) < v:
                            eo.wait_ge(dsems[k], v)

            @block.tensor
            def _(t):
                run("pe", t)

            @block.scalar
            def _(a):
                run("act", a)

            @block.vector
            def _(v):
                run("dve", v)

            @block.gpsimd
            def _(g):
                run("pool", g)

            @block.sync
            def _(s):
                run("sp", s)


def mm_group(P, out, pairs, reads, writes):
    n = len(pairs)
    ev = None
    for i, (l, r) in enumerate(pairs):
        fn = (lambda l=l, r=r, i=i: (lambda t: t.matmul(out, lhsT=l, rhs=r, start=(i == 0), stop=(i == n - 1))))()
        edge = (i == 0 or i == n - 1)
        ev = P.op("pe", fn, reads=reads if edge else (), writes=writes if edge else ())
    return ev


class Dram:
    def __init__(self, nc, ext_in=(), ext_out=()):
        self.nc = nc
        self.ext_in = set(ext_in)
        self.ext_out = set(ext_out)
        self.t = {}

    def get(self, name, shape, dtype, kind=None):
        if name in self.t:
            return self.t[name]
        if kind is None:
            kind = "ExternalInput" if name in self.ext_in else ("ExternalOutput" if name in self.ext_out else "Internal")
        ap = self.nc.dram_tensor(name, list(shape), dtype, kind=kind).ap()
        self.t[name] = ap
        return ap


def bc(ap, shape):
    return ap.to_broadcast(list(shape))


def phase_mod(nc, dr, tail=None):
    cvec = dr.get("cvec", [2, D], F32)
    w_mod = dr.get("w_mod", [2, D, 6 * D], F32)
    b_mod = dr.get("b_mod", [2, 6 * D], F32)
    modv = dr.get("modv", [2, 2, 6 * D], F32)
    P = Prog(nc, "p0")
    with ExitStack() as st:
        cT = st.enter_context(nc.sbuf_tensor("p0_cT", [128, 8, 2], F32))
        bm = st.enter_context(nc.sbuf_tensor("p0_bm", [2, 2, 6 * D], F32))
        mrow = st.enter_context(nc.sbuf_tensor("p0_mrow", [2, 2, 6 * D], F32))
        wblk = [st.enter_context(nc.sbuf_tensor(f"p0_w{i}", [128, 8, 512], F32)) for i in range(2)]
        pm = [st.enter_context(nc.psum_tensor(f"p0_pm{i}", [2, 512], F32)) for i in range(2)]
        for r in range(2):
            P.dma("sp", cT[:, :, r], cvec[r, :].rearrange("(kc p) -> p kc", p=128), key="c", writes=["cT"],
                  allow_slow_non_contiguous=True)
        for l in range(2):
            P.dma("sp", bm[:, l, :], b_mod[l, :].partition_broadcast(2),
                  key="bm", writes=[("bm", l)])
        P.op("act", lambda a: a.activation(out=cT[:], in_=cT[:], func=AF.Silu), reads=["cT"], writes=["cT"])
        i = 0
        for l in range(2):
            wv = w_mod[l].rearrange("(kc p) n -> p kc n", p=128)
            for blk in range(12):
                s = i % 2
                P.dma("sp", wblk[s][:], wv[:, :, blk * 512:(blk + 1) * 512], key=("w", s), writes=[("w", s)])
                mm_group(P, pm[s][:], [(cT[:, kc, :], wblk[s][:, kc, :]) for kc in range(8)],
                         reads=["cT", ("w", s)], writes=[("pm", s)])
                P.op("dve", (lambda s=s, l=l, blk=blk: lambda v: v.tensor_tensor(
                    out=mrow[:, l, blk * 512:(blk + 1) * 512], in0=pm[s][:], in1=bm[:, l, blk * 512:(blk + 1) * 512],
                    op=ALU.add))(), reads=[("pm", s), ("bm", l)], writes=["mrow"])
                i += 1
        P.dma("sp", modv[:, :, :], mrow[:], key="out", reads=["mrow"])
        if tail is not None:
            tail(P)
        P.flush()


class NormT:
    def __init__(self, nc, st, name, nbuf=2):
        self.nc = nc
        self.n = name
        self.nbuf = nbuf
        sb = lambda nm, shp, dt: st.enter_context(nc.sbuf_tensor(f"{name}_{nm}", shp, dt))
        self.xs = [sb(f"xs{i}", [128, D], BF16) for i in range(nbuf)]
        self.junk = [sb(f"junk{i}", [128, D], BF16) for i in range(nbuf)]
        self.ss = [sb(f"ss{i}", [128, 1], F32) for i in range(nbuf)]
        self.rstd = [sb(f"rstd{i}", [128, 1], F32) for i in range(nbuf)]
        self.tmp = [sb(f"tmp{i}", [128, 8, 128], F32) for i in range(nbuf)]
        self.ident = sb("ident", [128, 128], BF16)
        self.eps = sb("eps", [128, 1], F32)
        self.pT = [st.enter_context(nc.psum_tensor(f"{name}_pT{i}", [128, 8, 128], BF16)) for i in range(nbuf)]
        self.i = 0

    def init(self, P):
        n = self.n
        P.op("pool", lambda g: g.memset(self.ident[:], 0.0), writes=[(n, "ident")])
        P.op("pool", lambda g: g.affine_select(out=self.ident[:], in_=self.ident[:], pattern=[[-1, 128]],
                                               compare_op=ALU.not_equal, fill=1.0, base=0, channel_multiplier=1),
             reads=[(n, "ident")], writes=[(n, "ident")])
        P.op("pool", lambda g: g.memset(self.eps[:], EPS), writes=[(n, "eps")])

    def emit(self, P, xt, xt_key, scaleT, shiftT, par_keys, out, out_key, mean_div=float(D)):
        n = self.n
        s = self.i % self.nbuf
        self.i += 1
        self.last_rstd = (self.rstd[s], (n, "rstd", s))
        xs, junk, ss, rstd, tmp, pT = self.xs[s], self.junk[s], self.ss[s], self.rstd[s], self.tmp[s], self.pT[s]
        P.op("act", lambda a: a.activation(out=junk[:], in_=xt, func=AF.Square, accum_out=ss[:]),
             reads=[xt_key], writes=[(n, "junk", s), (n, "ss", s)])
        P.op("act", lambda a: a.activation(out=ss[:], in_=ss[:], func=AF.Sqrt, scale=1.0 / mean_div, bias=self.eps[:]),
             reads=[(n, "ss", s), (n, "eps")], writes=[(n, "ss", s)])
        P.op("dve", lambda v: v.reciprocal(out=rstd[:], in_=ss[:]), reads=[(n, "ss", s)], writes=[(n, "rstd", s)])
        P.op("dve", lambda v: v.tensor_scalar(out=xs[:], in0=xt, scalar1=rstd[:, 0:1], scalar2=None, op0=ALU.mult),
             reads=[xt_key, (n, "rstd", s)], writes=[(n, "xs", s)])
        for kc in range(8):
            edge = kc in (0, 7)
            P.op("pe", (lambda kc=kc: lambda t: t.transpose(out=pT[:, kc, :], in_=xs[:, kc * 128:(kc + 1) * 128],
                                                            identity=self.ident[:]))(),
                 reads=[(n, "xs", s), (n, "ident")] if edge else (), writes=[(n, "pT", s)] if edge else ())
        P.op("dve", lambda v: v.tensor_tensor(out=tmp[:], in0=pT[:], in1=bc(scaleT.unsqueeze(2), [128, 8, 128]), op=ALU.mult),
             reads=[(n, "pT", s)] + list(par_keys), writes=[(n, "tmp", s)])
        return P.op("pool", lambda g: g.tensor_tensor(out=out, in0=tmp[:], in1=bc(shiftT.unsqueeze(2), [128, 8, 128]), op=ALU.add),
                    reads=[(n, "tmp", s)] + list(par_keys), writes=[out_key])


def load_mod_T(P, nc, st, name, modv, r, l, which, norm_w_row):
    sb = lambda nm, shp: st.enter_context(nc.sbuf_tensor(f"{name}_{nm}", shp, F32))
    shT, scT, nwT, scaleT = sb("shT", [128, 8]), sb("scT", [128, 8]), sb("nwT", [128, 8]), sb("scaleT", [128, 8])
    o = 3 * D * which
    k = f"{name}_ld"
    P.dma("sp", shT[:], modv[r, l, o:o + D].rearrange("(kc p) -> p kc", p=128), key=k, writes=[(name, "shT")],
          allow_slow_non_contiguous=True)
    P.dma("sp", scT[:], modv[r, l, o + D:o + 2 * D].rearrange("(kc p) -> p kc", p=128), key=k, writes=[(name, "scT")],
          allow_slow_non_contiguous=True)
    P.dma("sp", nwT[:], norm_w_row.rearrange("(kc p) -> p kc", p=128), key=k, writes=[(name, "nwT")],
          allow_slow_non_contiguous=True)
    P.op("dve", lambda v: v.scalar_tensor_tensor(out=scaleT[:], in0=scT[:], scalar=1.0, in1=nwT[:], op0=ALU.add, op1=ALU.mult),
         reads=[(name, "scT"), (name, "nwT")], writes=[(name, "scaleT")])
    return scaleT[:, :], shT[:, :], [(name, "scaleT"), (name, "shT")]


def phase_qkv(nc, dr, n_groups=SEQ // 512, n_own=TOK // 512):
    x_b = dr.get("x_b", [SEQ, D], F32)
    x_own = dr.get("x_own", [TOK, D], F32)
    ctx_b = dr.get("ctx_b", [CTX, D], F32)
    modv = dr.get("modv", [2, 2, 6 * D], F32)
    norm1_w = dr.get("norm1_w", [2, D], F32)
    wqkv = dr.get("wqkv", [D, 3 * D], F32)
    wqk_perm = dr.get("wqk_perm", [D, 2 * D], F32)
    cos_all = dr.get("cos_all", [128, SEQ], F32)
    sin_all = dr.get("sin_all", [128, SEQ], F32)
    cos_own = dr.get("cos_own", [128, TOK], F32)
    sin_own = dr.get("sin_own", [128, TOK], F32)
    KT_all = dr.get("KT_all", [H, 128, NKEY], BF16)
    V_all = dr.get("V_all", [128, NKT, H * VW], BF16)
    QT_own = dr.get("QT_own", [H, 128, TOK], BF16)

    P = Prog(nc, "p1")
    with ExitStack() as st:
        sb = lambda nm, shp, dt: st.enter_context(nc.sbuf_tensor(f"p1_{nm}", shp, dt))
        ps = lambda nm, shp, dt: st.enter_context(nc.psum_tensor(f"p1_{nm}", shp, dt))
        nt = NormT(nc, st, "p1n")
        nt.init(P)
        sc_b, sh_b, pk_b = load_mod_T(P, nc, st, "p1mb", modv, 0, 0, 0, norm1_w[0, :])
        sc_c, sh_c, pk_c = load_mod_T(P, nc, st, "p1mc", modv, 1, 0, 0, norm1_w[0, :])
        wv3 = wqkv.rearrange("(kc p) n -> p kc n", p=128)
        wp3 = wqk_perm.rearrange("(kc p) n -> p kc n", p=128)
        Wq, Wk, Wv = sb("Wq", [128, 8, D], BF16), sb("Wk", [128, 8, D], BF16), sb("Wv", [128, 8, D], BF16)
        Wqp, Wkp = sb("Wqp", [128, 8, D], BF16), sb("Wkp", [128, 8, D], BF16)
        for (w, src, nm) in ((Wk, wv3[:, :, D:2 * D], "Wk"), (Wkp, wp3[:, :, D:2 * D], "Wkp"), (Wv, wv3[:, :, 2 * D:3 * D], "Wv"),
                             (Wq, wv3[:, :, 0:D], "Wq"), (Wqp, wp3[:, :, 0:D], "Wqp")):
            for kc in range(8):
                P.dma("pool", w[:, kc, :], src[:, kc, :], key="wld", writes=[nm])
        xt = [sb(f"xt{i}", [128, D], F32) for i in range(2)]
        hT = [sb(f"hT{i}", [128, 8, 512], BF16) for i in range(2)]
        cs = [sb(f"cos{i}", [128, 512], F32) for i in range(2)]
        sn = [sb(f"sin{i}", [128, 512], F32) for i in range(2)]
        t1 = [sb(f"t1_{i}", [128, 512], F32) for i in range(2)]
        t2 = [sb(f"t2_{i}", [128, 512], F32) for i in range(2)]
        ko = [sb(f"ko{i}", [128, 512], BF16) for i in range(2)]
        vsb = [sb(f"vsb{i}", [128, H, VW], BF16) for i in range(2)]
        pk1 = [ps(f"pk1_{i}", [128, 512], F32) for i in range(2)]
        pk2 = [ps(f"pk2_{i}", [128, 512], F32) for i in range(2)]
        pv = [ps(f"pv{i}", [128, 512], F32) for i in range(2)]
        for i in range(2):
            P.op("pool", (lambda i=i: lambda g: g.memset(vsb[i][:], 0.0))(), writes=[("vsb", i)])
            P.op("pool", (lambda i=i: lambda g: g.memset(vsb[i][:, :, 128:129], 1.0))(), reads=[("vsb", i)], writes=[("vsb", i)])

        cnt = {"xt": 0, "k": 0, "v": 0}
        groups = []

        def add_group(src, nsub, scaleT, shiftT, pkeys, mode, tab=None, tok0=0, key_col0=0, kt0=0):
            groups.append(dict(src=src, nsub=nsub, scaleT=scaleT, shiftT=shiftT, pkeys=pkeys, mode=mode, tab=tab, tok0=tok0,
                               key_col0=key_col0, kt0=kt0, gs=len(groups) % 2))

        def a_sub(G, sub):
            gs = G["gs"]
            if sub >= G["nsub"]:
                return
            if sub == 0 and G["mode"] != "ctx":
                P.dma("sp", cs[gs][:], G["tab"][0][:, G["tok0"]:G["tok0"] + 512], key=("tab", gs), writes=[("cs", gs)])
                P.dma("sp", sn[gs][:], G["tab"][1][:, G["tok0"]:G["tok0"] + 512], key=("tab", gs), writes=[("sn", gs)])
            xs_ = cnt["xt"] % 2
            cnt["xt"] += 1
            P.dma("sp", xt[xs_][:], G["src"][sub * 128:(sub + 1) * 128, :], key=("xt", xs_), writes=[("xt", xs_)])
            nt.emit(P, xt[xs_][:], ("xt", xs_), G["scaleT"], G["shiftT"], G["pkeys"], hT[gs][:, :, sub * 128:(sub + 1) * 128], ("hT", gs))

        def part_v(G):
            gs, mode = G["gs"], G["mode"]
            if mode not in ("kv", "ctx"):
                return
            for sub in range(G["nsub"]):
                vs = cnt["v"] % 2
                cnt["v"] += 1
                for half in range(2):
                    pvs = half
                    mm_group(P, pv[pvs][:], [(hT[gs][:, kc, sub * 128:(sub + 1) * 128], Wv[:, kc, half * 512:(half + 1) * 512])
                                              for kc in range(8)], reads=[("hT", gs), "Wv"], writes=[("pv", pvs)])
                    P.op("act", (lambda vs=vs, pvs=pvs, half=half: lambda a: a.copy(
                        out=vsb[vs][:, 4 * half:4 * half + 4, 0:128], in_=pv[pvs][:].rearrange("p (h d) -> p h d", h=4)))(),
                         reads=[("pv", pvs)], writes=[("vsb", vs)])
                P.dma("act", V_all[:, G["kt0"] + sub, :], vsb[vs][:].rearrange("p h d -> p (h d)"), key=("vst", vs), reads=[("vsb", vs)])

        def part_k(G, nxt):
            gs, mode = G["gs"], G["mode"]
            n = G["nsub"] * 128
            tok0 = G["tok0"]
            W, Wp = (Wq, Wqp) if mode == "q" else (Wk, Wkp)
            wn, wpn = ("Wq", "Wqp") if mode == "q" else ("Wk", "Wkp")
            for h in range(H):
                ks = cnt["k"] % 2
                cnt["k"] += 1
                mm_group(P, pk1[ks][:, 0:n], [(W[:, kc, h * 128:(h + 1) * 128], hT[gs][:, kc, 0:n]) for kc in range(8)],
                         reads=[("hT", gs), wn], writes=[("pk1", ks)])
                if mode == "ctx":
                    P.op("act", (lambda ks=ks: lambda a: a.copy(out=ko[ks][:, 0:n], in_=pk1[ks][:, 0:n]))(),
                         reads=[("pk1", ks)], writes=[("ko", ks)])
                else:
                    mm_group(P, pk2[ks][:, 0:n], [(Wp[:, kc, h * 128:(h + 1) * 128], hT[gs][:, kc, 0:n]) for kc in range(8)],
                             reads=[("hT", gs), wpn], writes=[("pk2", ks)])
                    P.op("dve", (lambda ks=ks, gs=gs: lambda v: v.tensor_tensor(out=t1[ks][:], in0=pk1[ks][:], in1=cs[gs][:], op=ALU.mult))(),
                         reads=[("pk1", ks), ("cs", gs)], writes=[("t1", ks)])
                    P.op("dve", (lambda ks=ks, gs=gs: lambda v: v.tensor_tensor(out=t2[ks][:], in0=pk2[ks][:], in1=sn[gs][:], op=ALU.mult))(),
                         reads=[("pk2", ks), ("sn", gs)], writes=[("t2", ks)])
                    P.op("pool", (lambda ks=ks: lambda g: g.tensor_tensor(out=ko[ks][:], in0=t1[ks][:], in1=t2[ks][:], op=ALU.add))(),
                         reads=[("t1", ks), ("t2", ks)], writes=[("ko", ks)])
                dst = QT_own[h, :, tok0:tok0 + n] if mode == "q" else KT_all[h, :, G["key_col0"]:G["key_col0"] + n]
                P.dma("pool", dst, ko[ks][:, 0:n], key=("kst", ks), reads=[("ko", ks)])
                if nxt is not None and h % 2 == 1:
                    a_sub(nxt, h // 2)

        add_group(ctx_b, 2, sc_c, sh_c, pk_c, "ctx", key_col0=0, kt0=0)
        for g in range(n_groups):
            add_group(x_b[g * 512:(g + 1) * 512, :], 4, sc_b, sh_b, pk_b, "kv", tab=(cos_all, sin_all), tok0=g * 512,
                      key_col0=CTX + g * 512, kt0=2 + g * 4)
        for g in range(n_own):
            add_group(x_own[g * 512:(g + 1) * 512, :], 4, sc_b, sh_b, pk_b, "q", tab=(cos_own, sin_own), tok0=g * 512)
        for sub in range(4):
            a_sub(groups[0], sub)
        for gi, G in enumerate(groups):
            nxt = groups[gi + 1] if gi + 1 < len(groups) else None
            part_v(G)
            part_k(G, nxt)
        P.flush()


def phase_attn(nc, dr, n_heads=H, n_qg=TOK // 512, n_kt=NKT, pre=None):
    KT_all = dr.get("KT_all", [H, 128, NKEY], BF16)
    V_all = dr.get("V_all", [128, NKT, H * VW], BF16)
    QT_own = dr.get("QT_own", [H, 128, TOK], BF16)
    lamv = dr.get("lamv", [4, 64], F32)
    subln_w = dr.get("subln_w", [128], F32)
    OT = dr.get("OT", [H, 128, TOK], BF16)

    P = Prog(nc, "p2")
    with ExitStack() as st:
        sb = lambda nm, shp, dt: st.enter_context(nc.sbuf_tensor(f"p2_{nm}", shp, dt))
        ps = lambda nm, shp, dt: st.enter_context(nc.psum_tensor(f"p2_{nm}", shp, dt))
        KT = [sb(f"KT{i}", [128, NKEY], BF16) for i in range(2)]
        V = [sb(f"V{i}", [128, NKT, VW], BF16) for i in range(2)]
        QT = [sb(f"QT{i}", [128, TOK], BF16) for i in range(2)]
        e = [sb(f"e{i}", [128, 2, 512], BF16) for i in range(3)]
        osb = sb("osb", [128, 8, VW], F32)
        rs = sb("rs", [128, 8], F32)
        nlr = sb("nlr", [128, 4], F32)
        o0 = sb("o0", [128, 128], F32)
        o1 = sb("o1", [128, 4, 128], F32)
        sq = sb("sq", [128, 128], F32)
        ms = sb("ms", [128, 4], F32)
        on = sb("on", [128, 4, 128], BF16)
        oTs = [sb(f"oTs{i}", [128, 512], BF16) for i in range(2)]
        ident = sb("ident", [128, 128], BF16)
        lamt = sb("lamt", [128, 4, 64], F32)
        lp = sb("lp", [128, 2, 64], F32)
        ls = sb("ls", [128, 2], F32)
        nlam = sb("nlam", [128, 1], F32)
        swb = sb("swb", [128, 128], F32)
        epsb = sb("epsb", [128, 1], F32)
        S = [ps(f"S{i}", [128, 2, 512], F32) for i in range(2)]
        po = ps("po", [128, 3, 512], F32)
        pTo = ps("pTo", [128, 4, 128], BF16)

        def acc(c, qs):
            i = c * 4 + qs
            return po[:, i // 3, (i % 3) * VW:(i % 3 + 1) * VW]

        P.op("pool", lambda g: g.memset(ident[:], 0.0), writes=["ident"])
        P.op("pool", lambda g: g.affine_select(out=ident[:], in_=ident[:], pattern=[[-1, 128]], compare_op=ALU.not_equal,
                                               fill=1.0, base=0, channel_multiplier=1), reads=["ident"], writes=["ident"])
        P.op("pool", lambda g: g.memset(epsb[:], EPS), writes=["epsb"])
        for i in range(4):
            P.dma("sp", lamt[:, i, :], lamv[i, :].partition_broadcast(128), key="c0", writes=["lamt"])
        P.dma("sp", swb[:], subln_w.partition_broadcast(128), key="c1", writes=["swb"])
        P.op("dve", lambda v: v.tensor_scalar(out=swb[:], in0=swb[:], scalar1=1.0 - LAM_INIT0, scalar2=None, op0=ALU.mult),
             reads=["swb"], writes=["swb"])
        P.op("dve", lambda v: v.tensor_tensor(out=lp[:, 0, :], in0=lamt[:, 0, :], in1=lamt[:, 1, :], op=ALU.mult), reads=["lamt"], writes=["lp"])
        P.op("dve", lambda v: v.tensor_tensor(out=lp[:, 1, :], in0=lamt[:, 2, :], in1=lamt[:, 3, :], op=ALU.mult), reads=["lamt", "lp"], writes=["lp"])
        P.op("dve", lambda v: v.reduce_sum(out=ls[:], in_=lp[:], axis=AX.X), reads=["lp"], writes=["ls"])
        P.op("act", lambda a: a.activation(out=ls[:], in_=ls[:], func=AF.Exp), reads=["ls"], writes=["ls"])
        P.op("dve", lambda v: v.scalar_tensor_tensor(out=nlam[:], in0=ls[:, 1:2], scalar=-LAM_INIT0, in1=ls[:, 0:1],
                                                     op0=ALU.add, op1=ALU.subtract), reads=["ls"], writes=["nlam"])

        def load_head(h):
            hs = h % 2
            nch = 5
            kw = NKEY // nch
            tw = NKT // nch
            for c in range(nch):
                P.dma("sp", KT[hs][:, c * kw:(c + 1) * kw], KT_all[h, :, c * kw:(c + 1) * kw], key=("KT", hs, c), writes=[("KT", hs, c)])
                P.dma("sp", V[hs][:, c * tw:(c + 1) * tw, :], V_all[:, c * tw:(c + 1) * tw, h * VW:(h + 1) * VW], key=("V", hs, c),
                      writes=[("V", hs, c)])
            P.dma("sp", QT[hs][:], QT_own[h, :, :], key=("QT", hs), writes=[("QT", hs)])

        steps = [(h, g, j) for h in range(n_heads) for g in range(n_qg) for j in range(n_kt)]

        def qk(i):
            h, g, j = steps[i]
            hs, ss = h % 2, i % 2
            ev = None
            for c in range(2):
                ev = P.op("pe", (lambda c=c: lambda t: t.matmul(S[ss][:, c, :], lhsT=KT[hs][64 * c:64 * c + 64, j * 128:(j + 1) * 128],
                                                                rhs=QT[hs][64 * c:64 * c + 64, g * 512:(g + 1) * 512],
                                                                start=True, stop=True))(),
                          reads=[("KT", hs, min(4, j // 26)), ("QT", hs)], writes=[("S", ss)])
            return ev

        def finalize_a(h, g):
            for b in range(3):
                nb = 3 if b < 2 else 2
                P.op("dve", (lambda b=b, nb=nb: lambda v: v.tensor_copy(
                    out=osb[:, 3 * b:3 * b + nb, :], in_=po[:, b, 0:nb * VW].rearrange("p (a w) -> p a w", w=VW)))(),
                     reads=["po"], writes=["osb"])
            P.op("dve", lambda v: v.reciprocal(out=rs[:], in_=osb[:, :, 128]), reads=["osb"], writes=["rs"])
            P.op("dve", lambda v: v.tensor_scalar(out=nlr[:], in0=rs[:, 4:8], scalar1=nlam[:, 0:1], scalar2=None, op0=ALU.mult),
                 reads=["rs", "nlam"], writes=["nlr"])
            for qs in range(4):
                P.op("dve", (lambda qs=qs: lambda v: v.tensor_scalar(out=o0[:], in0=osb[:, qs, 0:128], scalar1=rs[:, qs:qs + 1],
                                                                     scalar2=None, op0=ALU.mult))(), reads=["osb", "rs"], writes=["o0"])
                P.op("dve", (lambda qs=qs: lambda v: v.scalar_tensor_tensor(out=o1[:, qs, :], in0=osb[:, 4 + qs, 0:128], scalar=nlr[:, qs:qs + 1],
                                                                            in1=o0[:], op0=ALU.mult, op1=ALU.add))(),
                     reads=["osb", "nlr", "o0"], writes=["o1"])
                P.op("dve", (lambda qs=qs: lambda v: v.tensor_tensor(out=sq[:], in0=o1[:, qs, :], in1=o1[:, qs, :], op=ALU.mult))(),
                     reads=["o1"], writes=["sq"])
                P.op("dve", (lambda qs=qs: lambda v: v.reduce_sum(out=ms[:, qs:qs + 1], in_=sq[:], axis=AX.X))(), reads=["sq"], writes=["ms"])

        def finalize_b1(h, g):
            P.op("act", lambda a: a.activation(out=ms[:], in_=ms[:], func=AF.Ln, scale=1.0 / 128, bias=epsb[:]), reads=["ms", "epsb"], writes=["ms"])
            P.op("act", lambda a: a.activation(out=ms[:], in_=ms[:], func=AF.Exp, scale=-0.5), reads=["ms"], writes=["ms"])
            for qs in range(4):
                P.op("dve", (lambda qs=qs: lambda v: v.scalar_tensor_tensor(out=on[:, qs, :], in0=o1[:, qs, :], scalar=ms[:, qs:qs + 1],
                                                                            in1=swb[:], op0=ALU.mult, op1=ALU.mult))(),
                     reads=["o1", "ms", "swb"], writes=["on"])

        def finalize_b2(h, g, fi):
            for qs in range(4):
                P.op("pe", (lambda qs=qs: lambda t: t.transpose(out=pTo[:, qs, :], in_=on[:, qs, :], identity=ident[:]))(),
                     reads=["on", "ident"] if qs in (0, 3) else (), writes=["pTo"] if qs in (0, 3) else ())
            fs = fi % 2
            P.op("dve", lambda v: v.tensor_copy(out=oTs[fs][:], in_=pTo[:].rearrange("p a b -> p (a b)")), reads=["pTo"], writes=[("oTs", fs)])
            P.dma("pool", OT[h, :, g * 512:(g + 1) * 512], oTs[fs][:], key=("ost", fs), reads=[("oTs", fs)])

        load_head(0)
        if pre is not None:
            pre(P)
        qk(0)
        if len(steps) > 1:
            qk(1)
        fi = 0
        pending = []
        for i, (h, g, j) in enumerate(steps):
            ss = i % 2
            es = i % 3
            hs = h % 2
            if g == 0 and j == 0 and h + 1 < n_heads:
                load_head(h + 1)
            P.op("act", (lambda ss=ss, es=es: lambda a: a.activation(out=e[es][:], in_=S[ss][:], func=AF.Exp))(),
                 reads=[("S", ss)], writes=[("e", es)])
            if i + 2 < len(steps):
                qk(i + 2)
            for c in range(2):
                for qs in range(4):
                    first = (c == 0 and qs == 0)
                    last = (c == 1 and qs == 3)
                    P.op("pe", (lambda c=c, qs=qs, j=j, es=es, hs=hs: lambda t: t.matmul(
                        acc(c, qs), lhsT=e[es][:, c, qs * 128:(qs + 1) * 128], rhs=V[hs][:, j, :],
                        start=(j == 0 and (c * 4 + qs) % 3 == 0), stop=(j == n_kt - 1)))(),
                         reads=[("e", es), ("V", hs, min(4, j // 26))] if (first or last) else (), writes=["po"] if (first or last) else ())
            if j == n_kt - 1:
                finalize_a(h, g)
                pending.append((i + 6, (lambda h=h, g=g: lambda: finalize_b1(h, g))()))
                pending.append((i + 12, (lambda h=h, g=g, fi=fi: lambda: finalize_b2(h, g, fi))()))
                fi += 1
            while pending and pending[0][0] <= i:
                pending.pop(0)[1]()
        for (_, fn) in pending:
            fn()
        P.flush()


def wcast_decl(dr, pfx, n_e, F):
    return (dr.get(f"{pfx}_WG", [n_e, 128, 8, F], BF16), dr.get(f"{pfx}_WU", [n_e, 128, 8, F], BF16),
            dr.get(f"{pfx}_WD", [n_e, 128, F // 128, D], BF16))


def phase_wcast(nc, dr, P, pfx, wg, wu, wd, n_e, F, key, gs=None):
    WG, WU, WD = wcast_decl(dr, pfx, n_e, F)
    for e in range(n_e):
        for (dst, src) in ((WG, wg), (WU, wu)):
            sv = src[e].rearrange("(kc p) f -> p kc f", p=128)
            for kc in range(8):
                if gs is not None:
                    P.dma_g("pool", dst[e, :, kc, :], sv[:, kc, :], gs)
                else:
                    P.dma("pool", dst[e, :, kc, :], sv[:, kc, :], key=key, writes=[(pfx, "w")])
        sv = wd[e].rearrange("(fc p) n -> p fc n", p=128)
        nfc = F // 128
        for f0 in range(0, nfc, 7):
            f1 = min(nfc, f0 + 7)
            if gs is not None:
                P.dma_g("pool", WD[e, :, f0:f1, :], sv[:, f0:f1, :], gs)
            else:
                P.dma("pool", WD[e, :, f0:f1, :], sv[:, f0:f1, :], key=key, writes=[(pfx, "w")])


def phase_wo(nc, dr, n_groups=TOK // 512):
    OT = dr.get("OT", [H, 128, TOK], BF16)
    x_own = dr.get("x_own", [TOK, D], F32)
    w_o = dr.get("w_o", [D, D], F32)
    modv = dr.get("modv", [2, 2, 6 * D], F32)
    X1A = dr.get("X1A", [TOK, D], F32)
    P = Prog(nc, "p3a")
    with ExitStack() as st:
        sb = lambda nm, shp, dt: st.enter_context(nc.sbuf_tensor(f"p3a_{nm}", shp, dt))
        ps = lambda nm, shp, dt: st.enter_context(nc.psum_tensor(f"p3a_{nm}", shp, dt))
        Wo = sb("Wo", [128, 8, D], BF16)
        g1b = sb("g1b", [128, D], F32)
        oT = [sb(f"oT{i}", [128, 8, 512], BF16) for i in range(2)]
        xt = [sb(f"xt{i}", [128, D], F32) for i in range(2)]
        tmp = [sb(f"tmp{i}", [128, D], F32) for i in range(2)]
        xo = [sb(f"xo{i}", [128, D], F32) for i in range(2)]
        py = [ps(f"py{i}", [128, 2, 512], F32) for i in range(2)]
        wv = w_o.rearrange("(h p) n -> p h n", p=128)
        for h in range(8):
            P.dma("pool", Wo[:, h, :], wv[:, h, :], key="w", writes=["Wo"])
        P.dma("sp", g1b[:], modv[0, 0, 2 * D:3 * D].partition_broadcast(128), key="g", writes=["g1b"])
        i = 0
        for g in range(n_groups):
            gs = g % 2
            P.dma("sp", oT[gs][:], OT[:, :, g * 512:(g + 1) * 512].rearrange("h p t -> p h t"), key=("oT", gs), writes=[("oT", gs)])
            for sub in range(4):
                s = i % 2
                i += 1
                r0 = g * 512 + sub * 128
                P.dma("sp", xt[s][:], x_own[r0:r0 + 128, :], key=("xt", s), writes=[("xt", s)])
                for half in range(2):
                    mm_group(P, py[s][:, half, :], [(oT[gs][:, h, sub * 128:(sub + 1) * 128], Wo[:, h, half * 512:(half + 1) * 512])
                                                    for h in range(8)], reads=[("oT", gs), "Wo"], writes=[("py", s)])
                P.op("dve", (lambda s=s: lambda v: v.tensor_tensor(out=tmp[s][:], in0=py[s][:].rearrange("p a b -> p (a b)"),
                                                                   in1=g1b[:], op=ALU.mult))(),
                     reads=[("py", s), "g1b"], writes=[("tmp", s)])
                P.op("pool", (lambda s=s: lambda g_: g_.tensor_tensor(out=xo[s][:], in0=tmp[s][:], in1=xt[s][:], op=ALU.add))(),
                     reads=[("tmp", s), ("xt", s)], writes=[("xo", s)])
                P.dma("act", X1A[r0:r0 + 128, :], xo[s][:], key=("st", s), reads=[("xo", s)])
        P.flush()


class SwigluStream:
    def __init__(self, nc, st, name, nfc_max):
        sb = lambda nm, shp, dt: st.enter_context(nc.sbuf_tensor(f"{name}_{nm}", shp, dt))
        ps = lambda nm, shp, dt: st.enter_context(nc.psum_tensor(f"{name}_{nm}", shp, dt))
        self.n = name
        self.nw = 2
        self.wg = [sb(f"wg{i}", [128, 8, 512], BF16) for i in range(self.nw)]
        self.wu = [sb(f"wu{i}", [128, 8, 512], BF16) for i in range(self.nw)]
        self.wd = [sb(f"wd{i}", [128, nfc_max, 512], BF16) for i in range(2)]
        self.aT = sb("aT", [128, nfc_max, 512], BF16)
        self.sg = [sb(f"sg{i}", [128, 512], F32) for i in range(2)]
        self.pg = [ps(f"pg{i}", [128, 512], F32) for i in range(2)]
        self.pu = [ps(f"pu{i}", [128, 512], F32) for i in range(2)]
        self.pd = [ps(f"pd{i}", [128, 512], F32) for i in range(2)]
        self.ip = 0
        self.ic = 0
        self.idn = 0
        self.ih = 0

    def run(self, P, hT, hT_key, WG_e, WU_e, WD_e, nfc, evac):
        n = self.n
        pieces = [(f0, min(nfc, f0 + 4)) for f0 in range(0, nfc, 4)]
        for (f0, f1) in pieces:
            ws = self.ip % self.nw
            self.ip += 1
            w = (f1 - f0) * 128
            P.dma("sp", self.wg[ws][:, :, 0:w], WG_e[:, :, f0 * 128:f1 * 128], key=(n, "wg", ws), writes=[(n, "wg", ws)])
            P.dma("sp", self.wu[ws][:, :, 0:w], WU_e[:, :, f0 * 128:f1 * 128], key=(n, "wu", ws), writes=[(n, "wu", ws)])
            for fc in range(f0, f1):
                c = self.ic % 2
                self.ic += 1
                o = (fc - f0) * 128
                mm_group(P, self.pg[c][:], [(self.wg[ws][:, kc, o:o + 128], hT[:, kc, :]) for kc in range(8)],
                         reads=[hT_key, (n, "wg", ws)], writes=[(n, "pg", c)])
                mm_group(P, self.pu[c][:], [(self.wu[ws][:, kc, o:o + 128], hT[:, kc, :]) for kc in range(8)],
                         reads=[hT_key, (n, "wu", ws)], writes=[(n, "pu", c)])
                P.op("act", (lambda c=c: lambda a: a.activation(out=self.sg[c][:], in_=self.pg[c][:], func=AF.Silu))(),
                     reads=[(n, "pg", c)], writes=[(n, "sg", c)])
                P.op("dve", (lambda c=c, fc=fc: lambda v: v.tensor_tensor(out=self.aT[:, fc, :], in0=self.pu[c][:], in1=self.sg[c][:],
                                                                          op=ALU.mult))(),
                     reads=[(n, "pu", c), (n, "sg", c)], writes=[(n, "aT")])
        for half in range(2):
            hs = self.ih % 2
            self.ih += 1
            for f0 in range(0, nfc, 7):
                f1 = min(nfc, f0 + 7)
                P.dma("sp", self.wd[hs][:, f0:f1, :], WD_e[:, f0:f1, half * 512:(half + 1) * 512], key=(n, "wd", hs), writes=[(n, "wd", hs)])
            for sub in range(4):
                d = self.idn % 2
                self.idn += 1
                mm_group(P, self.pd[d][:], [(self.aT[:, fc, sub * 128:(sub + 1) * 128], self.wd[hs][:, fc, :]) for fc in range(nfc)],
                         reads=[(n, "aT"), (n, "wd", hs)], writes=[(n, "pd", d)])
                evac(P, sub, half, self.pd[d][:], (n, "pd", d))


def phase_ffn(nc, dr, gsem, n_groups=TOK // 512):
    X1A = dr.get("X1A", [TOK, D], F32)
    X1 = dr.get("X1", [TOK, D], F32)
    modv = dr.get("modv", [2, 2, 6 * D], F32)
    norm2_w = dr.get("norm2_w", [2, D], F32)
    WG, WU, WD = wcast_decl(dr, "ffn", 1, FFN)
    P = Prog(nc, "p3b")
    with ExitStack() as st:
        sb = lambda nm, shp, dt: st.enter_context(nc.sbuf_tensor(f"p3b_{nm}", shp, dt))
        nt = NormT(nc, st, "p3bn")
        nt.init(P)
        scT, shT, pk = load_mod_T(P, nc, st, "p3bm", modv, 0, 0, 1, norm2_w[0, :])
        sw = SwigluStream(nc, st, "p3bs", FFN // 128)
        g2b = sb("g2b", [128, D], F32)
        P.dma("sp", g2b[:], modv[0, 0, 5 * D:6 * D].partition_broadcast(128), key="g", writes=["g2b"])
        xg = [sb(f"xg{i}", [128, 4, D], F32) for i in range(2)]
        hT = [sb(f"hT{i}", [128, 8, 512], BF16) for i in range(2)]
        tmp = [sb(f"tmp{i}", [128, 512], F32) for i in range(2)]
        if gsem is not None:
            P.op("sp", lambda s: s.wait_ge(gsem[0], gsem[1]))
        cnt = {"t": 0}
        for g in range(n_groups):
            gs = g % 2
            for sub in range(4):
                r0 = g * 512 + sub * 128
                P.dma("sp", xg[gs][:, sub, :], X1A[r0:r0 + 128, :], key=("xg", gs), writes=[("xg", gs, sub)])
                nt.emit(P, xg[gs][:, sub, :], ("xg", gs, sub), scT, shT, pk, hT[gs][:, :, sub * 128:(sub + 1) * 128], ("hT", gs))

            def evac(P, sub, half, pap, pkey, g=g, gs=gs):
                t = cnt["t"] % 2
                cnt["t"] += 1
                P.op("dve", lambda v: v.tensor_tensor(out=tmp[t][:], in0=pap, in1=g2b[:, half * 512:(half + 1) * 512], op=ALU.mult),
                     reads=[pkey, "g2b"], writes=[("tmp", t)])
                P.op("pool", lambda g_: g_.tensor_tensor(out=xg[gs][:, sub, half * 512:(half + 1) * 512], in0=tmp[t][:],
                                                         in1=xg[gs][:, sub, half * 512:(half + 1) * 512], op=ALU.add),
                     reads=[("tmp", t), ("xg", gs, sub)], writes=[("xg", gs, sub)])
                if half == 1:
                    r0 = g * 512 + sub * 128
                    P.dma("act", X1[r0:r0 + 128, :], xg[gs][:, sub, :], key=("st", gs), reads=[("xg", gs, sub)])

            sw.run(P, hT[gs], ("hT", gs), WG[0], WU[0], WD[0], FFN // 128, evac)
        P.flush()


def phase_gmlp(nc, dr, n_tiles=TOK // 128):
    X1 = dr.get("X1", [TOK, D], F32)
    X2 = dr.get("X2", [TOK, D], F32)
    modv = dr.get("modv", [2, 2, 6 * D], F32)
    norm1_w = dr.get("norm1_w", [2, D], F32)
    gm_w_in = dr.get("gm_w_in", [D, 4 * D], F32)
    gm_vn_w = dr.get("gm_vn_w", [2 * D], F32)
    gm_vn_b = dr.get("gm_vn_b", [2 * D], F32)
    gm_wsT = dr.get("gm_wsT", [8, 128, 128], F32)
    gm_bsT = dr.get("gm_bsT", [128, 8], F32)
    gm_w_out = dr.get("gm_w_out", [2 * D, D], F32)
    P = Prog(nc, "p4")
    with ExitStack() as st:
        sb = lambda nm, shp, dt: st.enter_context(nc.sbuf_tensor(f"p4_{nm}", shp, dt))
        ps = lambda nm, shp, dt: st.enter_context(nc.psum_tensor(f"p4_{nm}", shp, dt))
        nt = NormT(nc, st, "p4n", nbuf=1)
        nt.init(P)
        scT, shT, pk = load_mod_T(P, nc, st, "p4m", modv, 0, 1, 0, norm1_w[1, :])
        Win = sb("Win", [128, 8, 4 * D], BF16)
        Wout = sb("Wout", [128, 16, D], BF16)
        WsT = sb("WsT", [128, 8, 128], BF16)
        bsT = sb("bsT", [128, 8], F32)
        vnw = sb("vnw", [128, 2 * D], F32)
        vnb = sb("vnb", [128, 2 * D], F32)
        g1b = sb("g1b", [128, D], F32)
        epsb = sb("epsb", [128, 1], F32)
        xt = [sb(f"xt{i}", [128, D], F32) for i in range(3)]
        hT = [sb(f"hT{i}", [128, 8, 128], BF16) for i in range(2)]
        u_sb = [sb(f"u{i}", [128, 2 * D], BF16) for i in range(3)]
        v_sb = [sb(f"v{i}", [128, 2 * D], F32) for i in range(2)]
        vb = [sb(f"vb{i}", [128, 2 * D], BF16) for i in range(2)]
        gt = [sb(f"gt{i}", [128, 2 * D], BF16) for i in range(2)]
        gT = [sb(f"gT{i}", [128, 16, 128], BF16) for i in range(2)]
        stats = [sb(f"stats{i}", [128, 4, 6], F32) for i in range(2)]
        mv = [sb(f"mv{i}", [128, 2], F32) for i in range(2)]
        rstd = [sb(f"rstd{i}", [128, 1], F32) for i in range(2)]
        xo = [sb(f"xo{i}", [128, D], F32) for i in range(2)]
        pz = [ps(f"pz{i}", [128, 512], F32) for i in range(2)]
        psS = [ps(f"ps{i}", [128, 512], F32) for i in range(2)]
        pgT = [ps(f"pgT{i}", [128, 8, 128], BF16) for i in range(2)]
        py = ps("py", [128, 512], F32)
        wv = gm_w_in.rearrange("(kc p) n -> p kc n", p=128)
        for kc in range(8):
            for q in range(2):
                P.dma("pool", Win[:, kc, q * 2048:(q + 1) * 2048], wv[:, kc, q * 2048:(q + 1) * 2048], key="w", writes=["Win"])
        wo = gm_w_out.rearrange("(fc p) n -> p fc n", p=128)
        for fc in range(0, 16, 4):
            P.dma("pool", Wout[:, fc:fc + 4, :], wo[:, fc:fc + 4, :], key="w", writes=["Wout"])
        P.dma("pool", WsT[:], gm_wsT.rearrange("g q p -> q g p"), key="w", writes=["WsT"])
        P.dma("sp", bsT[:], gm_bsT[:, :], key="c", writes=["bsT"])
        P.dma("sp", vnw[:], gm_vn_w.partition_broadcast(128), key="c", writes=["vnw"])
        P.dma("sp", vnb[:], gm_vn_b.partition_broadcast(128), key="c", writes=["vnb"])
        P.dma("sp", g1b[:], modv[0, 1, 2 * D:3 * D].partition_broadcast(128), key="c", writes=["g1b"])
        P.op("pool", lambda g: g.memset(epsb[:], EPS), writes=["epsb"])
        cz = {"z": 0}

        def s1a(t):
            s3, s2 = t % 3, t % 2
            r0 = t * 128
            P.dma("sp", xt[s3][:], X1[r0:r0 + 128, :], key=("xt", s3), writes=[("xt", s3)])
            nt.emit(P, xt[s3][:], ("xt", s3), scT, shT, pk, hT[s2][:], ("hT", s2))

        def s1b(t):
            s3, s2 = t % 3, t % 2
            for cg in range(8):
                z = cz["z"] % 2
                cz["z"] += 1
                mm_group(P, pz[z][:], [(hT[s2][:, kc, :], Win[:, kc, cg * 512:(cg + 1) * 512]) for kc in range(8)],
                         reads=[("hT", s2), "Win"], writes=[("pz", z)])
                dst = u_sb[s3][:, cg * 512:(cg + 1) * 512] if cg < 4 else v_sb[s2][:, (cg - 4) * 512:(cg - 3) * 512]
                P.op("act", (lambda z=z, dst=dst: lambda a: a.activation(out=dst, in_=pz[z][:], func=AF.Gelu))(),
                     reads=[("pz", z)], writes=[("u", s3) if cg < 4 else ("v", s2)])

        def s2(t):
            s = t % 2
            for c in range(4):
                P.op("dve", (lambda c=c: lambda v: v.bn_stats(out=stats[s][:, c, :], in_=v_sb[s][:, c * 512:(c + 1) * 512]))(),
                     reads=[("v", s)], writes=[("stats", s)])
            P.op("dve", lambda v: v.bn_aggr(out=mv[s][:], in_=stats[s][:].rearrange("p a b -> p (a b)")), reads=[("stats", s)], writes=[("mv", s)])
            P.op("act", lambda a: a.activation(out=rstd[s][:], in_=mv[s][:, 1:2], func=AF.Sqrt, bias=epsb[:]),
                 reads=[("mv", s), "epsb"], writes=[("rstd", s)])
            P.op("dve", lambda v: v.reciprocal(out=rstd[s][:], in_=rstd[s][:]), reads=[("rstd", s)], writes=[("rstd", s)])
            P.op("dve", lambda v: v.tensor_scalar(out=v_sb[s][:], in0=v_sb[s][:], scalar1=mv[s][:, 0:1], scalar2=rstd[s][:, 0:1],
                                                  op0=ALU.subtract, op1=ALU.mult), reads=[("v", s), ("mv", s), ("rstd", s)], writes=[("v", s)])
            P.op("pool", lambda g: g.tensor_tensor(out=v_sb[s][:], in0=v_sb[s][:], in1=vnw[:], op=ALU.mult), reads=[("v", s), "vnw"], writes=[("v", s)])
            P.op("pool", lambda g: g.tensor_tensor(out=vb[s][:], in0=v_sb[s][:], in1=vnb[:], op=ALU.add), reads=[("v", s), "vnb"], writes=[("vb", s)])

        def s3a(t):
            s, s3 = t % 2, t % 3
            for gp in range(4):
                sp_ = gp % 2
                for k in range(2):
                    g_ = gp * 2 + k
                    P.op("pe", (lambda g_=g_, k=k, sp_=sp_: lambda t_: t_.matmul(psS[sp_][:, k * 256:(k + 1) * 256], lhsT=WsT[:, g_, :],
                                                                               rhs=vb[s][:, g_ * 256:(g_ + 1) * 256], start=(k == 0), stop=True))(),
                         reads=[("vb", s), "WsT"], writes=[("psS", sp_)])
                for k in range(2):
                    g_ = gp * 2 + k
                    P.op("dve", (lambda g_=g_, k=k, sp_=sp_: lambda v: v.scalar_tensor_tensor(
                        out=gt[s][:, g_ * 256:(g_ + 1) * 256], in0=psS[sp_][:, k * 256:(k + 1) * 256], scalar=bsT[:, g_:g_ + 1],
                        in1=u_sb[s3][:, g_ * 256:(g_ + 1) * 256], op0=ALU.add, op1=ALU.mult))(),
                         reads=[("psS", sp_), "bsT", ("u", s3)], writes=[("gt", s)])

        def s3b(t):
            s, s3 = t % 2, t % 3
            r0 = t * 128
            for hb in range(2):
                for k in range(8):
                    fc = hb * 8 + k
                    P.op("pe", (lambda fc=fc, k=k, hb=hb: lambda t_: t_.transpose(out=pgT[hb][:, k, :], in_=gt[s][:, fc * 128:(fc + 1) * 128],
                                                                                 identity=nt.ident[:]))(),
                         reads=[("gt", s), (nt.n, "ident")] if k in (0, 7) else (), writes=[("pgT", hb)] if k in (0, 7) else ())
                P.op("act", (lambda hb=hb: lambda a: a.copy(out=gT[s][:, hb * 8:(hb + 1) * 8, :], in_=pgT[hb][:]))(),
                     reads=[("pgT", hb)], writes=[("gT", s)])
            for half in range(2):
                mm_group(P, py[:], [(gT[s][:, fc, :], Wout[:, fc, half * 512:(half + 1) * 512]) for fc in range(16)],
                         reads=[("gT", s), "Wout"], writes=["py"])
                P.op("dve", (lambda half=half: lambda v: v.tensor_tensor(out=xo[s][:, half * 512:(half + 1) * 512], in0=py[:],
                                                                         in1=g1b[:, half * 512:(half + 1) * 512], op=ALU.mult))(),
                     reads=["py", "g1b"], writes=[("xo", s)])
            P.op("pool", lambda g: g.tensor_tensor(out=xo[s][:], in0=xo[s][:], in1=xt[s3][:], op=ALU.add),
                 reads=[("xo", s), ("xt", s3)], writes=[("xo", s)])
            P.dma("act", X2[r0:r0 + 128, :], xo[s][:], key=("st", s), reads=[("xo", s)])

        for k in range(n_tiles + 2):
            if k < n_tiles:
                s1a(k)
            if 0 <= k - 2 < n_tiles:
                s3a(k - 2)
            if k < n_tiles:
                s1b(k)
            if 0 <= k - 2 < n_tiles:
                s3b(k - 2)
            if 0 <= k - 1 < n_tiles:
                s2(k - 1)
        P.flush()


def phase_moe(nc, dr, gsem, n_groups=TOK // 512, n_exp=NEXP):
    X2 = dr.get("X2", [TOK, D], F32)
    OUT = dr.get("out", [TOK, D], F32)
    modv = dr.get("modv", [2, 2, 6 * D], F32)
    norm2_w = dr.get("norm2_w", [2, D], F32)
    final_w = dr.get("final_norm_w", [D], F32)
    w_router = dr.get("moe_w_router", [D, NEXP], F32)
    WG, WU, WD = wcast_decl(dr, "moe", NEXP, EXP_D)
    P = Prog(nc, "p5")
    with ExitStack() as st:
        sb = lambda nm, shp, dt: st.enter_context(nc.sbuf_tensor(f"p5_{nm}", shp, dt))
        ps = lambda nm, shp, dt: st.enter_context(nc.psum_tensor(f"p5_{nm}", shp, dt))
        nt = NormT(nc, st, "p5n", nbuf=1)
        nt.init(P)
        scT, shT, pk = load_mod_T(P, nc, st, "p5m", modv, 0, 1, 1, norm2_w[1, :])
        sw = SwigluStream(nc, st, "p5s", EXP_D // 128)
        g2b = sb("g2b", [128, D], F32)
        fnb = sb("fnb", [128, D], F32)
        P.dma("sp", g2b[:], modv[0, 1, 5 * D:6 * D].partition_broadcast(128), key="g", writes=["g2b"])
        P.dma("sp", fnb[:], final_w.partition_broadcast(128), key="g", writes=["fnb"])
        xg = sb("xg", [128, 4, D], F32)
        yacc = sb("yacc", [128, 4, D], F32)
        hT = sb("hT", [128, 8, 512], BF16)
        identf = sb("identf", [128, 128], F32)
        wr = sb("wr", [128, 8, NEXP], F32)
        wrs = sb("wrs", [128, 8, NEXP], F32)
        shTb = sb("shTb", [128, 8, 128], F32)
        rbias = sb("rbias", [128, NEXP], F32)
        xT32 = sb("xT32", [128, 8, 128], F32)
        lg = sb("lg", [128, 4, NEXP], F32)
        m8 = sb("m8", [128, 4, 8], F32)
        nm1 = sb("nm1", [128, 4], F32)
        msk = sb("msk", [128, 4, NEXP], F32)
        ex = sb("ex", [128, 4, NEXP], F32)
        den = sb("den", [128, 4], F32)
        gates = sb("gates", [128, 4, NEXP], F32)
        ss = sb("ss", [128, 1], F32)
        rstd = sb("rstd", [128, 1], F32)
        junk = sb("junk", [128, D], BF16)
        epsb = sb("epsb", [128, 1], F32)
        prt = ps("prt", [128, 4, 128], F32)
        P.op("pool", lambda g: g.memset(identf[:], 0.0), writes=["identf"])
        P.op("pool", lambda g: g.affine_select(out=identf[:], in_=identf[:], pattern=[[-1, 128]], compare_op=ALU.not_equal,
                                               fill=1.0, base=0, channel_multiplier=1), reads=["identf"], writes=["identf"])
        P.op("pool", lambda g: g.memset(epsb[:], EPS), writes=["epsb"])
        P.dma("sp", wr[:], w_router.rearrange("(kc p) e -> p kc e", p=128), key="g", writes=["wr"])
        P.op("dve", lambda v: v.tensor_tensor(out=wrs[:], in0=wr[:], in1=bc(scT.unsqueeze(2), [128, 8, NEXP]), op=ALU.mult),
             reads=["wr"] + pk, writes=["wrs"])
        P.op("dve", lambda v: v.tensor_copy(out=shTb[:], in_=bc(shT.unsqueeze(2), [128, 8, 128])), reads=pk, writes=["shTb"])
        mm_group(P, prt[:, 0, 0:NEXP], [(shTb[:, kc, :], wr[:, kc, :]) for kc in range(8)], reads=["shTb", "wr"], writes=["prt"])
        P.op("dve", lambda v: v.tensor_copy(out=rbias[:], in_=prt[:, 0, 0:NEXP]), reads=["prt"], writes=["rbias"])
        if gsem is not None:
            P.op("sp", lambda s: s.wait_ge(gsem[0], gsem[1]))
        for g in range(n_groups):
            for sub in range(4):
                r0 = g * 512 + sub * 128
                P.dma("sp", xg[:, sub, :], X2[r0:r0 + 128, :], key="xg", writes=[("xg", sub)])
                nt.emit(P, xg[:, sub, :], ("xg", sub), scT, shT, pk, hT[:, :, sub * 128:(sub + 1) * 128], "hT")
                rs_ap, rs_key = nt.last_rstd
                for hb in range(2):
                    for k in range(4):
                        kc = hb * 4 + k
                        P.op("pe", (lambda kc=kc, k=k, sub=sub: lambda t: t.matmul(prt[:, k, :], lhsT=xg[:, sub, kc * 128:(kc + 1) * 128],
                                                                                  rhs=identf[:], start=(k == 0), stop=True))(),
                             reads=[("xg", sub), "identf"] if k in (0, 3) else (), writes=["prt"] if k in (0, 3) else ())
                    P.op("dve", (lambda hb=hb: lambda v: v.tensor_copy(out=xT32[:, hb * 4:(hb + 1) * 4, :], in_=prt[:]))(),
                         reads=["prt"], writes=["xT32"])
                mm_group(P, prt[:, 0, 0:NEXP], [(xT32[:, kc, :], wrs[:, kc, :]) for kc in range(8)], reads=["xT32", "wrs"], writes=["prt"])
                P.op("dve", (lambda sub=sub, rs_ap=rs_ap: lambda v: v.scalar_tensor_tensor(
                    out=lg[:, sub, :], in0=prt[:, 0, 0:NEXP], scalar=rs_ap[:, 0:1], in1=rbias[:], op0=ALU.mult, op1=ALU.add))(),
                     reads=["prt", rs_key, "rbias"], writes=["lg"])
            for sub in range(4):
                P.op("dve", (lambda sub=sub: lambda v: v.max(out=m8[:, sub, :], in_=lg[:, sub, :]))(), reads=["lg"], writes=["m8"])
            P.op("dve", lambda v: v.tensor_scalar(out=nm1[:], in0=m8[:, :, 0], scalar1=-1.0, scalar2=None, op0=ALU.mult), reads=["m8"], writes=["nm1"])
            for sub in range(4):
                P.op("dve", (lambda sub=sub: lambda v: v.tensor_scalar(out=msk[:, sub, :], in0=lg[:, sub, :], scalar1=m8[:, sub, 1:2], scalar2=None,
                                                                      op0=ALU.is_ge))(), reads=["lg", "m8"], writes=["msk"])
                P.op("act", (lambda sub=sub: lambda a: a.activation(out=ex[:, sub, :], in_=lg[:, sub, :], func=AF.Exp, bias=nm1[:, sub:sub + 1]))(),
                     reads=["lg", "nm1"], writes=["ex"])
            P.op("dve", lambda v: v.tensor_tensor(out=ex[:], in0=ex[:], in1=msk[:], op=ALU.mult), reads=["ex", "msk"], writes=["ex"])
            P.op("dve", lambda v: v.reduce_sum(out=den[:], in_=ex[:], axis=AX.X), reads=["ex"], writes=["den"])
            P.op("dve", lambda v: v.reciprocal(out=den[:], in_=den[:]), reads=["den"], writes=["den"])
            P.op("dve", lambda v: v.tensor_tensor(out=gates[:], in0=ex[:], in1=bc(den[:, :].unsqueeze(2), [128, 4, NEXP]), op=ALU.mult),
                 reads=["ex", "den"], writes=["gates"])
            for e in range(n_exp):
                def evac(P, sub, half, pap, pkey, e=e):
                    dst = yacc[:, sub, half * 512:(half + 1) * 512]
                    if e == 0:
                        P.op("dve", lambda v: v.tensor_scalar(out=dst, in0=pap, scalar1=gates[:, sub, e:e + 1], scalar2=None, op0=ALU.mult),
                             reads=[pkey, "gates"], writes=[("yacc", sub, half)])
                    else:
                        P.op("dve", lambda v: v.scalar_tensor_tensor(out=dst, in0=pap, scalar=gates[:, sub, e:e + 1], in1=dst,
                                                                     op0=ALU.mult, op1=ALU.add),
                             reads=[pkey, "gates", ("yacc", sub, half)], writes=[("yacc", sub, half)])
                sw.run(P, hT, "hT", WG[e], WU[e], WD[e], EXP_D // 128, evac)
            for sub in range(4):
                r0 = g * 512 + sub * 128
                P.op("pool", (lambda sub=sub: lambda g_: g_.tensor_tensor(out=yacc[:, sub, :], in0=yacc[:, sub, :], in1=g2b[:], op=ALU.mult))(),
                     reads=[("yacc", sub, 0), ("yacc", sub, 1), "g2b"], writes=[("yacc", sub, 0), ("yacc", sub, 1)])
                P.op("pool", (lambda sub=sub: lambda g_: g_.tensor_tensor(out=xg[:, sub, :], in0=yacc[:, sub, :], in1=xg[:, sub, :], op=ALU.add))(),
                     reads=[("yacc", sub, 0), ("yacc", sub, 1), ("xg", sub)], writes=[("xg", sub)])
                P.op("act", (lambda sub=sub: lambda a: a.activation(out=junk[:], in_=xg[:, sub, :], func=AF.Square, accum_out=ss[:]))(),
                     reads=[("xg", sub)], writes=["junk", "ss"])
                P.op("act", lambda a: a.activation(out=ss[:], in_=ss[:], func=AF.Sqrt, scale=1.0 / D, bias=epsb[:]), reads=["ss", "epsb"], writes=["ss"])
                P.op("dve", lambda v: v.reciprocal(out=rstd[:], in_=ss[:]), reads=["ss"], writes=["rstd"])
                P.op("dve", (lambda sub=sub: lambda v: v.scalar_tensor_tensor(out=yacc[:, sub, :], in0=xg[:, sub, :], scalar=rstd[:, 0:1], in1=fnb[:],
                                                                             op0=ALU.mult, op1=ALU.mult))(),
                     reads=[("xg", sub), "rstd", "fnb"], writes=[("yacc", sub, 0), ("yacc", sub, 1)])
                P.dma("act", OUT[r0:r0 + 128, :], yacc[:, sub, :], key="ost", reads=[("yacc", sub, 0), ("yacc", sub, 1)])
        P.flush()


EXT_IN = ["x_b", "x_own", "ctx_b", "cvec", "w_mod", "b_mod", "norm1_w", "norm2_w", "final_norm_w", "wqkv", "wqk_perm",
          "cos_all", "sin_all", "cos_own", "sin_own", "lamv", "subln_w", "w_o", "ffn_wg", "ffn_wu", "ffn_wd",
          "gm_w_in", "gm_vn_w", "gm_vn_b", "gm_wsT", "gm_bsT", "gm_w_out", "moe_w_router", "moe_wg", "moe_wu", "moe_wd"]


def build_program():
    nc = bass.Bass("TRN2", target_bir_lowering=False)
    dr = Dram(nc, ext_in=EXT_IN, ext_out=["out"])
    fwg = dr.get("ffn_wg", [1, D, FFN], F32)
    fwu = dr.get("ffn_wu", [1, D, FFN], F32)
    fwd = dr.get("ffn_wd", [1, FFN, D], F32)
    mwg = dr.get("moe_wg", [NEXP, D, EXP_D], F32)
    mwu = dr.get("moe_wu", [NEXP, D, EXP_D], F32)
    mwd = dr.get("moe_wd", [NEXP, EXP_D, D], F32)
    with nc.semaphore("g_wcast") as gw:
        gs = {"sem": gw, "count": 0}

        def casts(P):
            phase_wcast(nc, dr, P, "ffn", fwg, fwu, fwd, 1, FFN, "wc", gs=gs)
            phase_wcast(nc, dr, P, "moe", mwg, mwu, mwd, NEXP, EXP_D, "wc", gs=gs)

        phase_mod(nc, dr)
        phase_qkv(nc, dr)
        phase_attn(nc, dr, pre=casts)
        gsem = (gw, gs["count"])
        phase_wo(nc, dr)
        phase_ffn(nc, dr, gsem)
        phase_gmlp(nc, dr)
        phase_moe(nc, dr, gsem)
    return nc


def _perm_cols():
    idx = np.arange(D)
    d = idx % 64
    partner = np.where(d % 32 < 16, d + 16, d - 16)
    return (idx // 64) * 64 + partner


def _rope_tables():
    t = np.arange(SEQ)
    row = (t // 64).astype(np.float32)
    col = (t % 64).astype(np.float32)
    inv_freq = (np.float32(10000.0) ** (-np.arange(16, dtype=np.float32) / np.float32(16))).astype(np.float32)
    d = np.arange(128) % 64
    pos = np.where((d >= 32)[:, None], col[None, :], row[None, :]).astype(np.float32)
    ang = (pos * inv_freq[d % 16][:, None]).astype(np.float32)
    sgn = np.where(d % 32 < 16, -1.0, 1.0).astype(np.float32)
    return np.cos(ang).astype(np.float32), (np.sin(ang) * sgn[:, None]).astype(np.float32)


def make_in_maps(inp):
    f32 = lambda a: np.ascontiguousarray(np.asarray(a, dtype=np.float32))
    x, c, ctx = f32(inp["x"]), f32(inp["c"]), f32(inp["ctx"])
    wqkv = f32(inp["da_w_qkv"])[0]
    pc = _perm_cols()
    wqk_perm = np.ascontiguousarray(np.concatenate([wqkv[:, 0:D][:, pc], wqkv[:, D:2 * D][:, pc]], axis=1))
    cos_all, sin_all = _rope_tables()
    lamv = np.stack([f32(inp["da_lambda_q1"])[0], f32(inp["da_lambda_k1"])[0], f32(inp["da_lambda_q2"])[0], f32(inp["da_lambda_k2"])[0]])
    shared = dict(
        w_mod=f32(inp["w_mod"]), b_mod=f32(inp["b_mod"]), norm1_w=f32(inp["norm1_w"]), norm2_w=f32(inp["norm2_w"]),
        final_norm_w=f32(inp["final_norm_w"]), wqkv=wqkv, wqk_perm=wqk_perm, cos_all=cos_all, sin_all=sin_all,
        lamv=f32(lamv), subln_w=f32(inp["da_subln_w"])[0], w_o=f32(inp["da_w_o"])[0],
        ffn_wg=f32(inp["ffn_w_gate"]), ffn_wu=f32(inp["ffn_w_up"]), ffn_wd=f32(inp["ffn_w_down"]),
        gm_w_in=f32(inp["gm_w_in"])[0], gm_vn_w=f32(inp["gm_vnorm_w"])[0], gm_vn_b=f32(inp["gm_vnorm_b"])[0],
        gm_wsT=np.ascontiguousarray(f32(inp["gm_w_s"])[0].transpose(0, 2, 1)), gm_bsT=np.ascontiguousarray(f32(inp["gm_b_s"])[0].T),
        gm_w_out=f32(inp["gm_w_out"])[0], moe_w_router=f32(inp["moe_w_router"])[0],
        moe_wg=f32(inp["moe_w_gate"])[0], moe_wu=f32(inp["moe_w_up"])[0], moe_wd=f32(inp["moe_w_down"])[0],
    )
    maps = []
    for core in range(8):
        b, r = core // 4, core % 4
        t0 = r * TOK
        m = dict(shared)
        m["x_b"] = x[b]
        m["x_own"] = np.ascontiguousarray(x[b, t0:t0 + TOK])
        m["ctx_b"] = ctx[b]
        m["cvec"] = np.ascontiguousarray(np.stack([c[b], f32(inp["c_ctx"])]))
        m["cos_own"] = np.ascontiguousarray(cos_all[:, t0:t0 + TOK] * np.float32(0.125))
        m["sin_own"] = np.ascontiguousarray(sin_all[:, t0:t0 + TOK] * np.float32(0.125))
        maps.append(m)
    return maps


def kernel(**inputs):
    maps = make_in_maps(inputs)
    nc = build_program()
    res = run_bass_kernel_spmd(nc, maps, core_ids=list(range(8)))
    out = np.empty((2, SEQ, D), np.float32)
    for core in range(8):
        b, r = core // 4, core % 4
        out[b, r * TOK:(r + 1) * TOK] = res.results[core]["out"]
    return out
```

```python
import math
from contextlib import ExitStack

import numpy as np
import concourse.bass as bass
import concourse.mybir as mybir
from concourse.bass_utils import run_bass_kernel_spmd

F32 = mybir.dt.float32
BF16 = mybir.dt.bfloat16
AF = mybir.ActivationFunctionType
ALU = mybir.AluOpType
AX = mybir.AxisListType

D = 1024
SEQ = 16384
CTX = 256
NKEY = SEQ + CTX
NKT = NKEY // 128
TOK = 4096
H = 8
FFN = 2816
EXP_D = 3584
NEXP = 8
EPS = 1e-6
VW = 130
LAM_INIT0 = 0.8 - 0.6 * math.exp(-0.3 * 0)


class Ev:
    __slots__ = ("eng", "needed", "value", "key")

    def __init__(self, eng, key=None):
        self.eng = eng
        self.needed = False
        self.value = None
        self.key = key


class Prog:
    ENGS = ("pe", "act", "dve", "pool", "sp")

    def __init__(self, nc, name):
        self.nc = nc
        self.name = name
        self.ops = {e: [] for e in self.ENGS}
        self.lastw = {}
        self.readers = {}
        self.dma_cnt = {}

    def _deps(self, reads, writes, dma_key=None):
        deps = []
        for k in reads:
            e = self.lastw.get(k)
            if e is not None:
                deps.append(e)
        for k in writes:
            e = self.lastw.get(k)
            if e is not None:
                deps.append(e)
            deps.extend(self.readers.get(k, {}).values())
        out = []
        for d in deps:
            if d.eng == "dma":
                if dma_key is not None and d.key == dma_key:
                    continue
                if d.value != self.dma_cnt[d.key]:
                    d2 = Ev("dma", d.key)
                    d2.value = self.dma_cnt[d.key]
                    d = d2
            d.needed = True
            out.append(d)
        return out

    def _commit(self, ev, reads, writes):
        rk = ev.key if ev.eng == "dma" else ev.eng
        for k in reads:
            self.readers.setdefault(k, {})[rk] = ev
        for k in writes:
            self.lastw[k] = ev
            self.readers[k] = {}

    def op(self, eng, fn, reads=(), writes=(), extra=()):
        deps = self._deps(reads, writes)
        for d in extra:
            if d is not None:
                d.needed = True
                deps.append(d)
        ev = Ev(eng)
        self.ops[eng].append((fn, deps, ev))
        self._commit(ev, reads, writes)
        return ev

    def dma(self, q, out, in_, key, reads=(), writes=(), **kw):
        deps = self._deps(reads, writes, dma_key=key)
        ev = Ev("dma", key)
        self.dma_cnt[key] = self.dma_cnt.get(key, 0) + 16
        ev.value = self.dma_cnt[key]
        ev.needed = True
        self.ops[q].append((lambda e: e.dma_start(out=out, in_=in_, **kw), deps, ev))
        self._commit(ev, reads, writes)
        return ev

    def dma_g(self, q, out, in_, gs, **kw):
        ev = Ev("gdma", gs["sem"])
        gs["count"] += 16
        self.ops[q].append((lambda e: e.dma_start(out=out, in_=in_, **kw), [], ev))
        return ev

    def flush(self):
        nc = self.nc
        with ExitStack() as st:
            sems = {e: st.enter_context(nc.semaphore(f"{self.name}_s_{e}")) for e in ("pe", "act", "dve", "pool")}
            dsems = {}
            for i, k in enumerate(self.dma_cnt):
                dsems[k] = st.enter_context(nc.semaphore(f"{self.name}_d{i}"))
            for e in self.ENGS:
                c = 0
                for (_, _, ev) in self.ops[e]:
                    if ev.eng not in ("dma", "gdma") and ev.needed:
                        c += 1
                        ev.value = c
            block = st.enter_context(nc.Block())

            def run(e, eo):
                waited = {}
                for (fn, deps, ev) in self.ops[e]:
                    for d in deps:
                        if d.eng == "dma":
                            sk = ("d", d.key)
                            s = dsems[d.key]
                        else:
                            if d.eng == "pe" and e == "pe":
                                continue
                            sk = ("e", d.eng)
                            s = sems[d.eng]
                        if waited.get(sk, 0) >= d.value:
                            continue
                        eo.wait_ge(s, d.value)
                        waited[sk] = d.value
                    ins = fn(eo)
                    if ev.eng == "dma":
                        ins.then_inc(dsems[ev.key], 16)
                    elif ev.eng == "gdma":
                        ins.then_inc(ev.key, 16)
                    elif ev.needed:
                        ins.then_inc(sems[e], 1)
                if e == "sp":
                    for k, v in self.dma_cnt.items():
                        if waited.get(("d", k), 0) < v:
                            eo.wait_ge(dsems[k], v)

            @block.tensor
            def _(t):
                run("pe", t)

            @block.scalar
            def _(a):
                run("act", a)

            @block.vector
            def _(v):
                run("dve", v)

            @block.gpsimd
            def _(g):
                run("pool", g)

            @block.sync
            def _(s):
                run("sp", s)


def mm_group(P, out, pairs, reads, writes):
    n = len(pairs)
    ev = None
    for i, (l, r) in enumerate(pairs):
        fn = (lambda l=l, r=r, i=i: (lambda t: t.matmul(out, lhsT=l, rhs=r, start=(i == 0), stop=(i == n - 1))))()
        edge = (i == 0 or i == n - 1)
        ev = P.op("pe", fn, reads=reads if edge else (), writes=writes if edge else ())
    return ev


class Dram:
    def __init__(self, nc, ext_in=(), ext_out=()):
        self.nc = nc
        self.ext_in = set(ext_in)
        self.ext_out = set(ext_out)
        self.t = {}

    def get(self, name, shape, dtype, kind=None):
        if name in self.t:
            return self.t[name]
        if kind is None:
            kind = "ExternalInput" if name in self.ext_in else ("ExternalOutput" if name in self.ext_out else "Internal")
        ap = self.nc.dram_tensor(name, list(shape), dtype, kind=kind).ap()
        self.t[name] = ap
        return ap


def bc(ap, shape):
    return ap.to_broadcast(list(shape))


def phase_mod(nc, dr, tail=None):
    cvec = dr.get("cvec", [2, D], F32)
    w_mod = dr.get("w_mod", [2, D, 6 * D], F32)
    b_mod = dr.get("b_mod", [2, 6 * D], F32)
    modv = dr.get("modv", [2, 2, 6 * D], F32)
    P = Prog(nc, "p0")
    with ExitStack() as st:
        cT = st.enter_context(nc.sbuf_tensor("p0_cT", [128, 8, 2], F32))
        bm = st.enter_context(nc.sbuf_tensor("p0_bm", [2, 2, 6 * D], F32))
        mrow = st.enter_context(nc.sbuf_tensor("p0_mrow", [2, 2, 6 * D], F32))
        wblk = [st.enter_context(nc.sbuf_tensor(f"p0_w{i}", [128, 8, 512], F32)) for i in range(2)]
        pm = [st.enter_context(nc.psum_tensor(f"p0_pm{i}", [2, 512], F32)) for i in range(2)]
        for r in range(2):
            P.dma("sp", cT[:, :, r], cvec[r, :].rearrange("(kc p) -> p kc", p=128), key="c", writes=["cT"],
                  allow_slow_non_contiguous=True)
        for l in range(2):
            P.dma("sp", bm[:, l, :], b_mod[l, :].partition_broadcast(2),
                  key="bm", writes=[("bm", l)])
        P.op("act", lambda a: a.activation(out=cT[:], in_=cT[:], func=AF.Silu), reads=["cT"], writes=["cT"])
        i = 0
        for l in range(2):
            wv = w_mod[l].rearrange("(kc p) n -> p kc n", p=128)
            for blk in range(12):
                s = i % 2
                P.dma("sp", wblk[s][:], wv[:, :, blk * 512:(blk + 1) * 512], key=("w", s), writes=[("w", s)])
                mm_group(P, pm[s][:], [(cT[:, kc, :], wblk[s][:, kc, :]) for kc in range(8)],
                         reads=["cT", ("w", s)], writes=[("pm", s)])
                P.op("dve", (lambda s=s, l=l, blk=blk: lambda v: v.tensor_tensor(
                    out=mrow[:, l, blk * 512:(blk + 1) * 512], in0=pm[s][:], in1=bm[:, l, blk * 512:(blk + 1) * 512],
                    op=ALU.add))(), reads=[("pm", s), ("bm", l)], writes=["mrow"])
                i += 1
        P.dma("sp", modv[:, :, :], mrow[:], key="out", reads=["mrow"])
        if tail is not None:
            tail(P)
        P.flush()


class NormT:
    def __init__(self, nc, st, name, nbuf=2):
        self.nc = nc
        self.n = name
        self.nbuf = nbuf
        sb = lambda nm, shp, dt: st.enter_context(nc.sbuf_tensor(f"{name}_{nm}", shp, dt))
        self.xs = [sb(f"xs{i}", [128, D], BF16) for i in range(nbuf)]
        self.junk = [sb(f"junk{i}", [128, D], BF16) for i in range(nbuf)]
        self.ss = [sb(f"ss{i}", [128, 1], F32) for i in range(nbuf)]
        self.rstd = [sb(f"rstd{i}", [128, 1], F32) for i in range(nbuf)]
        self.tmp = [sb(f"tmp{i}", [128, 8, 128], F32) for i in range(nbuf)]
        self.ident = sb("ident", [128, 128], BF16)
        self.eps = sb("eps", [128, 1], F32)
        self.pT = [st.enter_context(nc.psum_tensor(f"{name}_pT{i}", [128, 8, 128], BF16)) for i in range(nbuf)]
        self.i = 0

    def init(self, P):
        n = self.n
        P.op("pool", lambda g: g.memset(self.ident[:], 0.0), writes=[(n, "ident")])
        P.op("pool", lambda g: g.affine_select(out=self.ident[:], in_=self.ident[:], pattern=[[-1, 128]],
                                               compare_op=ALU.not_equal, fill=1.0, base=0, channel_multiplier=1),
             reads=[(n, "ident")], writes=[(n, "ident")])
        P.op("pool", lambda g: g.memset(self.eps[:], EPS), writes=[(n, "eps")])

    def emit(self, P, xt, xt_key, scaleT, shiftT, par_keys, out, out_key, mean_div=float(D)):
        n = self.n
        s = self.i % self.nbuf
        self.i += 1
        self.last_rstd = (self.rstd[s], (n, "rstd", s))
        xs, junk, ss, rstd, tmp, pT = self.xs[s], self.junk[s], self.ss[s], self.rstd[s], self.tmp[s], self.pT[s]
        P.op("act", lambda a: a.activation(out=junk[:], in_=xt, func=AF.Square, accum_out=ss[:]),
             reads=[xt_key], writes=[(n, "junk", s), (n, "ss", s)])
        P.op("act", lambda a: a.activation(out=ss[:], in_=ss[:], func=AF.Sqrt, scale=1.0 / mean_div, bias=self.eps[:]),
             reads=[(n, "ss", s), (n, "eps")], writes=[(n, "ss", s)])
        P.op("dve", lambda v: v.reciprocal(out=rstd[:], in_=ss[:]), reads=[(n, "ss", s)], writes=[(n, "rstd", s)])
        P.op("dve", lambda v: v.tensor_scalar(out=xs[:], in0=xt, scalar1=rstd[:, 0:1], scalar2=None, op0=ALU.mult),
             reads=[xt_key, (n, "rstd", s)], writes=[(n, "xs", s)])
        for kc in range(8):
            edge = kc in (0, 7)
            P.op("pe", (lambda kc=kc: lambda t: t.transpose(out=pT[:, kc, :], in_=xs[:, kc * 128:(kc + 1) * 128],
                                                            identity=self.ident[:]))(),
                 reads=[(n, "xs", s), (n, "ident")] if edge else (), writes=[(n, "pT", s)] if edge else ())
        P.op("dve", lambda v: v.tensor_tensor(out=tmp[:], in0=pT[:], in1=bc(scaleT.unsqueeze(2), [128, 8, 128]), op=ALU.mult),
             reads=[(n, "pT", s)] + list(par_keys), writes=[(n, "tmp", s)])
        return P.op("pool", lambda g: g.tensor_tensor(out=out, in0=tmp[:], in1=bc(shiftT.unsqueeze(2), [128, 8, 128]), op=ALU.add),
                    reads=[(n, "tmp", s)] + list(par_keys), writes=[out_key])


def load_mod_T(P, nc, st, name, modv, r, l, which, norm_w_row):
    sb = lambda nm, shp: st.enter_context(nc.sbuf_tensor(f"{name}_{nm}", shp, F32))
    shT, scT, nwT, scaleT = sb("shT", [128, 8]), sb("scT", [128, 8]), sb("nwT", [128, 8]), sb("scaleT", [128, 8])
    o = 3 * D * which
    k = f"{name}_ld"
    P.dma("sp", shT[:], modv[r, l, o:o + D].rearrange("(kc p) -> p kc", p=128), key=k, writes=[(name, "shT")],
          allow_slow_non_contiguous=True)
    P.dma("sp", scT[:], modv[r, l, o + D:o + 2 * D].rearrange("(kc p) -> p kc", p=128), key=k, writes=[(name, "scT")],
          allow_slow_non_contiguous=True)
    P.dma("sp", nwT[:], norm_w_row.rearrange("(kc p) -> p kc", p=128), key=k, writes=[(name, "nwT")],
          allow_slow_non_contiguous=True)
    P.op("dve", lambda v: v.scalar_tensor_tensor(out=scaleT[:], in0=scT[:], scalar=1.0, in1=nwT[:], op0=ALU.add, op1=ALU.mult),
         reads=[(name, "scT"), (name, "nwT")], writes=[(name, "scaleT")])
    return scaleT[:, :], shT[:, :], [(name, "scaleT"), (name, "shT")]


def phase_qkv(nc, dr, n_groups=SEQ // 512, n_own=TOK // 512):
    x_b = dr.get("x_b", [SEQ, D], F32)
    x_own = dr.get("x_own", [TOK, D], F32)
    ctx_b = dr.get("ctx_b", [CTX, D], F32)
    modv = dr.get("modv", [2, 2, 6 * D], F32)
    norm1_w = dr.get("norm1_w", [2, D], F32)
    wqkv = dr.get("wqkv", [D, 3 * D], F32)
    wqk_perm = dr.get("wqk_perm", [D, 2 * D], F32)
    cos_all = dr.get("cos_all", [128, SEQ], F32)
    sin_all = dr.get("sin_all", [128, SEQ], F32)
    cos_own = dr.get("cos_own", [128, TOK], F32)
    sin_own = dr.get("sin_own", [128, TOK], F32)
    KT_all = dr.get("KT_all", [H, 128, NKEY], BF16)
    V_all = dr.get("V_all", [128, NKT, H * VW], BF16)
    QT_own = dr.get("QT_own", [H, 128, TOK], BF16)

    P = Prog(nc, "p1")
    with ExitStack() as st:
        sb = lambda nm, shp, dt: st.enter_context(nc.sbuf_tensor(f"p1_{nm}", shp, dt))
        ps = lambda nm, shp, dt: st.enter_context(nc.psum_tensor(f"p1_{nm}", shp, dt))
        nt = NormT(nc, st, "p1n")
        nt.init(P)
        sc_b, sh_b, pk_b = load_mod_T(P, nc, st, "p1mb", modv, 0, 0, 0, norm1_w[0, :])
        sc_c, sh_c, pk_c = load_mod_T(P, nc, st, "p1mc", modv, 1, 0, 0, norm1_w[0, :])
        wv3 = wqkv.rearrange("(kc p) n -> p kc n", p=128)
        wp3 = wqk_perm.rearrange("(kc p) n -> p kc n", p=128)
        Wq, Wk, Wv = sb("Wq", [128, 8, D], BF16), sb("Wk", [128, 8, D], BF16), sb("Wv", [128, 8, D], BF16)
        Wqp, Wkp = sb("Wqp", [128, 8, D], BF16), sb("Wkp", [128, 8, D], BF16)
        for (w, src, nm) in ((Wk, wv3[:, :, D:2 * D], "Wk"), (Wkp, wp3[:, :, D:2 * D], "Wkp"), (Wv, wv3[:, :, 2 * D:3 * D], "Wv"),
                             (Wq, wv3[:, :, 0:D], "Wq"), (Wqp, wp3[:, :, 0:D], "Wqp")):
            for kc in range(8):
                P.dma("pool", w[:, kc, :], src[:, kc, :], key="wld", writes=[nm])
        xt = [sb(f"xt{i}", [128, D], F32) for i in range(2)]
        hT = [sb(f"hT{i}", [128, 8, 512], BF16) for i in range(2)]
        cs = [sb(f"cos{i}", [128, 512], F32) for i in range(2)]
        sn = [sb(f"sin{i}", [128, 512], F32) for i in range(2)]
        t1 = [sb(f"t1_{i}", [128, 512], F32) for i in range(2)]
        t2 = [sb(f"t2_{i}", [128, 512], F32) for i in range(2)]
        ko = [sb(f"ko{i}", [128, 512], BF16) for i in range(2)]
        vsb = [sb(f"vsb{i}", [128, H, VW], BF16) for i in range(2)]
        pk1 = [ps(f"pk1_{i}", [128, 512], F32) for i in range(2)]
        pk2 = [ps(f"pk2_{i}", [128, 512], F32) for i in range(2)]
        pv = [ps(f"pv{i}", [128, 512], F32) for i in range(2)]
        for i in range(2):
            P.op("pool", (lambda i=i: lambda g: g.memset(vsb[i][:], 0.0))(), writes=[("vsb", i)])
            P.op("pool", (lambda i=i: lambda g: g.memset(vsb[i][:, :, 128:129], 1.0))(), reads=[("vsb", i)], writes=[("vsb", i)])

        cnt = {"xt": 0, "k": 0, "v": 0}
        groups = []

        def add_group(src, nsub, scaleT, shiftT, pkeys, mode, tab=None, tok0=0, key_col0=0, kt0=0):
            groups.append(dict(src=src, nsub=nsub, scaleT=scaleT, shiftT=shiftT, pkeys=pkeys, mode=mode, tab=tab, tok0=tok0,
                               key_col0=key_col0, kt0=kt0, gs=len(groups) % 2))

        def a_sub(G, sub):
            gs = G["gs"]
            if sub >= G["nsub"]:
                return
            if sub == 0 and G["mode"] != "ctx":
                P.dma("sp", cs[gs][:], G["tab"][0][:, G["tok0"]:G["tok0"] + 512], key=("tab", gs), writes=[("cs", gs)])
                P.dma("sp", sn[gs][:], G["tab"][1][:, G["tok0"]:G["tok0"] + 512], key=("tab", gs), writes=[("sn", gs)])
            xs_ = cnt["xt"] % 2
            cnt["xt"] += 1
            P.dma("sp", xt[xs_][:], G["src"][sub * 128:(sub + 1) * 128, :], key=("xt", xs_), writes=[("xt", xs_)])
            nt.emit(P, xt[xs_][:], ("xt", xs_), G["scaleT"], G["shiftT"], G["pkeys"], hT[gs][:, :, sub * 128:(sub + 1) * 128], ("hT", gs))

        def part_v(G):
            gs, mode = G["gs"], G["mode"]
            if mode not in ("kv", "ctx"):
                return
            for sub in range(G["nsub"]):
                vs = cnt["v"] % 2
                cnt["v"] += 1
                for half in range(2):
                    pvs = half
                    mm_group(P, pv[pvs][:], [(hT[gs][:, kc, sub * 128:(sub + 1) * 128], Wv[:, kc, half * 512:(half + 1) * 512])
                                              for kc in range(8)], reads=[("hT", gs), "Wv"], writes=[("pv", pvs)])
                    P.op("act", (lambda vs=vs, pvs=pvs, half=half: lambda a: a.copy(
                        out=vsb[vs][:, 4 * half:4 * half + 4, 0:128], in_=pv[pvs][:].rearrange("p (h d) -> p h d", h=4)))(),
                         reads=[("pv", pvs)], writes=[("vsb", vs)])
                P.dma("act", V_all[:, G["kt0"] + sub, :], vsb[vs][:].rearrange("p h d -> p (h d)"), key=("vst", vs), reads=[("vsb", vs)])

        def part_k(G, nxt):
            gs, mode = G["gs"], G["mode"]
            n = G["nsub"] * 128
            tok0 = G["tok0"]
            W, Wp = (Wq, Wqp) if mode == "q" else (Wk, Wkp)
            wn, wpn = ("Wq", "Wqp") if mode == "q" else ("Wk", "Wkp")
            for h in range(H):
                ks = cnt["k"] % 2
                cnt["k"] += 1
                mm_group(P, pk1[ks][:, 0:n], [(W[:, kc, h * 128:(h + 1) * 128], hT[gs][:, kc, 0:n]) for kc in range(8)],
                         reads=[("hT", gs), wn], writes=[("pk1", ks)])
                if mode == "ctx":
                    P.op("act", (lambda ks=ks: lambda a: a.copy(out=ko[ks][:, 0:n], in_=pk1[ks][:, 0:n]))(),
                         reads=[("pk1", ks)], writes=[("ko", ks)])
                else:
                    mm_group(P, pk2[ks][:, 0:n], [(Wp[:, kc, h * 128:(h + 1) * 128], hT[gs][:, kc, 0:n]) for kc in range(8)],
                             reads=[("hT", gs), wpn], writes=[("pk2", ks)])
                    P.op("dve", (lambda ks=ks, gs=gs: lambda v: v.tensor_tensor(out=t1[ks][:], in0=pk1[ks][:], in1=cs[gs][:], op=ALU.mult))(),
                         reads=[("pk1", ks), ("cs", gs)], writes=[("t1", ks)])
                    P.op("dve", (lambda ks=ks, gs=gs: lambda v: v.tensor_tensor(out=t2[ks][:], in0=pk2[ks][:], in1=sn[gs][:], op=ALU.mult))(),
                         reads=[("pk2", ks), ("sn", gs)], writes=[("t2", ks)])
                    P.op("pool", (lambda ks=ks: lambda g: g.tensor_tensor(out=ko[ks][:], in0=t1[ks][:], in1=t2[ks][:], op=ALU.add))(),
                         reads=[("t1", ks), ("t2", ks)], writes=[("ko", ks)])
                dst = QT_own[h, :, tok0:tok0 + n] if mode == "q" else KT_all[h, :, G["key_col0"]:G["key_col0"] + n]
                P.dma("pool", dst, ko[ks][:, 0:n], key=("kst", ks), reads=[("ko", ks)])
                if nxt is not None and h % 2 == 1:
                    a_sub(nxt, h // 2)

        add_group(ctx_b, 2, sc_c, sh_c, pk_c, "ctx", key_col0=0, kt0=0)
        for g in range(n_groups):
            add_group(x_b[g * 512:(g + 1) * 512, :], 4, sc_b, sh_b, pk_b, "kv", tab=(cos_all, sin_all), tok0=g * 512,
                      key_col0=CTX + g * 512, kt0=2 + g * 4)
        for g in range(n_own):
            add_group(x_own[g * 512:(g + 1) * 512, :], 4, sc_b, sh_b, pk_b, "q", tab=(cos_own, sin_own), tok0=g * 512)
        for sub in range(4):
            a_sub(groups[0], sub)
        for gi, G in enumerate(groups):
            nxt = groups[gi + 1] if gi + 1 < len(groups) else None
            part_v(G)
            part_k(G, nxt)
        P.flush()


def phase_attn(nc, dr, n_heads=H, n_qg=TOK // 512, n_kt=NKT, pre=None):
    KT_all = dr.get("KT_all", [H, 128, NKEY], BF16)
    V_all = dr.get("V_all", [128, NKT, H * VW], BF16)
    QT_own = dr.get("QT_own", [H, 128, TOK], BF16)
    lamv = dr.get("lamv", [4, 64], F32)
    subln_w = dr.get("subln_w", [128], F32)
    OT = dr.get("OT", [H, 128, TOK], BF16)

    P = Prog(nc, "p2")
    with ExitStack() as st:
        sb = lambda nm, shp, dt: st.enter_context(nc.sbuf_tensor(f"p2_{nm}", shp, dt))
        ps = lambda nm, shp, dt: st.enter_context(nc.psum_tensor(f"p2_{nm}", shp, dt))
        KT = [sb(f"KT{i}", [128, NKEY], BF16) for i in range(2)]
        V = [sb(f"V{i}", [128, NKT, VW], BF16) for i in range(2)]
        QT = [sb(f"QT{i}", [128, TOK], BF16) for i in range(2)]
        e = [sb(f"e{i}", [128, 2, 512], BF16) for i in range(3)]
        osb = sb("osb", [128, 8, VW], F32)
        rs = sb("rs", [128, 8], F32)
        nlr = sb("nlr", [128, 4], F32)
        o0 = sb("o0", [128, 128], F32)
        o1 = sb("o1", [128, 4, 128], F32)
        sq = sb("sq", [128, 128], F32)
        ms = sb("ms", [128, 4], F32)
        on = sb("on", [128, 4, 128], BF16)
        oTs = [sb(f"oTs{i}", [128, 512], BF16) for i in range(2)]
        ident = sb("ident", [128, 128], BF16)
        lamt = sb("lamt", [128, 4, 64], F32)
        lp = sb("lp", [128, 2, 64], F32)
        ls = sb("ls", [128, 2], F32)
        nlam = sb("nlam", [128, 1], F32)
        swb = sb("swb", [128, 128], F32)
        epsb = sb("epsb", [128, 1], F32)
        S = [ps(f"S{i}", [128, 2, 512], F32) for i in range(2)]
        po = ps("po", [128, 3, 512], F32)
        pTo = ps("pTo", [128, 4, 128], BF16)

        def acc(c, qs):
            i = c * 4 + qs
            return po[:, i // 3, (i % 3) * VW:(i % 3 + 1) * VW]

        P.op("pool", lambda g: g.memset(ident[:], 0.0), writes=["ident"])
        P.op("pool", lambda g: g.affine_select(out=ident[:], in_=ident[:], pattern=[[-1, 128]], compare_op=ALU.not_equal,
                                               fill=1.0, base=0, channel_multiplier=1), reads=["ident"], writes=["ident"])
        P.op("pool", lambda g: g.memset(epsb[:], EPS), writes=["epsb"])
        for i in range(4):
            P.dma("sp", lamt[:, i, :], lamv[i, :].partition_broadcast(128), key="c0", writes=["lamt"])
        P.dma("sp", swb[:], subln_w.partition_broadcast(128), key="c1", writes=["swb"])
        P.op("dve", lambda v: v.tensor_scalar(out=swb[:], in0=swb[:], scalar1=1.0 - LAM_INIT0, scalar2=None, op0=ALU.mult),
             reads=["swb"], writes=["swb"])
        P.op("dve", lambda v: v.tensor_tensor(out=lp[:, 0, :], in0=lamt[:, 0, :], in1=lamt[:, 1, :], op=ALU.mult), reads=["lamt"], writes=["lp"])
        P.op("dve", lambda v: v.tensor_tensor(out=lp[:, 1, :], in0=lamt[:, 2, :], in1=lamt[:, 3, :], op=ALU.mult), reads=["lamt", "lp"], writes=["lp"])
        P.op("dve", lambda v: v.reduce_sum(out=ls[:], in_=lp[:], axis=AX.X), reads=["lp"], writes=["ls"])
        P.op("act", lambda a: a.activation(out=ls[:], in_=ls[:], func=AF.Exp), reads=["ls"], writes=["ls"])
        P.op("dve", lambda v: v.scalar_tensor_tensor(out=nlam[:], in0=ls[:, 1:2], scalar=-LAM_INIT0, in1=ls[:, 0:1],
                                                     op0=ALU.add, op1=ALU.subtract), reads=["ls"], writes=["nlam"])

        def load_head(h):
            hs = h % 2
            nch = 5
            kw = NKEY // nch
            tw = NKT // nch
            for c in range(nch):
                P.dma("sp", KT[hs][:, c * kw:(c + 1) * kw], KT_all[h, :, c * kw:(c + 1) * kw], key=("KT", hs, c), writes=[("KT", hs, c)])
                P.dma("sp", V[hs][:, c * tw:(c + 1) * tw, :], V_all[:, c * tw:(c + 1) * tw, h * VW:(h + 1) * VW], key=("V", hs, c),
                      writes=[("V", hs, c)])
            P.dma("sp", QT[hs][:], QT_own[h, :, :], key=("QT", hs), writes=[("QT", hs)])

        steps = [(h, g, j) for h in range(n_heads) for g in range(n_qg) for j in range(n_kt)]

        def qk(i):
            h, g, j = steps[i]
            hs, ss = h % 2, i % 2
            ev = None
            for c in range(2):
                ev = P.op("pe", (lambda c=c: lambda t: t.matmul(S[ss][:, c, :], lhsT=KT[hs][64 * c:64 * c + 64, j * 128:(j + 1) * 128],
                                                                rhs=QT[hs][64 * c:64 * c + 64, g * 512:(g + 1) * 512],
                                                                start=True, stop=True))(),
                          reads=[("KT", hs, min(4, j // 26)), ("QT", hs)], writes=[("S", ss)])
            return ev

        def finalize_a(h, g):
            for b in range(3):
                nb = 3 if b < 2 else 2
                P.op("dve", (lambda b=b, nb=nb: lambda v: v.tensor_copy(
                    out=osb[:, 3 * b:3 * b + nb, :], in_=po[:, b, 0:nb * VW].rearrange("p (a w) -> p a w", w=VW)))(),
                     reads=["po"], writes=["osb"])
            P.op("dve", lambda v: v.reciprocal(out=rs[:], in_=osb[:, :, 128]), reads=["osb"], writes=["rs"])
            P.op("dve", lambda v: v.tensor_scalar(out=nlr[:], in0=rs[:, 4:8], scalar1=nlam[:, 0:1], scalar2=None, op0=ALU.mult),
                 reads=["rs", "nlam"], writes=["nlr"])
            for qs in range(4):
                P.op("dve", (lambda qs=qs: lambda v: v.tensor_scalar(out=o0[:], in0=osb[:, qs, 0:128], scalar1=rs[:, qs:qs + 1],
                                                                     scalar2=None, op0=ALU.mult))(), reads=["osb", "rs"], writes=["o0"])
                P.op("dve", (lambda qs=qs: lambda v: v.scalar_tensor_tensor(out=o1[:, qs, :], in0=osb[:, 4 + qs, 0:128], scalar=nlr[:, qs:qs + 1],
                                                                            in1=o0[:], op0=ALU.mult, op1=ALU.add))(),
                     reads=["osb", "nlr", "o0"], writes=["o1"])
                P.op("dve", (lambda qs=qs: lambda v: v.tensor_tensor(out=sq[:], in0=o1[:, qs, :], in1=o1[:, qs, :], op=ALU.mult))(),
                     reads=["o1"], writes=["sq"])
                P.op("dve", (lambda qs=qs: lambda v: v.reduce_sum(out=ms[:, qs:qs + 1], in_=sq[:], axis=AX.X))(), reads=["sq"], writes=["ms"])

        def finalize_b1(h, g):
            P.op("act", lambda a: a.activation(out=ms[:], in_=ms[:], func=AF.Ln, scale=1.0 / 128, bias=epsb[:]), reads=["ms", "epsb"], writes=["ms"])
            P.op("act", lambda a: a.activation(out=ms[:], in_=ms[:], func=AF.Exp, scale=-0.5), reads=["ms"], writes=["ms"])
            for qs in range(4):
                P.op("dve", (lambda qs=qs: lambda v: v.scalar_tensor_tensor(out=on[:, qs, :], in0=o1[:, qs, :], scalar=ms[:, qs:qs + 1],
                                                                            in1=swb[:], op0=ALU.mult, op1=ALU.mult))(),
                     reads=["o1", "ms", "swb"], writes=["on"])

        def finalize_b2(h, g, fi):
            for qs in range(4):
                P.op("pe", (lambda qs=qs: lambda t: t.transpose(out=pTo[:, qs, :], in_=on[:, qs, :], identity=ident[:]))(),
                     reads=["on", "ident"] if qs in (0, 3) else (), writes=["pTo"] if qs in (0, 3) else ())
            fs = fi % 2
            P.op("dve", lambda v: v.tensor_copy(out=oTs[fs][:], in_=pTo[:].rearrange("p a b -> p (a b)")), reads=["pTo"], writes=[("oTs", fs)])
            P.dma("pool", OT[h, :, g * 512:(g + 1) * 512], oTs[fs][:], key=("ost", fs), reads=[("oTs", fs)])

        load_head(0)
        if pre is not None:
            pre(P)
        qk(0)
        if len(steps) > 1:
            qk(1)
        fi = 0
        pending = []
        for i, (h, g, j) in enumerate(steps):
            ss = i % 2
            es = i % 3
            hs = h % 2
            if g == 0 and j == 0 and h + 1 < n_heads:
                load_head(h + 1)
            P.op("act", (lambda ss=ss, es=es: lambda a: a.activation(out=e[es][:], in_=S[ss][:], func=AF.Exp))(),
                 reads=[("S", ss)], writes=[("e", es)])
            if i + 2 < len(steps):
                qk(i + 2)
            for c in range(2):
                for qs in range(4):
                    first = (c == 0 and qs == 0)
                    last = (c == 1 and qs == 3)
                    P.op("pe", (lambda c=c, qs=qs, j=j, es=es, hs=hs: lambda t: t.matmul(
                        acc(c, qs), lhsT=e[es][:, c, qs * 128:(qs + 1) * 128], rhs=V[hs][:, j, :],
                        start=(j == 0 and (c * 4 + qs) % 3 == 0), stop=(j == n_kt - 1)))(),
                         reads=[("e", es), ("V", hs, min(4, j // 26))] if (first or last) else (), writes=["po"] if (first or last) else ())
            if j == n_kt - 1:
                finalize_a(h, g)
                pending.append((i + 6, (lambda h=h, g=g: lambda: finalize_b1(h, g))()))
                pending.append((i + 12, (lambda h=h, g=g, fi=fi: lambda: finalize_b2(h, g, fi))()))
                fi += 1
            while pending and pending[0][0] <= i:
                pending.pop(0)[1]()
        for (_, fn) in pending:
            fn()
        P.flush()


def wcast_decl(dr, pfx, n_e, F):
    return (dr.get(f"{pfx}_WG", [n_e, 128, 8, F], BF16), dr.get(f"{pfx}_WU", [n_e, 128, 8, F], BF16),
            dr.get(f"{pfx}_WD", [n_e, 128, F // 128, D], BF16))


def phase_wcast(nc, dr, P, pfx, wg, wu, wd, n_e, F, key, gs=None):
    WG, WU, WD = wcast_decl(dr, pfx, n_e, F)
    for e in range(n_e):
        for (dst, src) in ((WG, wg), (WU, wu)):
            sv = src[e].rearrange("(kc p) f -> p kc f", p=128)
            for kc in range(8):
                if gs is not None:
                    P.dma_g("pool", dst[e, :, kc, :], sv[:, kc, :], gs)
                else:
                    P.dma("pool", dst[e, :, kc, :], sv[:, kc, :], key=key, writes=[(pfx, "w")])
        sv = wd[e].rearrange("(fc p) n -> p fc n", p=128)
        nfc = F // 128
        for f0 in range(0, nfc, 7):
            f1 = min(nfc, f0 + 7)
            if gs is not None:
                P.dma_g("pool", WD[e, :, f0:f1, :], sv[:, f0:f1, :], gs)
            else:
                P.dma("pool", WD[e, :, f0:f1, :], sv[:, f0:f1, :], key=key, writes=[(pfx, "w")])


def phase_wo(nc, dr, n_groups=TOK // 512):
    OT = dr.get("OT", [H, 128, TOK], BF16)
    x_own = dr.get("x_own", [TOK, D], F32)
    w_o = dr.get("w_o", [D, D], F32)
    modv = dr.get("modv", [2, 2, 6 * D], F32)
    X1A = dr.get("X1A", [TOK, D], F32)
    P = Prog(nc, "p3a")
    with ExitStack() as st:
        sb = lambda nm, shp, dt: st.enter_context(nc.sbuf_tensor(f"p3a_{nm}", shp, dt))
        ps = lambda nm, shp, dt: st.enter_context(nc.psum_tensor(f"p3a_{nm}", shp, dt))
        Wo = sb("Wo", [128, 8, D], BF16)
        g1b = sb("g1b", [128, D], F32)
        oT = [sb(f"oT{i}", [128, 8, 512], BF16) for i in range(2)]
        xt = [sb(f"xt{i}", [128, D], F32) for i in range(2)]
        tmp = [sb(f"tmp{i}", [128, D], F32) for i in range(2)]
        xo = [sb(f"xo{i}", [128, D], F32) for i in range(2)]
        py = [ps(f"py{i}", [128, 2, 512], F32) for i in range(2)]
        wv = w_o.rearrange("(h p) n -> p h n", p=128)
        for h in range(8):
            P.dma("pool", Wo[:, h, :], wv[:, h, :], key="w", writes=["Wo"])
        P.dma("sp", g1b[:], modv[0, 0, 2 * D:3 * D].partition_broadcast(128), key="g", writes=["g1b"])
        i = 0
        for g in range(n_groups):
            gs = g % 2
            P.dma("sp", oT[gs][:], OT[:, :, g * 512:(g + 1) * 512].rearrange("h p t -> p h t"), key=("oT", gs), writes=[("oT", gs)])
            for sub in range(4):
                s = i % 2
                i += 1
                r0 = g * 512 + sub * 128
                P.dma("sp", xt[s][:], x_own[r0:r0 + 128, :], key=("xt", s), writes=[("xt", s)])
                for half in range(2):
                    mm_group(P, py[s][:, half, :], [(oT[gs][:, h, sub * 128:(sub + 1) * 128], Wo[:, h, half * 512:(half + 1) * 512])
                                                    for h in range(8)], reads=[("oT", gs), "Wo"], writes=[("py", s)])
                P.op("dve", (lambda s=s: lambda v: v.tensor_tensor(out=tmp[s][:], in0=py[s][:].rearrange("p a b -> p (a b)"),
                                                                   in1=g1b[:], op=ALU.mult))(),
                     reads=[("py", s), "g1b"], writes=[("tmp", s)])
                P.op("pool", (lambda s=s: lambda g_: g_.tensor_tensor(out=xo[s][:], in0=tmp[s][:], in1=xt[s][:], op=ALU.add))(),
                     reads=[("tmp", s), ("xt", s)], writes=[("xo", s)])
                P.dma("act", X1A[r0:r0 + 128, :], xo[s][:], key=("st", s), reads=[("xo", s)])
        P.flush()


class SwigluStream:
    def __init__(self, nc, st, name, nfc_max):
        sb = lambda nm, shp, dt: st.enter_context(nc.sbuf_tensor(f"{name}_{nm}", shp, dt))
        ps = lambda nm, shp, dt: st.enter_context(nc.psum_tensor(f"{name}_{nm}", shp, dt))
        self.n = name
        self.nw = 2
        self.wg = [sb(f"wg{i}", [128, 8, 512], BF16) for i in range(self.nw)]
        self.wu = [sb(f"wu{i}", [128, 8, 512], BF16) for i in range(self.nw)]
        self.wd = [sb(f"wd{i}", [128, nfc_max, 512], BF16) for i in range(2)]
        self.aT = sb("aT", [128, nfc_max, 512], BF16)
        self.sg = [sb(f"sg{i}", [128, 512], F32) for i in range(2)]
        self.pg = [ps(f"pg{i}", [128, 512], F32) for i in range(2)]
        self.pu = [ps(f"pu{i}", [128, 512], F32) for i in range(2)]
        self.pd = [ps(f"pd{i}", [128, 512], F32) for i in range(2)]
        self.ip = 0
        self.ic = 0
        self.idn = 0
        self.ih = 0

    def run(self, P, hT, hT_key, WG_e, WU_e, WD_e, nfc, evac):
        n = self.n
        pieces = [(f0, min(nfc, f0 + 4)) for f0 in range(0, nfc, 4)]
        for (f0, f1) in pieces:
            ws = self.ip % self.nw
            self.ip += 1
            w = (f1 - f0) * 128
            P.dma("sp", self.wg[ws][:, :, 0:w], WG_e[:, :, f0 * 128:f1 * 128], key=(n, "wg", ws), writes=[(n, "wg", ws)])
            P.dma("sp", self.wu[ws][:, :, 0:w], WU_e[:, :, f0 * 128:f1 * 128], key=(n, "wu", ws), writes=[(n, "wu", ws)])
            for fc in range(f0, f1):
                c = self.ic % 2
                self.ic += 1
                o = (fc - f0) * 128
                mm_group(P, self.pg[c][:], [(self.wg[ws][:, kc, o:o + 128], hT[:, kc, :]) for kc in range(8)],
                         reads=[hT_key, (n, "wg", ws)], writes=[(n, "pg", c)])
                mm_group(P, self.pu[c][:], [(self.wu[ws][:, kc, o:o + 128], hT[:, kc, :]) for kc in range(8)],
                         reads=[hT_key, (n, "wu", ws)], writes=[(n, "pu", c)])
                P.op("act", (lambda c=c: lambda a: a.activation(out=self.sg[c][:], in_=self.pg[c][:], func=AF.Silu))(),
                     reads=[(n, "pg", c)], writes=[(n, "sg", c)])
                P.op("dve", (lambda c=c, fc=fc: lambda v: v.tensor_tensor(out=self.aT[:, fc, :], in0=self.pu[c][:], in1=self.sg[c][:],
                                                                          op=ALU.mult))(),
                     reads=[(n, "pu", c), (n, "sg", c)], writes=[(n, "aT")])
        for half in range(2):
            hs = self.ih % 2
            self.ih += 1
            for f0 in range(0, nfc, 7):
                f1 = min(nfc, f0 + 7)
                P.dma("sp", self.wd[hs][:, f0:f1, :], WD_e[:, f0:f1, half * 512:(half + 1) * 512], key=(n, "wd", hs), writes=[(n, "wd", hs)])
            for sub in range(4):
                d = self.idn % 2
                self.idn += 1
                mm_group(P, self.pd[d][:], [(self.aT[:, fc, sub * 128:(sub + 1) * 128], self.wd[hs][:, fc, :]) for fc in range(nfc)],
                         reads=[(n, "aT"), (n, "wd", hs)], writes=[(n, "pd", d)])
                evac(P, sub, half, self.pd[d][:], (n, "pd", d))


def phase_ffn(nc, dr, gsem, n_groups=TOK // 512):
    X1A = dr.get("X1A", [TOK, D], F32)
    X1 = dr.get("X1", [TOK, D], F32)
    modv = dr.get("modv", [2, 2, 6 * D], F32)
    norm2_w = dr.get("norm2_w", [2, D], F32)
    WG, WU, WD = wcast_decl(dr, "ffn", 1, FFN)
    P = Prog(nc, "p3b")
    with ExitStack() as st:
        sb = lambda nm, shp, dt: st.enter_context(nc.sbuf_tensor(f"p3b_{nm}", shp, dt))
        nt = NormT(nc, st, "p3bn")
        nt.init(P)
        scT, shT, pk = load_mod_T(P, nc, st, "p3bm", modv, 0, 0, 1, norm2_w[0, :])
        sw = SwigluStream(nc, st, "p3bs", FFN // 128)
        g2b = sb("g2b", [128, D], F32)
        P.dma("sp", g2b[:], modv[0, 0, 5 * D:6 * D].partition_broadcast(128), key="g", writes=["g2b"])
        xg = [sb(f"xg{i}", [128, 4, D], F32) for i in range(2)]
        hT = [sb(f"hT{i}", [128, 8, 512], BF16) for i in range(2)]
        tmp = [sb(f"tmp{i}", [128, 512], F32) for i in range(2)]
        if gsem is not None:
            P.op("sp", lambda s: s.wait_ge(gsem[0], gsem[1]))
        cnt = {"t": 0}
        for g in range(n_groups):
            gs = g % 2
            for sub in range(4):
                r0 = g * 512 + sub * 128
                P.dma("sp", xg[gs][:, sub, :], X1A[r0:r0 + 128, :], key=("xg", gs), writes=[("xg", gs, sub)])
                nt.emit(P, xg[gs][:, sub, :], ("xg", gs, sub), scT, shT, pk, hT[gs][:, :, sub * 128:(sub + 1) * 128], ("hT", gs))

            def evac(P, sub, half, pap, pkey, g=g, gs=gs):
                t = cnt["t"] % 2
                cnt["t"] += 1
                P.op("dve", lambda v: v.tensor_tensor(out=tmp[t][:], in0=pap, in1=g2b[:, half * 512:(half + 1) * 512], op=ALU.mult),
                     reads=[pkey, "g2b"], writes=[("tmp", t)])
                P.op("pool", lambda g_: g_.tensor_tensor(out=xg[gs][:, sub, half * 512:(half + 1) * 512], in0=tmp[t][:],
                                                         in1=xg[gs][:, sub, half * 512:(half + 1) * 512], op=ALU.add),
                     reads=[("tmp", t), ("xg", gs, sub)], writes=[("xg", gs, sub)])
                if half == 1:
                    r0 = g * 512 + sub * 128
                    P.dma("act", X1[r0:r0 + 128, :], xg[gs][:, sub, :], key=("st", gs), reads=[("xg", gs, sub)])

            sw.run(P, hT[gs], ("hT", gs), WG[0], WU[0], WD[0], FFN // 128, evac)
        P.flush()


def phase_gmlp(nc, dr, n_tiles=TOK // 128):
    X1 = dr.get("X1", [TOK, D], F32)
    X2 = dr.get("X2", [TOK, D], F32)
    modv = dr.get("modv", [2, 2, 6 * D], F32)
    norm1_w = dr.get("norm1_w", [2, D], F32)
    gm_w_in = dr.get("gm_w_in", [D, 4 * D], F32)
    gm_vn_w = dr.get("gm_vn_w", [2 * D], F32)
    gm_vn_b = dr.get("gm_vn_b", [2 * D], F32)
    gm_wsT = dr.get("gm_wsT", [8, 128, 128], F32)
    gm_bsT = dr.get("gm_bsT", [128, 8], F32)
    gm_w_out = dr.get("gm_w_out", [2 * D, D], F32)
    P = Prog(nc, "p4")
    with ExitStack() as st:
        sb = lambda nm, shp, dt: st.enter_context(nc.sbuf_tensor(f"p4_{nm}", shp, dt))
        ps = lambda nm, shp, dt: st.enter_context(nc.psum_tensor(f"p4_{nm}", shp, dt))
        nt = NormT(nc, st, "p4n", nbuf=1)
        nt.init(P)
        scT, shT, pk = load_mod_T(P, nc, st, "p4m", modv, 0, 1, 0, norm1_w[1, :])
        Win = sb("Win", [128, 8, 4 * D], BF16)
        Wout = sb("Wout", [128, 16, D], BF16)
        WsT = sb("WsT", [128, 8, 128], BF16)
        bsT = sb("bsT", [128, 8], F32)
        vnw = sb("vnw", [128, 2 * D], F32)
        vnb = sb("vnb", [128, 2 * D], F32)
        g1b = sb("g1b", [128, D], F32)
        epsb = sb("epsb", [128, 1], F32)
        xt = [sb(f"xt{i}", [128, D], F32) for i in range(3)]
        hT = [sb(f"hT{i}", [128, 8, 128], BF16) for i in range(2)]
        u_sb = [sb(f"u{i}", [128, 2 * D], BF16) for i in range(3)]
        v_sb = [sb(f"v{i}", [128, 2 * D], F32) for i in range(2)]
        vb = [sb(f"vb{i}", [128, 2 * D], BF16) for i in range(2)]
        gt = [sb(f"gt{i}", [128, 2 * D], BF16) for i in range(2)]
        gT = [sb(f"gT{i}", [128, 16, 128], BF16) for i in range(2)]
        stats = [sb(f"stats{i}", [128, 4, 6], F32) for i in range(2)]
        mv = [sb(f"mv{i}", [128, 2], F32) for i in range(2)]
        rstd = [sb(f"rstd{i}", [128, 1], F32) for i in range(2)]
        xo = [sb(f"xo{i}", [128, D], F32) for i in range(2)]
        pz = [ps(f"pz{i}", [128, 512], F32) for i in range(2)]
        psS = [ps(f"ps{i}", [128, 512], F32) for i in range(2)]
        pgT = [ps(f"pgT{i}", [128, 8, 128], BF16) for i in range(2)]
        py = ps("py", [128, 512], F32)
        wv = gm_w_in.rearrange("(kc p) n -> p kc n", p=128)
        for kc in range(8):
            for q in range(2):
                P.dma("pool", Win[:, kc, q * 2048:(q + 1) * 2048], wv[:, kc, q * 2048:(q + 1) * 2048], key="w", writes=["Win"])
        wo = gm_w_out.rearrange("(fc p) n -> p fc n", p=128)
        for fc in range(0, 16, 4):
            P.dma("pool", Wout[:, fc:fc + 4, :], wo[:, fc:fc + 4, :], key="w", writes=["Wout"])
        P.dma("pool", WsT[:], gm_wsT.rearrange("g q p -> q g p"), key="w", writes=["WsT"])
        P.dma("sp", bsT[:], gm_bsT[:, :], key="c", writes=["bsT"])
        P.dma("sp", vnw[:], gm_vn_w.partition_broadcast(128), key="c", writes=["vnw"])
        P.dma("sp", vnb[:], gm_vn_b.partition_broadcast(128), key="c", writes=["vnb"])
        P.dma("sp", g1b[:], modv[0, 1, 2 * D:3 * D].partition_broadcast(128), key="c", writes=["g1b"])
        P.op("pool", lambda g: g.memset(epsb[:], EPS), writes=["epsb"])
        cz = {"z": 0}

        def s1a(t):
            s3, s2 = t % 3, t % 2
            r0 = t * 128
            P.dma("sp", xt[s3][:], X1[r0:r0 + 128, :], key=("xt", s3), writes=[("xt", s3)])
            nt.emit(P, xt[s3][:], ("xt", s3), scT, shT, pk, hT[s2][:], ("hT", s2))

        def s1b(t):
            s3, s2 = t % 3, t % 2
            for cg in range(8):
                z = cz["z"] % 2
                cz["z"] += 1
                mm_group(P, pz[z][:], [(hT[s2][:, kc, :], Win[:, kc, cg * 512:(cg + 1) * 512]) for kc in range(8)],
                         reads=[("hT", s2), "Win"], writes=[("pz", z)])
                dst = u_sb[s3][:, cg * 512:(cg + 1) * 512] if cg < 4 else v_sb[s2][:, (cg - 4) * 512:(cg - 3) * 512]
                P.op("act", (lambda z=z, dst=dst: lambda a: a.activation(out=dst, in_=pz[z][:], func=AF.Gelu))(),
                     reads=[("pz", z)], writes=[("u", s3) if cg < 4 else ("v", s2)])

        def s2(t):
            s = t % 2
            for c in range(4):
                P.op("dve", (lambda c=c: lambda v: v.bn_stats(out=stats[s][:, c, :], in_=v_sb[s][:, c * 512:(c + 1) * 512]))(),
                     reads=[("v", s)], writes=[("stats", s)])
            P.op("dve", lambda v: v.bn_aggr(out=mv[s][:], in_=stats[s][:].rearrange("p a b -> p (a b)")), reads=[("stats", s)], writes=[("mv", s)])
            P.op("act", lambda a: a.activation(out=rstd[s][:], in_=mv[s][:, 1:2], func=AF.Sqrt, bias=epsb[:]),
                 reads=[("mv", s), "epsb"], writes=[("rstd", s)])
            P.op("dve", lambda v: v.reciprocal(out=rstd[s][:], in_=rstd[s][:]), reads=[("rstd", s)], writes=[("rstd", s)])
            P.op("dve", lambda v: v.tensor_scalar(out=v_sb[s][:], in0=v_sb[s][:], scalar1=mv[s][:, 0:1], scalar2=rstd[s][:, 0:1],
                                                  op0=ALU.subtract, op1=ALU.mult), reads=[("v", s), ("mv", s), ("rstd", s)], writes=[("v", s)])
            P.op("pool", lambda g: g.tensor_tensor(out=v_sb[s][:], in0=v_sb[s][:], in1=vnw[:], op=ALU.mult), reads=[("v", s), "vnw"], writes=[("v", s)])
            P.op("pool", lambda g: g.tensor_tensor(out=vb[s][:], in0=v_sb[s][:], in1=vnb[:], op=ALU.add), reads=[("v", s), "vnb"], writes=[("vb", s)])

        def s3a(t):
            s, s3 = t % 2, t % 3
            for gp in range(4):
                sp_ = gp % 2
                for k in range(2):
                    g_ = gp * 2 + k
                    P.op("pe", (lambda g_=g_, k=k, sp_=sp_: lambda t_: t_.matmul(psS[sp_][:, k * 256:(k + 1) * 256], lhsT=WsT[:, g_, :],
                                                                               rhs=vb[s][:, g_ * 256:(g_ + 1) * 256], start=(k == 0), stop=True))(),
                         reads=[("vb", s), "WsT"], writes=[("psS", sp_)])
                for k in range(2):
                    g_ = gp * 2 + k
                    P.op("dve", (lambda g_=g_, k=k, sp_=sp_: lambda v: v.scalar_tensor_tensor(
                        out=gt[s][:, g_ * 256:(g_ + 1) * 256], in0=psS[sp_][:, k * 256:(k + 1) * 256], scalar=bsT[:, g_:g_ + 1],
                        in1=u_sb[s3][:, g_ * 256:(g_ + 1) * 256], op0=ALU.add, op1=ALU.mult))(),
                         reads=[("psS", sp_), "bsT", ("u", s3)], writes=[("gt", s)])

        def s3b(t):
            s, s3 = t % 2, t % 3
            r0 = t * 128
            for hb in range(2):
                for k in range(8):
                    fc = hb * 8 + k
                    P.op("pe", (lambda fc=fc, k=k, hb=hb: lambda t_: t_.transpose(out=pgT[hb][:, k, :], in_=gt[s][:, fc * 128:(fc + 1) * 128],
                                                                                 identity=nt.ident[:]))(),
                         reads=[("gt", s), (nt.n, "ident")] if k in (0, 7) else (), writes=[("pgT", hb)] if k in (0, 7) else ())
                P.op("act", (lambda hb=hb: lambda a: a.copy(out=gT[s][:, hb * 8:(hb + 1) * 8, :], in_=pgT[hb][:]))(),
                     reads=[("pgT", hb)], writes=[("gT", s)])
            for half in range(2):
                mm_group(P, py[:], [(gT[s][:, fc, :], Wout[:, fc, half * 512:(half + 1) * 512]) for fc in range(16)],
                         reads=[("gT", s), "Wout"], writes=["py"])
                P.op("dve", (lambda half=half: lambda v: v.tensor_tensor(out=xo[s][:, half * 512:(half + 1) * 512], in0=py[:],
                                                                         in1=g1b[:, half * 512:(half + 1) * 512], op=ALU.mult))(),
                     reads=["py", "g1b"], writes=[("xo", s)])
            P.op("pool", lambda g: g.tensor_tensor(out=xo[s][:], in0=xo[s][:], in1=xt[s3][:], op=ALU.add),
                 reads=[("xo", s), ("xt", s3)], writes=[("xo", s)])
            P.dma("act", X2[r0:r0 + 128, :], xo[s][:], key=("st", s), reads=[("xo", s)])

        for k in range(n_tiles + 2):
            if k < n_tiles:
                s1a(k)
            if 0 <= k - 2 < n_tiles:
                s3a(k - 2)
            if k < n_tiles:
                s1b(k)
            if 0 <= k - 2 < n_tiles:
                s3b(k - 2)
            if 0 <= k - 1 < n_tiles:
                s2(k - 1)
        P.flush()


def phase_moe(nc, dr, gsem, n_groups=TOK // 512, n_exp=NEXP):
    X2 = dr.get("X2", [TOK, D], F32)
    OUT = dr.get("out", [TOK, D], F32)
    modv = dr.get("modv", [2, 2, 6 * D], F32)
    norm2_w = dr.get("norm2_w", [2, D], F32)
    final_w = dr.get("final_norm_w", [D], F32)
    w_router = dr.get("moe_w_router", [D, NEXP], F32)
    WG, WU, WD = wcast_decl(dr, "moe", NEXP, EXP_D)
    P = Prog(nc, "p5")
    with ExitStack() as st:
        sb = lambda nm, shp, dt: st.enter_context(nc.sbuf_tensor(f"p5_{nm}", shp, dt))
        ps = lambda nm, shp, dt: st.enter_context(nc.psum_tensor(f"p5_{nm}", shp, dt))
        nt = NormT(nc, st, "p5n", nbuf=1)
        nt.init(P)
        scT, shT, pk = load_mod_T(P, nc, st, "p5m", modv, 0, 1, 1, norm2_w[1, :])
        sw = SwigluStream(nc, st, "p5s", EXP_D // 128)
        g2b = sb("g2b", [128, D], F32)
        fnb = sb("fnb", [128, D], F32)
        P.dma("sp", g2b[:], modv[0, 1, 5 * D:6 * D].partition_broadcast(128), key="g", writes=["g2b"])
        P.dma("sp", fnb[:], final_w.partition_broadcast(128), key="g", writes=["fnb"])
        xg = sb("xg", [128, 4, D], F32)
        yacc = sb("yacc", [128, 4, D], F32)
        hT = sb("hT", [128, 8, 512], BF16)
        identf = sb("identf", [128, 128], F32)
        wr = sb("wr", [128, 8, NEXP], F32)
        wrs = sb("wrs", [128, 8, NEXP], F32)
        shTb = sb("shTb", [128, 8, 128], F32)
        rbias = sb("rbias", [128, NEXP], F32)
        xT32 = sb("xT32", [128, 8, 128], F32)
        lg = sb("lg", [128, 4, NEXP], F32)
        m8 = sb("m8", [128, 4, 8], F32)
        nm1 = sb("nm1", [128, 4], F32)
        msk = sb("msk", [128, 4, NEXP], F32)
        ex = sb("ex", [128, 4, NEXP], F32)
        den = sb("den", [128, 4], F32)
        gates = sb("gates", [128, 4, NEXP], F32)
        ss = sb("ss", [128, 1], F32)
        rstd = sb("rstd", [128, 1], F32)
        junk = sb("junk", [128, D], BF16)
        epsb = sb("epsb", [128, 1], F32)
        prt = ps("prt", [128, 4, 128], F32)
        P.op("pool", lambda g: g.memset(identf[:], 0.0), writes=["identf"])
        P.op("pool", lambda g: g.affine_select(out=identf[:], in_=identf[:], pattern=[[-1, 128]], compare_op=ALU.not_equal,
                                               fill=1.0, base=0, channel_multiplier=1), reads=["identf"], writes=["identf"])
        P.op("pool", lambda g: g.memset(epsb[:], EPS), writes=["epsb"])
        P.dma("sp", wr[:], w_router.rearrange("(kc p) e -> p kc e", p=128), key="g", writes=["wr"])
        P.op("dve", lambda v: v.tensor_tensor(out=wrs[:], in0=wr[:], in1=bc(scT.unsqueeze(2), [128, 8, NEXP]), op=ALU.mult),
             reads=["wr"] + pk, writes=["wrs"])
        P.op("dve", lambda v: v.tensor_copy(out=shTb[:], in_=bc(shT.unsqueeze(2), [128, 8, 128])), reads=pk, writes=["shTb"])
        mm_group(P, prt[:, 0, 0:NEXP], [(shTb[:, kc, :], wr[:, kc, :]) for kc in range(8)], reads=["shTb", "wr"], writes=["prt"])
        P.op("dve", lambda v: v.tensor_copy(out=rbias[:], in_=prt[:, 0, 0:NEXP]), reads=["prt"], writes=["rbias"])
        if gsem is not None:
            P.op("sp", lambda s: s.wait_ge(gsem[0], gsem[1]))
        for g in range(n_groups):
            for sub in range(4):
                r0 = g * 512 + sub * 128
                P.dma("sp", xg[:, sub, :], X2[r0:r0 + 128, :], key="xg", writes=[("xg", sub)])
                nt.emit(P, xg[:, sub, :], ("xg", sub), scT, shT, pk, hT[:, :, sub * 128:(sub + 1) * 128], "hT")
                rs_ap, rs_key = nt.last_rstd
                for hb in range(2):
                    for k in range(4):
                        kc = hb * 4 + k
                        P.op("pe", (lambda kc=kc, k=k, sub=sub: lambda t: t.matmul(prt[:, k, :], lhsT=xg[:, sub, kc * 128:(kc + 1) * 128],
                                                                                  rhs=identf[:], start=(k == 0), stop=True))(),
                             reads=[("xg", sub), "identf"] if k in (0, 3) else (), writes=["prt"] if k in (0, 3) else ())
                    P.op("dve", (lambda hb=hb: lambda v: v.tensor_copy(out=xT32[:, hb * 4:(hb + 1) * 4, :], in_=prt[:]))(),
                         reads=["prt"], writes=["xT32"])
                mm_group(P, prt[:, 0, 0:NEXP], [(xT32[:, kc, :], wrs[:, kc, :]) for kc in range(8)], reads=["xT32", "wrs"], writes=["prt"])
                P.op("dve", (lambda sub=sub, rs_ap=rs_ap: lambda v: v.scalar_tensor_tensor(
                    out=lg[:, sub, :], in0=prt[:, 0, 0:NEXP], scalar=rs_ap[:, 0:1], in1=rbias[:], op0=ALU.mult, op1=ALU.add))(),
                     reads=["prt", rs_key, "rbias"], writes=["lg"])
            for sub in range(4):
                P.op("dve", (lambda sub=sub: lambda v: v.max(out=m8[:, sub, :], in_=lg[:, sub, :]))(), reads=["lg"], writes=["m8"])
            P.op("dve", lambda v: v.tensor_scalar(out=nm1[:], in0=m8[:, :, 0], scalar1=-1.0, scalar2=None, op0=ALU.mult), reads=["m8"], writes=["nm1"])
            for sub in range(4):
                P.op("dve", (lambda sub=sub: lambda v: v.tensor_scalar(out=msk[:, sub, :], in0=lg[:, sub, :], scalar1=m8[:, sub, 1:2], scalar2=None,
                                                                      op0=ALU.is_ge))(), reads=["lg", "m8"], writes=["msk"])
                P.op("act", (lambda sub=sub: lambda a: a.activation(out=ex[:, sub, :], in_=lg[:, sub, :], func=AF.Exp, bias=nm1[:, sub:sub + 1]))(),
                     reads=["lg", "nm1"], writes=["ex"])
            P.op("dve", lambda v: v.tensor_tensor(out=ex[:], in0=ex[:], in1=msk[:], op=ALU.mult), reads=["ex", "msk"], writes=["ex"])
            P.op("dve", lambda v: v.reduce_sum(out=den[:], in_=ex[:], axis=AX.X), reads=["ex"], writes=["den"])
            P.op("dve", lambda v: v.reciprocal(out=den[:], in_=den[:]), reads=["den"], writes=["den"])
            P.op("dve", lambda v: v.tensor_tensor(out=gates[:], in0=ex[:], in1=bc(den[:, :].unsqueeze(2), [128, 4, NEXP]), op=ALU.mult),
                 reads=["ex", "den"], writes=["gates"])
            for e in range(n_exp):
                def evac(P, sub, half, pap, pkey, e=e):
                    dst = yacc[:, sub, half * 512:(half + 1) * 512]
                    if e == 0:
                        P.op("dve", lambda v: v.tensor_scalar(out=dst, in0=pap, scalar1=gates[:, sub, e:e + 1], scalar2=None, op0=ALU.mult),
                             reads=[pkey, "gates"], writes=[("yacc", sub, half)])
                    else:
                        P.op("dve", lambda v: v.scalar_tensor_tensor(out=dst, in0=pap, scalar=gates[:, sub, e:e + 1], in1=dst,
                                                                     op0=ALU.mult, op1=ALU.add),
                             reads=[pkey, "gates", ("yacc", sub, half)], writes=[("yacc", sub, half)])
                sw.run(P, hT, "hT", WG[e], WU[e], WD[e], EXP_D // 128, evac)
            for sub in range(4):
                r0 = g * 512 + sub * 128
                P.op("pool", (lambda sub=sub: lambda g_: g_.tensor_tensor(out=yacc[:, sub, :], in0=yacc[:, sub, :], in1=g2b[:], op=ALU.mult))(),
                     reads=[("yacc", sub, 0), ("yacc", sub, 1), "g2b"], writes=[("yacc", sub, 0), ("yacc", sub, 1)])
                P.op("pool", (lambda sub=sub: lambda g_: g_.tensor_tensor(out=xg[:, sub, :], in0=yacc[:, sub, :], in1=xg[:, sub, :], op=ALU.add))(),
                     reads=[("yacc", sub, 0), ("yacc", sub, 1), ("xg", sub)], writes=[("xg", sub)])
                P.op("act", (lambda sub=sub: lambda a: a.activation(out=junk[:], in_=xg[:, sub, :], func=AF.Square, accum_out=ss[:]))(),
                     reads=[("xg", sub)], writes=["junk", "ss"])
                P.op("act", lambda a: a.activation(out=ss[:], in_=ss[:], func=AF.Sqrt, scale=1.0 / D, bias=epsb[:]), reads=["ss", "epsb"], writes=["ss"])
                P.op("dve", lambda v: v.reciprocal(out=rstd[:], in_=ss[:]), reads=["ss"], writes=["rstd"])
                P.op("dve", (lambda sub=sub: lambda v: v.scalar_tensor_tensor(out=yacc[:, sub, :], in0=xg[:, sub, :], scalar=rstd[:, 0:1], in1=fnb[:],
                                                                             op0=ALU.mult, op1=ALU.mult))(),
                     reads=[("xg", sub), "rstd", "fnb"], writes=[("yacc", sub, 0), ("yacc", sub, 1)])
                P.dma("act", OUT[r0:r0 + 128, :], yacc[:, sub, :], key="ost", reads=[("yacc", sub, 0), ("yacc", sub, 1)])
        P.flush()


EXT_IN = ["x_b", "x_own", "ctx_b", "cvec", "w_mod", "b_mod", "norm1_w", "norm2_w", "final_norm_w", "wqkv", "wqk_perm",
          "cos_all", "sin_all", "cos_own", "sin_own", "lamv", "subln_w", "w_o", "ffn_wg", "ffn_wu", "ffn_wd",
          "gm_w_in", "gm_vn_w", "gm_vn_b", "gm_wsT", "gm_bsT", "gm_w_out", "moe_w_router", "moe_wg", "moe_wu", "moe_wd"]


def build_program():
    nc = bass.Bass("TRN2", target_bir_lowering=False)
    dr = Dram(nc, ext_in=EXT_IN, ext_out=["out"])
    fwg = dr.get("ffn_wg", [1, D, FFN], F32)
    fwu = dr.get("ffn_wu", [1, D, FFN], F32)
    fwd = dr.get("ffn_wd", [1, FFN, D], F32)
    mwg = dr.get("moe_wg", [NEXP, D, EXP_D], F32)
    mwu = dr.get("moe_wu", [NEXP, D, EXP_D], F32)
    mwd = dr.get("moe_wd", [NEXP, EXP_D, D], F32)
    with nc.semaphore("g_wcast") as gw:
        gs = {"sem": gw, "count": 0}

        def casts(P):
            phase_wcast(nc, dr, P, "ffn", fwg, fwu, fwd, 1, FFN, "wc", gs=gs)
            phase_wcast(nc, dr, P, "moe", mwg, mwu, mwd, NEXP, EXP_D, "wc", gs=gs)

        phase_mod(nc, dr)
        nc.all_engine_barrier()
        phase_qkv(nc, dr)
        nc.all_engine_barrier()
        phase_attn(nc, dr, pre=casts)
        nc.all_engine_barrier()
        gsem = (gw, gs["count"])
        phase_wo(nc, dr)
        nc.all_engine_barrier()
        phase_ffn(nc, dr, gsem)
        nc.all_engine_barrier()
        phase_gmlp(nc, dr)
        nc.all_engine_barrier()
        phase_moe(nc, dr, gsem)
    return nc


def _perm_cols():
    idx = np.arange(D)
    d = idx % 64
    partner = np.where(d % 32 < 16, d + 16, d - 16)
    return (idx // 64) * 64 + partner


def _rope_tables():
    t = np.arange(SEQ)
    row = (t // 64).astype(np.float32)
    col = (t % 64).astype(np.float32)
    inv_freq = (np.float32(10000.0) ** (-np.arange(16, dtype=np.float32) / np.float32(16))).astype(np.float32)
    d = np.arange(128) % 64
    pos = np.where((d >= 32)[:, None], col[None, :], row[None, :]).astype(np.float32)
    ang = (pos * inv_freq[d % 16][:, None]).astype(np.float32)
    sgn = np.where(d % 32 < 16, -1.0, 1.0).astype(np.float32)
    return np.cos(ang).astype(np.float32), (np.sin(ang) * sgn[:, None]).astype(np.float32)


def make_in_maps(inp):
    f32 = lambda a: np.ascontiguousarray(np.asarray(a, dtype=np.float32))
    x, c, ctx = f32(inp["x"]), f32(inp["c"]), f32(inp["ctx"])
    wqkv = f32(inp["da_w_qkv"])[0]
    pc = _perm_cols()
    wqk_perm = np.ascontiguousarray(np.concatenate([wqkv[:, 0:D][:, pc], wqkv[:, D:2 * D][:, pc]], axis=1))
    cos_all, sin_all = _rope_tables()
    lamv = np.stack([f32(inp["da_lambda_q1"])[0], f32(inp["da_lambda_k1"])[0], f32(inp["da_lambda_q2"])[0], f32(inp["da_lambda_k2"])[0]])
    shared = dict(
        w_mod=f32(inp["w_mod"]), b_mod=f32(inp["b_mod"]), norm1_w=f32(inp["norm1_w"]), norm2_w=f32(inp["norm2_w"]),
        final_norm_w=f32(inp["final_norm_w"]), wqkv=wqkv, wqk_perm=wqk_perm, cos_all=cos_all, sin_all=sin_all,
        lamv=f32(lamv), subln_w=f32(inp["da_subln_w"])[0], w_o=f32(inp["da_w_o"])[0],
        ffn_wg=f32(inp["ffn_w_gate"]), ffn_wu=f32(inp["ffn_w_up"]), ffn_wd=f32(inp["ffn_w_down"]),
        gm_w_in=f32(inp["gm_w_in"])[0], gm_vn_w=f32(inp["gm_vnorm_w"])[0], gm_vn_b=f32(inp["gm_vnorm_b"])[0],
        gm_wsT=np.ascontiguousarray(f32(inp["gm_w_s"])[0].transpose(0, 2, 1)), gm_bsT=np.ascontiguousarray(f32(inp["gm_b_s"])[0].T),
        gm_w_out=f32(inp["gm_w_out"])[0], moe_w_router=f32(inp["moe_w_router"])[0],
        moe_wg=f32(inp["moe_w_gate"])[0], moe_wu=f32(inp["moe_w_up"])[0], moe_wd=f32(inp["moe_w_down"])[0],
    )
    maps = []
    for core in range(8):
        b, r = core // 4, core % 4
        t0 = r * TOK
        m = dict(shared)
        m["x_b"] = x[b]
        m["x_own"] = np.ascontiguousarray(x[b, t0:t0 + TOK])
        m["ctx_b"] = ctx[b]
        m["cvec"] = np.ascontiguousarray(np.stack([c[b], f32(inp["c_ctx"])]))
        m["cos_own"] = np.ascontiguousarray(cos_all[:, t0:t0 + TOK] * np.float32(0.125))
        m["sin_own"] = np.ascontiguousarray(sin_all[:, t0:t0 + TOK] * np.float32(0.125))
        maps.append(m)
    return maps


def kernel(**inputs):
    maps = make_in_maps(inputs)
    nc = build_program()
    res = run_bass_kernel_spmd(nc, maps, core_ids=list(range(8)))
    out = np.empty((2, SEQ, D), np.float32)
    for core in range(8):
        b, r = core // 4, core % 4
        out[b, r * TOK:(r + 1) * TOK] = res.results[core]["out"]
    return out
```

```python
import math
from contextlib import ExitStack

import numpy as np
import concourse.bass as bass
import concourse.mybir as mybir
from concourse.bass_utils import run_bass_kernel_spmd

F32 = mybir.dt.float32
BF16 = mybir.dt.bfloat16
AF = mybir.ActivationFunctionType
ALU = mybir.AluOpType
AX = mybir.AxisListType

D = 1024
SEQ = 16384
CTX = 256
NKEY = SEQ + CTX
NKT = NKEY // 128
TOK = 4096
H = 8
FFN = 2816
EXP_D = 3584
NEXP = 8
EPS = 1e-6
VW = 130
LAM_INIT0 = 0.8 - 0.6 * math.exp(-0.3 * 0)


class Ev:
    __slots__ = ("eng", "needed", "value", "key")

    def __init__(self, eng, key=None):
        self.eng = eng
        self.needed = False
        self.value = None
        self.key = key


class Prog:
    ENGS = ("pe", "act", "dve", "pool", "sp")

    def __init__(self, nc, name):
        self.nc = nc
        self.name = name
        self.ops = {e: [] for e in self.ENGS}
        self.lastw = {}
        self.readers = {}
        self.dma_cnt = {}

    def _deps(self, reads, writes, dma_key=None):
        deps = []
        for k in reads:
            e = self.lastw.get(k)
            if e is not None:
                deps.append(e)
        for k in writes:
            e = self.lastw.get(k)
            if e is not None:
                deps.append(e)
            deps.extend(self.readers.get(k, {}).values())
        out = []
        for d in deps:
            if d.eng == "dma":
                if dma_key is not None and d.key == dma_key:
                    continue
                if d.value != self.dma_cnt[d.key]:
                    d2 = Ev("dma", d.key)
                    d2.value = self.dma_cnt[d.key]
                    d = d2
            d.needed = True
            out.append(d)
        return out

    def _commit(self, ev, reads, writes):
        rk = ev.key if ev.eng == "dma" else ev.eng
        for k in reads:
            self.readers.setdefault(k, {})[rk] = ev
        for k in writes:
            self.lastw[k] = ev
            self.readers[k] = {}

    def op(self, eng, fn, reads=(), writes=(), extra=()):
        deps = self._deps(reads, writes)
        for d in extra:
            if d is not None:
                d.needed = True
                deps.append(d)
        ev = Ev(eng)
        self.ops[eng].append((fn, deps, ev))
        self._commit(ev, reads, writes)
        return ev

    def dma(self, q, out, in_, key, reads=(), writes=(), **kw):
        deps = self._deps(reads, writes, dma_key=key)
        ev = Ev("dma", key)
        self.dma_cnt[key] = self.dma_cnt.get(key, 0) + 16
        ev.value = self.dma_cnt[key]
        ev.needed = True
        self.ops[q].append((lambda e: e.dma_start(out=out, in_=in_, **kw), deps, ev))
        self._commit(ev, reads, writes)
        return ev

    def dma_g(self, q, out, in_, gs, **kw):
        ev = Ev("gdma", gs["sem"])
        gs["count"] += 16
        self.ops[q].append((lambda e: e.dma_start(out=out, in_=in_, **kw), [], ev))
        return ev

    def flush(self):
        nc = self.nc
        with ExitStack() as st:
            sems = {e: st.enter_context(nc.semaphore(f"{self.name}_s_{e}")) for e in ("pe", "act", "dve", "pool")}
            dsems = {}
            for i, k in enumerate(self.dma_cnt):
                dsems[k] = st.enter_context(nc.semaphore(f"{self.name}_d{i}"))
            for e in self.ENGS:
                c = 0
                for (_, _, ev) in self.ops[e]:
                    if ev.eng not in ("dma", "gdma") and ev.needed:
                        c += 1
                        ev.value = c
            block = st.enter_context(nc.Block())

            def run(e, eo):
                waited = {}
                for (fn, deps, ev) in self.ops[e]:
                    for d in deps:
                        if d.eng == "dma":
                            sk = ("d", d.key)
                            s = dsems[d.key]
                        else:
                            if d.eng == "pe" and e == "pe":
                                continue
                            sk = ("e", d.eng)
                            s = sems[d.eng]
                        if waited.get(sk, 0) >= d.value:
                            continue
                        eo.wait_ge(s, d.value)
                        waited[sk] = d.value
                    ins = fn(eo)
                    if ev.eng == "dma":
                        ins.then_inc(dsems[ev.key], 16)
                    elif ev.eng == "gdma":
                        ins.then_inc(ev.key, 16)
                    elif ev.needed:
                        ins.then_inc(sems[e], 1)
                if e == "sp":
                    for k, v in self.dma_cnt.items():
                        if waited.get(("d", k), 0) < v:
                            eo.wait_ge(dsems[k], v)

            @block.tensor
            def _(t):
                run("pe", t)

            @block.scalar
            def _(a):
                run("act", a)

            @block.vector
            def _(v):
                run("dve", v)

            @block.gpsimd
            def _(g):
                run("pool", g)

            @block.sync
            def _(s):
                run("sp", s)


def mm_group(P, out, pairs, reads, writes):
    n = len(pairs)
    ev = None
    for i, (l, r) in enumerate(pairs):
        fn = (lambda l=l, r=r, i=i: (lambda t: t.matmul(out, lhsT=l, rhs=r, start=(i == 0), stop=(i == n - 1))))()
        edge = (i == 0 or i == n - 1)
        ev = P.op("pe", fn, reads=reads if edge else (), writes=writes if edge else ())
    return ev


class Dram:
    def __init__(self, nc, ext_in=(), ext_out=()):
        self.nc = nc
        self.ext_in = set(ext_in)
        self.ext_out = set(ext_out)
        self.t = {}

    def get(self, name, shape, dtype, kind=None):
        if name in self.t:
            return self.t[name]
        if kind is None:
            kind = "ExternalInput" if name in self.ext_in else ("ExternalOutput" if name in self.ext_out else "Internal")
        ap = self.nc.dram_tensor(name, list(shape), dtype, kind=kind).ap()
        self.t[name] = ap
        return ap


def bc(ap, shape):
    return ap.to_broadcast(list(shape))


def phase_mod(nc, dr, tail=None):
    cvec = dr.get("cvec", [2, D], F32)
    w_mod = dr.get("w_mod", [2, D, 6 * D], F32)
    b_mod = dr.get("b_mod", [2, 6 * D], F32)
    modv = dr.get("modv", [2, 2, 6 * D], F32)
    P = Prog(nc, "p0")
    with ExitStack() as st:
        cT = st.enter_context(nc.sbuf_tensor("p0_cT", [128, 8, 2], F32))
        bm = st.enter_context(nc.sbuf_tensor("p0_bm", [2, 2, 6 * D], F32))
        mrow = st.enter_context(nc.sbuf_tensor("p0_mrow", [2, 2, 6 * D], F32))
        wblk = [st.enter_context(nc.sbuf_tensor(f"p0_w{i}", [128, 8, 512], F32)) for i in range(2)]
        pm = [st.enter_context(nc.psum_tensor(f"p0_pm{i}", [2, 512], F32)) for i in range(2)]
        for r in range(2):
            P.dma("sp", cT[:, :, r], cvec[r, :].rearrange("(kc p) -> p kc", p=128), key="c", writes=["cT"],
                  allow_slow_non_contiguous=True)
        for l in range(2):
            P.dma("sp", bm[:, l, :], b_mod[l, :].partition_broadcast(2),
                  key="bm", writes=[("bm", l)])
        P.op("act", lambda a: a.activation(out=cT[:], in_=cT[:], func=AF.Silu), reads=["cT"], writes=["cT"])
        i = 0
        for l in range(2):
            wv = w_mod[l].rearrange("(kc p) n -> p kc n", p=128)
            for blk in range(12):
                s = i % 2
                P.dma("sp", wblk[s][:], wv[:, :, blk * 512:(blk + 1) * 512], key=("w", s), writes=[("w", s)])
                mm_group(P, pm[s][:], [(cT[:, kc, :], wblk[s][:, kc, :]) for kc in range(8)],
                         reads=["cT", ("w", s)], writes=[("pm", s)])
                P.op("dve", (lambda s=s, l=l, blk=blk: lambda v: v.tensor_tensor(
                    out=mrow[:, l, blk * 512:(blk + 1) * 512], in0=pm[s][:], in1=bm[:, l, blk * 512:(blk + 1) * 512],
                    op=ALU.add))(), reads=[("pm", s), ("bm", l)], writes=["mrow"])
                i += 1
        P.dma("sp", modv[:, :, :], mrow[:], key="out", reads=["mrow"])
        if tail is not None:
            tail(P)
        P.flush()


class NormT:
    def __init__(self, nc, st, name, nbuf=2):
        self.nc = nc
        self.n = name
        self.nbuf = nbuf
        sb = lambda nm, shp, dt: st.enter_context(nc.sbuf_tensor(f"{name}_{nm}", shp, dt))
        self.xs = [sb(f"xs{i}", [128, D], BF16) for i in range(nbuf)]
        self.junk = [sb(f"junk{i}", [128, D], BF16) for i in range(nbuf)]
        self.ss = [sb(f"ss{i}", [128, 1], F32) for i in range(nbuf)]
        self.rstd = [sb(f"rstd{i}", [128, 1], F32) for i in range(nbuf)]
        self.tmp = [sb(f"tmp{i}", [128, 8, 128], F32) for i in range(nbuf)]
        self.ident = sb("ident", [128, 128], BF16)
        self.eps = sb("eps", [128, 1], F32)
        self.pT = [st.enter_context(nc.psum_tensor(f"{name}_pT{i}", [128, 8, 128], BF16)) for i in range(nbuf)]
        self.i = 0

    def init(self, P):
        n = self.n
        P.op("pool", lambda g: g.memset(self.ident[:], 0.0), writes=[(n, "ident")])
        P.op("pool", lambda g: g.affine_select(out=self.ident[:], in_=self.ident[:], pattern=[[-1, 128]],
                                               compare_op=ALU.not_equal, fill=1.0, base=0, channel_multiplier=1),
             reads=[(n, "ident")], writes=[(n, "ident")])
        P.op("pool", lambda g: g.memset(self.eps[:], EPS), writes=[(n, "eps")])

    def emit(self, P, xt, xt_key, scaleT, shiftT, par_keys, out, out_key, mean_div=float(D)):
        n = self.n
        s = self.i % self.nbuf
        self.i += 1
        self.last_rstd = (self.rstd[s], (n, "rstd", s))
        xs, junk, ss, rstd, tmp, pT = self.xs[s], self.junk[s], self.ss[s], self.rstd[s], self.tmp[s], self.pT[s]
        P.op("act", lambda a: a.activation(out=junk[:], in_=xt, func=AF.Square, accum_out=ss[:]),
             reads=[xt_key], writes=[(n, "junk", s), (n, "ss", s)])
        P.op("act", lambda a: a.activation(out=ss[:], in_=ss[:], func=AF.Sqrt, scale=1.0 / mean_div, bias=self.eps[:]),
             reads=[(n, "ss", s), (n, "eps")], writes=[(n, "ss", s)])
        P.op("dve", lambda v: v.reciprocal(out=rstd[:], in_=ss[:]), reads=[(n, "ss", s)], writes=[(n, "rstd", s)])
        P.op("dve", lambda v: v.tensor_scalar(out=xs[:], in0=xt, scalar1=rstd[:, 0:1], scalar2=None, op0=ALU.mult),
             reads=[xt_key, (n, "rstd", s)], writes=[(n, "xs", s)])
        for kc in range(8):
            edge = kc in (0, 7)
            P.op("pe", (lambda kc=kc: lambda t: t.transpose(out=pT[:, kc, :], in_=xs[:, kc * 128:(kc + 1) * 128],
                                                            identity=self.ident[:]))(),
                 reads=[(n, "xs", s), (n, "ident")] if edge else (), writes=[(n, "pT", s)] if edge else ())
        P.op("dve", lambda v: v.tensor_tensor(out=tmp[:], in0=pT[:], in1=bc(scaleT.unsqueeze(2), [128, 8, 128]), op=ALU.mult),
             reads=[(n, "pT", s)] + list(par_keys), writes=[(n, "tmp", s)])
        return P.op("pool", lambda g: g.tensor_tensor(out=out, in0=tmp[:], in1=bc(shiftT.unsqueeze(2), [128, 8, 128]), op=ALU.add),
                    reads=[(n, "tmp", s)] + list(par_keys), writes=[out_key])


def load_mod_T(P, nc, st, name, modv, r, l, which, norm_w_row):
    sb = lambda nm, shp: st.enter_context(nc.sbuf_tensor(f"{name}_{nm}", shp, F32))
    shT, scT, nwT, scaleT = sb("shT", [128, 8]), sb("scT", [128, 8]), sb("nwT", [128, 8]), sb("scaleT", [128, 8])
    o = 3 * D * which
    k = f"{name}_ld"
    P.dma("sp", shT[:], modv[r, l, o:o + D].rearrange("(kc p) -> p kc", p=128), key=k, writes=[(name, "shT")],
          allow_slow_non_contiguous=True)
    P.dma("sp", scT[:], modv[r, l, o + D:o + 2 * D].rearrange("(kc p) -> p kc", p=128), key=k, writes=[(name, "scT")],
          allow_slow_non_contiguous=True)
    P.dma("sp", nwT[:], norm_w_row.rearrange("(kc p) -> p kc", p=128), key=k, writes=[(name, "nwT")],
          allow_slow_non_contiguous=True)
    P.op("dve", lambda v: v.scalar_tensor_tensor(out=scaleT[:], in0=scT[:], scalar=1.0, in1=nwT[:], op0=ALU.add, op1=ALU.mult),
         reads=[(name, "scT"), (name, "nwT")], writes=[(name, "scaleT")])
    return scaleT[:, :], shT[:, :], [(name, "scaleT"), (name, "shT")]


def phase_qkv(nc, dr, n_groups=SEQ // 512, n_own=TOK // 512):
    x_b = dr.get("x_b", [SEQ, D], F32)
    x_own = dr.get("x_own", [TOK, D], F32)
    ctx_b = dr.get("ctx_b", [CTX, D], F32)
    modv = dr.get("modv", [2, 2, 6 * D], F32)
    norm1_w = dr.get("norm1_w", [2, D], F32)
    wqkv = dr.get("wqkv", [D, 3 * D], F32)
    wqk_perm = dr.get("wqk_perm", [D, 2 * D], F32)
    cos_all = dr.get("cos_all", [128, SEQ], F32)
    sin_all = dr.get("sin_all", [128, SEQ], F32)
    cos_own = dr.get("cos_own", [128, TOK], F32)
    sin_own = dr.get("sin_own", [128, TOK], F32)
    KT_all = dr.get("KT_all", [H, 128, NKEY], BF16)
    V_all = dr.get("V_all", [128, NKT, H * VW], BF16)
    QT_own = dr.get("QT_own", [H, 128, TOK], BF16)

    P = Prog(nc, "p1")
    with ExitStack() as st:
        sb = lambda nm, shp, dt: st.enter_context(nc.sbuf_tensor(f"p1_{nm}", shp, dt))
        ps = lambda nm, shp, dt: st.enter_context(nc.psum_tensor(f"p1_{nm}", shp, dt))
        nt = NormT(nc, st, "p1n")
        nt.init(P)
        sc_b, sh_b, pk_b = load_mod_T(P, nc, st, "p1mb", modv, 0, 0, 0, norm1_w[0, :])
        sc_c, sh_c, pk_c = load_mod_T(P, nc, st, "p1mc", modv, 1, 0, 0, norm1_w[0, :])
        wv3 = wqkv.rearrange("(kc p) n -> p kc n", p=128)
        wp3 = wqk_perm.rearrange("(kc p) n -> p kc n", p=128)
        Wq, Wk, Wv = sb("Wq", [128, 8, D], BF16), sb("Wk", [128, 8, D], BF16), sb("Wv", [128, 8, D], BF16)
        Wqp, Wkp = sb("Wqp", [128, 8, D], BF16), sb("Wkp", [128, 8, D], BF16)
        for (w, src, nm) in ((Wk, wv3[:, :, D:2 * D], "Wk"), (Wkp, wp3[:, :, D:2 * D], "Wkp"), (Wv, wv3[:, :, 2 * D:3 * D], "Wv"),
                             (Wq, wv3[:, :, 0:D], "Wq"), (Wqp, wp3[:, :, 0:D], "Wqp")):
            for kc in range(8):
                P.dma("pool", w[:, kc, :], src[:, kc, :], key="wld", writes=[nm])
        xt = [sb(f"xt{i}", [128, D], F32) for i in range(2)]
        hT = [sb(f"hT{i}", [128, 8, 512], BF16) for i in range(2)]
        cs = [sb(f"cos{i}", [128, 512], F32) for i in range(2)]
        sn = [sb(f"sin{i}", [128, 512], F32) for i in range(2)]
        t1 = [sb(f"t1_{i}", [128, 512], F32) for i in range(2)]
        t2 = [sb(f"t2_{i}", [128, 512], F32) for i in range(2)]
        ko = [sb(f"ko{i}", [128, 512], BF16) for i in range(2)]
        vsb = [sb(f"vsb{i}", [128, H, VW], BF16) for i in range(2)]
        pk1 = [ps(f"pk1_{i}", [128, 512], F32) for i in range(2)]
        pk2 = [ps(f"pk2_{i}", [128, 512], F32) for i in range(2)]
        pv = [ps(f"pv{i}", [128, 512], F32) for i in range(2)]
        for i in range(2):
            P.op("pool", (lambda i=i: lambda g: g.memset(vsb[i][:], 0.0))(), writes=[("vsb", i)])
            P.op("pool", (lambda i=i: lambda g: g.memset(vsb[i][:, :, 128:129], 1.0))(), reads=[("vsb", i)], writes=[("vsb", i)])

        cnt = {"xt": 0, "k": 0, "v": 0}
        groups = []

        def add_group(src, nsub, scaleT, shiftT, pkeys, mode, tab=None, tok0=0, key_col0=0, kt0=0):
            groups.append(dict(src=src, nsub=nsub, scaleT=scaleT, shiftT=shiftT, pkeys=pkeys, mode=mode, tab=tab, tok0=tok0,
                               key_col0=key_col0, kt0=kt0, gs=len(groups) % 2))

        def a_sub(G, sub):
            gs = G["gs"]
            if sub >= G["nsub"]:
                return
            if sub == 0 and G["mode"] != "ctx":
                P.dma("sp", cs[gs][:], G["tab"][0][:, G["tok0"]:G["tok0"] + 512], key=("tab", gs), writes=[("cs", gs)])
                P.dma("sp", sn[gs][:], G["tab"][1][:, G["tok0"]:G["tok0"] + 512], key=("tab", gs), writes=[("sn", gs)])
            xs_ = cnt["xt"] % 2
            cnt["xt"] += 1
            P.dma("sp", xt[xs_][:], G["src"][sub * 128:(sub + 1) * 128, :], key=("xt", xs_), writes=[("xt", xs_)])
            nt.emit(P, xt[xs_][:], ("xt", xs_), G["scaleT"], G["shiftT"], G["pkeys"], hT[gs][:, :, sub * 128:(sub + 1) * 128], ("hT", gs))

        def part_v(G):
            gs, mode = G["gs"], G["mode"]
            if mode not in ("kv", "ctx"):
                return
            for sub in range(G["nsub"]):
                vs = cnt["v"] % 2
                cnt["v"] += 1
                for half in range(2):
                    pvs = half
                    mm_group(P, pv[pvs][:], [(hT[gs][:, kc, sub * 128:(sub + 1) * 128], Wv[:, kc, half * 512:(half + 1) * 512])
                                              for kc in range(8)], reads=[("hT", gs), "Wv"], writes=[("pv", pvs)])
                    P.op("act", (lambda vs=vs, pvs=pvs, half=half: lambda a: a.copy(
                        out=vsb[vs][:, 4 * half:4 * half + 4, 0:128], in_=pv[pvs][:].rearrange("p (h d) -> p h d", h=4)))(),
                         reads=[("pv", pvs)], writes=[("vsb", vs)])
                P.dma("act", V_all[:, G["kt0"] + sub, :], vsb[vs][:].rearrange("p h d -> p (h d)"), key=("vst", vs), reads=[("vsb", vs)])

        def part_k(G, nxt):
            gs, mode = G["gs"], G["mode"]
            n = G["nsub"] * 128
            tok0 = G["tok0"]
            W, Wp = (Wq, Wqp) if mode == "q" else (Wk, Wkp)
            wn, wpn = ("Wq", "Wqp") if mode == "q" else ("Wk", "Wkp")
            for h in range(H):
                ks = cnt["k"] % 2
                cnt["k"] += 1
                mm_group(P, pk1[ks][:, 0:n], [(W[:, kc, h * 128:(h + 1) * 128], hT[gs][:, kc, 0:n]) for kc in range(8)],
                         reads=[("hT", gs), wn], writes=[("pk1", ks)])
                if mode == "ctx":
                    P.op("act", (lambda ks=ks: lambda a: a.copy(out=ko[ks][:, 0:n], in_=pk1[ks][:, 0:n]))(),
                         reads=[("pk1", ks)], writes=[("ko", ks)])
                else:
                    mm_group(P, pk2[ks][:, 0:n], [(Wp[:, kc, h * 128:(h + 1) * 128], hT[gs][:, kc, 0:n]) for kc in range(8)],
                             reads=[("hT", gs), wpn], writes=[("pk2", ks)])
                    P.op("dve", (lambda ks=ks, gs=gs: lambda v: v.tensor_tensor(out=t1[ks][:], in0=pk1[ks][:], in1=cs[gs][:], op=ALU.mult))(),
                         reads=[("pk1", ks), ("cs", gs)], writes=[("t1", ks)])
                    P.op("dve", (lambda ks=ks, gs=gs: lambda v: v.tensor_tensor(out=t2[ks][:], in0=pk2[ks][:], in1=sn[gs][:], op=ALU.mult))(),
                         reads=[("pk2", ks), ("sn", gs)], writes=[("t2", ks)])
                    P.op("pool", (lambda ks=ks: lambda g: g.tensor_tensor(out=ko[ks][:], in0=t1[ks][:], in1=t2[ks][:], op=ALU.add))(),
                         reads=[("t1", ks), ("t2", ks)], writes=[("ko", ks)])
                dst = QT_own[h, :, tok0:tok0 + n] if mode == "q" else KT_all[h, :, G["key_col0"]:G["key_col0"] + n]
                P.dma("pool", dst, ko[ks][:, 0:n], key=("kst", ks), reads=[("ko", ks)])
                if nxt is not None and h % 2 == 0:
                    a_sub(nxt, h // 2)

        add_group(ctx_b, 2, sc_c, sh_c, pk_c, "ctx", key_col0=0, kt0=0)
        for g in range(n_groups):
            add_group(x_b[g * 512:(g + 1) * 512, :], 4, sc_b, sh_b, pk_b, "kv", tab=(cos_all, sin_all), tok0=g * 512,
                      key_col0=CTX + g * 512, kt0=2 + g * 4)
        for g in range(n_own):
            add_group(x_own[g * 512:(g + 1) * 512, :], 4, sc_b, sh_b, pk_b, "q", tab=(cos_own, sin_own), tok0=g * 512)
        for sub in range(4):
            a_sub(groups[0], sub)
        for gi, G in enumerate(groups):
            nxt = groups[gi + 1] if gi + 1 < len(groups) else None
            part_v(G)
            part_k(G, nxt)
        P.flush()


def phase_attn(nc, dr, n_heads=H, n_qg=TOK // 512, n_kt=NKT, pre=None):
    KT_all = dr.get("KT_all", [H, 128, NKEY], BF16)
    V_all = dr.get("V_all", [128, NKT, H * VW], BF16)
    QT_own = dr.get("QT_own", [H, 128, TOK], BF16)
    lamv = dr.get("lamv", [4, 64], F32)
    subln_w = dr.get("subln_w", [128], F32)
    OT = dr.get("OT", [H, 128, TOK], BF16)

    P = Prog(nc, "p2")
    with ExitStack() as st:
        sb = lambda nm, shp, dt: st.enter_context(nc.sbuf_tensor(f"p2_{nm}", shp, dt))
        ps = lambda nm, shp, dt: st.enter_context(nc.psum_tensor(f"p2_{nm}", shp, dt))
        KT = [sb(f"KT{i}", [128, NKEY], BF16) for i in range(2)]
        V = [sb(f"V{i}", [128, NKT, VW], BF16) for i in range(2)]
        QT = [sb(f"QT{i}", [128, TOK], BF16) for i in range(2)]
        e = [sb(f"e{i}", [128, 2, 512], BF16) for i in range(3)]
        osb = sb("osb", [128, 8, VW], F32)
        rs = sb("rs", [128, 8], F32)
        nlr = sb("nlr", [128, 4], F32)
        o0 = sb("o0", [128, 128], F32)
        o1 = sb("o1", [128, 4, 128], F32)
        sq = sb("sq", [128, 128], F32)
        ms = sb("ms", [128, 4], F32)
        on = sb("on", [128, 4, 128], BF16)
        oTs = [sb(f"oTs{i}", [128, 512], BF16) for i in range(2)]
        ident = sb("ident", [128, 128], BF16)
        lamt = sb("lamt", [128, 4, 64], F32)
        lp = sb("lp", [128, 2, 64], F32)
        ls = sb("ls", [128, 2], F32)
        nlam = sb("nlam", [128, 1], F32)
        swb = sb("swb", [128, 128], F32)
        epsb = sb("epsb", [128, 1], F32)
        S = [ps(f"S{i}", [128, 2, 512], F32) for i in range(2)]
        po = ps("po", [128, 3, 512], F32)
        pTo = ps("pTo", [128, 4, 128], BF16)

        def acc(c, qs):
            i = c * 4 + qs
            return po[:, i // 3, (i % 3) * VW:(i % 3 + 1) * VW]

        P.op("pool", lambda g: g.memset(ident[:], 0.0), writes=["ident"])
        P.op("pool", lambda g: g.affine_select(out=ident[:], in_=ident[:], pattern=[[-1, 128]], compare_op=ALU.not_equal,
                                               fill=1.0, base=0, channel_multiplier=1), reads=["ident"], writes=["ident"])
        P.op("pool", lambda g: g.memset(epsb[:], EPS), writes=["epsb"])
        for i in range(4):
            P.dma("sp", lamt[:, i, :], lamv[i, :].partition_broadcast(128), key="c0", writes=["lamt"])
        P.dma("sp", swb[:], subln_w.partition_broadcast(128), key="c1", writes=["swb"])
        P.op("dve", lambda v: v.tensor_scalar(out=swb[:], in0=swb[:], scalar1=1.0 - LAM_INIT0, scalar2=None, op0=ALU.mult),
             reads=["swb"], writes=["swb"])
        P.op("dve", lambda v: v.tensor_tensor(out=lp[:, 0, :], in0=lamt[:, 0, :], in1=lamt[:, 1, :], op=ALU.mult), reads=["lamt"], writes=["lp"])
        P.op("dve", lambda v: v.tensor_tensor(out=lp[:, 1, :], in0=lamt[:, 2, :], in1=lamt[:, 3, :], op=ALU.mult), reads=["lamt", "lp"], writes=["lp"])
        P.op("dve", lambda v: v.reduce_sum(out=ls[:], in_=lp[:], axis=AX.X), reads=["lp"], writes=["ls"])
        P.op("act", lambda a: a.activation(out=ls[:], in_=ls[:], func=AF.Exp), reads=["ls"], writes=["ls"])
        P.op("dve", lambda v: v.scalar_tensor_tensor(out=nlam[:], in0=ls[:, 1:2], scalar=-LAM_INIT0, in1=ls[:, 0:1],
                                                     op0=ALU.add, op1=ALU.subtract), reads=["ls"], writes=["nlam"])

        def load_head(h):
            hs = h % 2
            nch = 5
            kw = NKEY // nch
            tw = NKT // nch
            for c in range(nch):
                P.dma("sp", KT[hs][:, c * kw:(c + 1) * kw], KT_all[h, :, c * kw:(c + 1) * kw], key=("KT", hs, c), writes=[("KT", hs, c)])
                P.dma("sp", V[hs][:, c * tw:(c + 1) * tw, :], V_all[:, c * tw:(c + 1) * tw, h * VW:(h + 1) * VW], key=("V", hs, c),
                      writes=[("V", hs, c)])
            P.dma("sp", QT[hs][:], QT_own[h, :, :], key=("QT", hs), writes=[("QT", hs)])

        steps = [(h, g, j) for h in range(n_heads) for g in range(n_qg) for j in range(n_kt)]

        def qk(i):
            h, g, j = steps[i]
            hs, ss = h % 2, i % 2
            ev = None
            for c in range(2):
                ev = P.op("pe", (lambda c=c: lambda t: t.matmul(S[ss][:, c, :], lhsT=KT[hs][64 * c:64 * c + 64, j * 128:(j + 1) * 128],
                                                                rhs=QT[hs][64 * c:64 * c + 64, g * 512:(g + 1) * 512],
                                                                start=True, stop=True))(),
                          reads=[("KT", hs, min(4, j // 26)), ("QT", hs)], writes=[("S", ss)])
            return ev

        def finalize_a(h, g):
            for b in range(3):
                nb = 3 if b < 2 else 2
                P.op("dve", (lambda b=b, nb=nb: lambda v: v.tensor_copy(
                    out=osb[:, 3 * b:3 * b + nb, :], in_=po[:, b, 0:nb * VW].rearrange("p (a w) -> p a w", w=VW)))(),
                     reads=["po"], writes=["osb"])
            P.op("dve", lambda v: v.reciprocal(out=rs[:], in_=osb[:, :, 128]), reads=["osb"], writes=["rs"])
            P.op("dve", lambda v: v.tensor_scalar(out=nlr[:], in0=rs[:, 4:8], scalar1=nlam[:, 0:1], scalar2=None, op0=ALU.mult),
                 reads=["rs", "nlam"], writes=["nlr"])
            for qs in range(4):
                P.op("dve", (lambda qs=qs: lambda v: v.tensor_scalar(out=o0[:], in0=osb[:, qs, 0:128], scalar1=rs[:, qs:qs + 1],
                                                                     scalar2=None, op0=ALU.mult))(), reads=["osb", "rs"], writes=["o0"])
                P.op("dve", (lambda qs=qs: lambda v: v.scalar_tensor_tensor(out=o1[:, qs, :], in0=osb[:, 4 + qs, 0:128], scalar=nlr[:, qs:qs + 1],
                                                                            in1=o0[:], op0=ALU.mult, op1=ALU.add))(),
                     reads=["osb", "nlr", "o0"], writes=["o1"])
                P.op("dve", (lambda qs=qs: lambda v: v.tensor_tensor(out=sq[:], in0=o1[:, qs, :], in1=o1[:, qs, :], op=ALU.mult))(),
                     reads=["o1"], writes=["sq"])
                P.op("dve", (lambda qs=qs: lambda v: v.reduce_sum(out=ms[:, qs:qs + 1], in_=sq[:], axis=AX.X))(), reads=["sq"], writes=["ms"])

        def finalize_b1(h, g):
            P.op("act", lambda a: a.activation(out=ms[:], in_=ms[:], func=AF.Ln, scale=1.0 / 128, bias=epsb[:]), reads=["ms", "epsb"], writes=["ms"])
            P.op("act", lambda a: a.activation(out=ms[:], in_=ms[:], func=AF.Exp, scale=-0.5), reads=["ms"], writes=["ms"])
            for qs in range(4):
                P.op("dve", (lambda qs=qs: lambda v: v.scalar_tensor_tensor(out=on[:, qs, :], in0=o1[:, qs, :], scalar=ms[:, qs:qs + 1],
                                                                            in1=swb[:], op0=ALU.mult, op1=ALU.mult))(),
                     reads=["o1", "ms", "swb"], writes=["on"])

        def finalize_b2(h, g, fi):
            for qs in range(4):
                P.op("pe", (lambda qs=qs: lambda t: t.transpose(out=pTo[:, qs, :], in_=on[:, qs, :], identity=ident[:]))(),
                     reads=["on", "ident"] if qs in (0, 3) else (), writes=["pTo"] if qs in (0, 3) else ())
            fs = fi % 2
            P.op("dve", lambda v: v.tensor_copy(out=oTs[fs][:], in_=pTo[:].rearrange("p a b -> p (a b)")), reads=["pTo"], writes=[("oTs", fs)])
            P.dma("pool", OT[h, :, g * 512:(g + 1) * 512], oTs[fs][:], key=("ost", fs), reads=[("oTs", fs)])

        load_head(0)
        if pre is not None:
            pre(P)
        qk(0)
        if len(steps) > 1:
            qk(1)
        fi = 0
        pending = []
        for i, (h, g, j) in enumerate(steps):
            ss = i % 2
            es = i % 3
            hs = h % 2
            if g == 0 and j == 0 and h + 1 < n_heads:
                load_head(h + 1)
            P.op("act", (lambda ss=ss, es=es: lambda a: a.activation(out=e[es][:], in_=S[ss][:], func=AF.Exp))(),
                 reads=[("S", ss)], writes=[("e", es)])
            if i + 2 < len(steps):
                qk(i + 2)
            for c in range(2):
                for qs in range(4):
                    first = (c == 0 and qs == 0)
                    last = (c == 1 and qs == 3)
                    P.op("pe", (lambda c=c, qs=qs, j=j, es=es, hs=hs: lambda t: t.matmul(
                        acc(c, qs), lhsT=e[es][:, c, qs * 128:(qs + 1) * 128], rhs=V[hs][:, j, :],
                        start=(j == 0 and (c * 4 + qs) % 3 == 0), stop=(j == n_kt - 1)))(),
                         reads=[("e", es), ("V", hs, min(4, j // 26))] if (first or last) else (), writes=["po"] if (first or last) else ())
            if j == n_kt - 1:
                finalize_a(h, g)
                pending.append((i + 6, (lambda h=h, g=g: lambda: finalize_b1(h, g))()))
                pending.append((i + 12, (lambda h=h, g=g, fi=fi: lambda: finalize_b2(h, g, fi))()))
                fi += 1
            while pending and pending[0][0] <= i:
                pending.pop(0)[1]()
        for (_, fn) in pending:
            fn()
        P.flush()


def wcast_decl(dr, pfx, n_e, F):
    return (dr.get(f"{pfx}_WG", [n_e, 128, 8, F], BF16), dr.get(f"{pfx}_WU", [n_e, 128, 8, F], BF16),
            dr.get(f"{pfx}_WD", [n_e, 128, F // 128, D], BF16))


def phase_wcast(nc, dr, P, pfx, wg, wu, wd, n_e, F, key, gs=None):
    WG, WU, WD = wcast_decl(dr, pfx, n_e, F)
    for e in range(n_e):
        for (dst, src) in ((WG, wg), (WU, wu)):
            sv = src[e].rearrange("(kc p) f -> p kc f", p=128)
            for kc in range(8):
                if gs is not None:
                    P.dma_g("pool", dst[e, :, kc, :], sv[:, kc, :], gs)
                else:
                    P.dma("pool", dst[e, :, kc, :], sv[:, kc, :], key=key, writes=[(pfx, "w")])
        sv = wd[e].rearrange("(fc p) n -> p fc n", p=128)
        nfc = F // 128
        for f0 in range(0, nfc, 7):
            f1 = min(nfc, f0 + 7)
            if gs is not None:
                P.dma_g("pool", WD[e, :, f0:f1, :], sv[:, f0:f1, :], gs)
            else:
                P.dma("pool", WD[e, :, f0:f1, :], sv[:, f0:f1, :], key=key, writes=[(pfx, "w")])


def phase_wo(nc, dr, n_groups=TOK // 512):
    OT = dr.get("OT", [H, 128, TOK], BF16)
    x_own = dr.get("x_own", [TOK, D], F32)
    w_o = dr.get("w_o", [D, D], F32)
    modv = dr.get("modv", [2, 2, 6 * D], F32)
    X1A = dr.get("X1A", [TOK, D], F32)
    P = Prog(nc, "p3a")
    with ExitStack() as st:
        sb = lambda nm, shp, dt: st.enter_context(nc.sbuf_tensor(f"p3a_{nm}", shp, dt))
        ps = lambda nm, shp, dt: st.enter_context(nc.psum_tensor(f"p3a_{nm}", shp, dt))
        Wo = sb("Wo", [128, 8, D], BF16)
        g1b = sb("g1b", [128, D], F32)
        oT = [sb(f"oT{i}", [128, 8, 512], BF16) for i in range(2)]
        xt = [sb(f"xt{i}", [128, D], F32) for i in range(2)]
        tmp = [sb(f"tmp{i}", [128, D], F32) for i in range(2)]
        xo = [sb(f"xo{i}", [128, D], F32) for i in range(2)]
        py = [ps(f"py{i}", [128, 2, 512], F32) for i in range(2)]
        wv = w_o.rearrange("(h p) n -> p h n", p=128)
        for h in range(8):
            P.dma("pool", Wo[:, h, :], wv[:, h, :], key="w", writes=["Wo"])
        P.dma("sp", g1b[:], modv[0, 0, 2 * D:3 * D].partition_broadcast(128), key="g", writes=["g1b"])
        i = 0
        for g in range(n_groups):
            gs = g % 2
            P.dma("sp", oT[gs][:], OT[:, :, g * 512:(g + 1) * 512].rearrange("h p t -> p h t"), key=("oT", gs), writes=[("oT", gs)])
            for sub in range(4):
                s = i % 2
                i += 1
                r0 = g * 512 + sub * 128
                P.dma("sp", xt[s][:], x_own[r0:r0 + 128, :], key=("xt", s), writes=[("xt", s)])
                for half in range(2):
                    mm_group(P, py[s][:, half, :], [(oT[gs][:, h, sub * 128:(sub + 1) * 128], Wo[:, h, half * 512:(half + 1) * 512])
                                                    for h in range(8)], reads=[("oT", gs), "Wo"], writes=[("py", s)])
                P.op("dve", (lambda s=s: lambda v: v.tensor_tensor(out=tmp[s][:], in0=py[s][:].rearrange("p a b -> p (a b)"),
                                                                   in1=g1b[:], op=ALU.mult))(),
                     reads=[("py", s), "g1b"], writes=[("tmp", s)])
                P.op("pool", (lambda s=s: lambda g_: g_.tensor_tensor(out=xo[s][:], in0=tmp[s][:], in1=xt[s][:], op=ALU.add))(),
                     reads=[("tmp", s), ("xt", s)], writes=[("xo", s)])
                P.dma("act", X1A[r0:r0 + 128, :], xo[s][:], key=("st", s), reads=[("xo", s)])
        P.flush()


class SwigluStream:
    def __init__(self, nc, st, name, nfc_max):
        sb = lambda nm, shp, dt: st.enter_context(nc.sbuf_tensor(f"{name}_{nm}", shp, dt))
        ps = lambda nm, shp, dt: st.enter_context(nc.psum_tensor(f"{name}_{nm}", shp, dt))
        self.n = name
        self.nw = 2
        self.wg = [sb(f"wg{i}", [128, 8, 512], BF16) for i in range(self.nw)]
        self.wu = [sb(f"wu{i}", [128, 8, 512], BF16) for i in range(self.nw)]
        self.wd = [sb(f"wd{i}", [128, nfc_max, 512], BF16) for i in range(2)]
        self.aT = sb("aT", [128, nfc_max, 512], BF16)
        self.sg = [sb(f"sg{i}", [128, 512], F32) for i in range(2)]
        self.pg = [ps(f"pg{i}", [128, 512], F32) for i in range(2)]
        self.pu = [ps(f"pu{i}", [128, 512], F32) for i in range(2)]
        self.pd = [ps(f"pd{i}", [128, 512], F32) for i in range(2)]
        self.ip = 0
        self.ic = 0
        self.idn = 0
        self.ih = 0

    def run(self, P, hT, hT_key, WG_e, WU_e, WD_e, nfc, evac):
        n = self.n
        pieces = [(f0, min(nfc, f0 + 4)) for f0 in range(0, nfc, 4)]
        for (f0, f1) in pieces:
            ws = self.ip % self.nw
            self.ip += 1
            w = (f1 - f0) * 128
            P.dma("sp", self.wg[ws][:, :, 0:w], WG_e[:, :, f0 * 128:f1 * 128], key=(n, "wg", ws), writes=[(n, "wg", ws)])
            P.dma("sp", self.wu[ws][:, :, 0:w], WU_e[:, :, f0 * 128:f1 * 128], key=(n, "wu", ws), writes=[(n, "wu", ws)])
            for fc in range(f0, f1):
                c = self.ic % 2
                self.ic += 1
                o = (fc - f0) * 128
                mm_group(P, self.pg[c][:], [(self.wg[ws][:, kc, o:o + 128], hT[:, kc, :]) for kc in range(8)],
                         reads=[hT_key, (n, "wg", ws)], writes=[(n, "pg", c)])
                mm_group(P, self.pu[c][:], [(self.wu[ws][:, kc, o:o + 128], hT[:, kc, :]) for kc in range(8)],
                         reads=[hT_key, (n, "wu", ws)], writes=[(n, "pu", c)])
                P.op("act", (lambda c=c: lambda a: a.activation(out=self.sg[c][:], in_=self.pg[c][:], func=AF.Silu))(),
                     reads=[(n, "pg", c)], writes=[(n, "sg", c)])
                P.op("dve", (lambda c=c, fc=fc: lambda v: v.tensor_tensor(out=self.aT[:, fc, :], in0=self.pu[c][:], in1=self.sg[c][:],
                                                                          op=ALU.mult))(),
                     reads=[(n, "pu", c), (n, "sg", c)], writes=[(n, "aT")])
        for half in range(2):
            hs = self.ih % 2
            self.ih += 1
            for f0 in range(0, nfc, 7):
                f1 = min(nfc, f0 + 7)
                P.dma("sp", self.wd[hs][:, f0:f1, :], WD_e[:, f0:f1, half * 512:(half + 1) * 512], key=(n, "wd", hs), writes=[(n, "wd", hs)])
            for sub in range(4):
                d = self.idn % 2
                self.idn += 1
                mm_group(P, self.pd[d][:], [(self.aT[:, fc, sub * 128:(sub + 1) * 128], self.wd[hs][:, fc, :]) for fc in range(nfc)],
                         reads=[(n, "aT"), (n, "wd", hs)], writes=[(n, "pd", d)])
                evac(P, sub, half, self.pd[d][:], (n, "pd", d))


def phase_ffn(nc, dr, gsem, n_groups=TOK // 512):
    X1A = dr.get("X1A", [TOK, D], F32)
    X1 = dr.get("X1", [TOK, D], F32)
    modv = dr.get("modv", [2, 2, 6 * D], F32)
    norm2_w = dr.get("norm2_w", [2, D], F32)
    WG, WU, WD = wcast_decl(dr, "ffn", 1, FFN)
    P = Prog(nc, "p3b")
    with ExitStack() as st:
        sb = lambda nm, shp, dt: st.enter_context(nc.sbuf_tensor(f"p3b_{nm}", shp, dt))
        nt = NormT(nc, st, "p3bn")
        nt.init(P)
        scT, shT, pk = load_mod_T(P, nc, st, "p3bm", modv, 0, 0, 1, norm2_w[0, :])
        sw = SwigluStream(nc, st, "p3bs", FFN // 128)
        g2b = sb("g2b", [128, D], F32)
        P.dma("sp", g2b[:], modv[0, 0, 5 * D:6 * D].partition_broadcast(128), key="g", writes=["g2b"])
        xg = [sb(f"xg{i}", [128, 4, D], F32) for i in range(2)]
        hT = [sb(f"hT{i}", [128, 8, 512], BF16) for i in range(2)]
        tmp = [sb(f"tmp{i}", [128, 512], F32) for i in range(2)]
        if gsem is not None:
            P.op("sp", lambda s: s.wait_ge(gsem[0], gsem[1]))
        cnt = {"t": 0}
        for g in range(n_groups):
            gs = g % 2
            for sub in range(4):
                r0 = g * 512 + sub * 128
                P.dma("sp", xg[gs][:, sub, :], X1A[r0:r0 + 128, :], key=("xg", gs), writes=[("xg", gs, sub)])
                nt.emit(P, xg[gs][:, sub, :], ("xg", gs, sub), scT, shT, pk, hT[gs][:, :, sub * 128:(sub + 1) * 128], ("hT", gs))

            def evac(P, sub, half, pap, pkey, g=g, gs=gs):
                t = cnt["t"] % 2
                cnt["t"] += 1
                P.op("dve", lambda v: v.tensor_tensor(out=tmp[t][:], in0=pap, in1=g2b[:, half * 512:(half + 1) * 512], op=ALU.mult),
                     reads=[pkey, "g2b"], writes=[("tmp", t)])
                P.op("pool", lambda g_: g_.tensor_tensor(out=xg[gs][:, sub, half * 512:(half + 1) * 512], in0=tmp[t][:],
                                                         in1=xg[gs][:, sub, half * 512:(half + 1) * 512], op=ALU.add),
                     reads=[("tmp", t), ("xg", gs, sub)], writes=[("xg", gs, sub)])
                if half == 1:
                    r0 = g * 512 + sub * 128
                    P.dma("act", X1[r0:r0 + 128, :], xg[gs][:, sub, :], key=("st", gs), reads=[("xg", gs, sub)])

            sw.run(P, hT[gs], ("hT", gs), WG[0], WU[0], WD[0], FFN // 128, evac)
        P.flush()


def phase_gmlp(nc, dr, n_tiles=TOK // 128):
    X1 = dr.get("X1", [TOK, D], F32)
    X2 = dr.get("X2", [TOK, D], F32)
    modv = dr.get("modv", [2, 2, 6 * D], F32)
    norm1_w = dr.get("norm1_w", [2, D], F32)
    gm_w_in = dr.get("gm_w_in", [D, 4 * D], F32)
    gm_vn_w = dr.get("gm_vn_w", [2 * D], F32)
    gm_vn_b = dr.get("gm_vn_b", [2 * D], F32)
    gm_wsT = dr.get("gm_wsT", [8, 128, 128], F32)
    gm_bsT = dr.get("gm_bsT", [128, 8], F32)
    gm_w_out = dr.get("gm_w_out", [2 * D, D], F32)
    P = Prog(nc, "p4")
    with ExitStack() as st:
        sb = lambda nm, shp, dt: st.enter_context(nc.sbuf_tensor(f"p4_{nm}", shp, dt))
        ps = lambda nm, shp, dt: st.enter_context(nc.psum_tensor(f"p4_{nm}", shp, dt))
        nt = NormT(nc, st, "p4n", nbuf=1)
        nt.init(P)
        scT, shT, pk = load_mod_T(P, nc, st, "p4m", modv, 0, 1, 0, norm1_w[1, :])
        Win = sb("Win", [128, 8, 4 * D], BF16)
        Wout = sb("Wout", [128, 16, D], BF16)
        WsT = sb("WsT", [128, 8, 128], BF16)
        bsT = sb("bsT", [128, 8], F32)
        vnw = sb("vnw", [128, 2 * D], F32)
        vnb = sb("vnb", [128, 2 * D], F32)
        g1b = sb("g1b", [128, D], F32)
        epsb = sb("epsb", [128, 1], F32)
        xt = [sb(f"xt{i}", [128, D], F32) for i in range(3)]
        hT = [sb(f"hT{i}", [128, 8, 128], BF16) for i in range(2)]
        u_sb = [sb(f"u{i}", [128, 2 * D], BF16) for i in range(3)]
        v_sb = [sb(f"v{i}", [128, 2 * D], F32) for i in range(2)]
        vb = [sb(f"vb{i}", [128, 2 * D], BF16) for i in range(2)]
        gt = [sb(f"gt{i}", [128, 2 * D], BF16) for i in range(2)]
        gT = [sb(f"gT{i}", [128, 16, 128], BF16) for i in range(2)]
        stats = [sb(f"stats{i}", [128, 4, 6], F32) for i in range(2)]
        mv = [sb(f"mv{i}", [128, 2], F32) for i in range(2)]
        rstd = [sb(f"rstd{i}", [128, 1], F32) for i in range(2)]
        xo = [sb(f"xo{i}", [128, D], F32) for i in range(2)]
        pz = [ps(f"pz{i}", [128, 512], F32) for i in range(2)]
        psS = [ps(f"ps{i}", [128, 512], F32) for i in range(2)]
        pgT = [ps(f"pgT{i}", [128, 8, 128], BF16) for i in range(2)]
        py = ps("py", [128, 512], F32)
        wv = gm_w_in.rearrange("(kc p) n -> p kc n", p=128)
        for kc in range(8):
            for q in range(2):
                P.dma("pool", Win[:, kc, q * 2048:(q + 1) * 2048], wv[:, kc, q * 2048:(q + 1) * 2048], key="w", writes=["Win"])
        wo = gm_w_out.rearrange("(fc p) n -> p fc n", p=128)
        for fc in range(0, 16, 4):
            P.dma("pool", Wout[:, fc:fc + 4, :], wo[:, fc:fc + 4, :], key="w", writes=["Wout"])
        P.dma("pool", WsT[:], gm_wsT.rearrange("g q p -> q g p"), key="w", writes=["WsT"])
        P.dma("sp", bsT[:], gm_bsT[:, :], key="c", writes=["bsT"])
        P.dma("sp", vnw[:], gm_vn_w.partition_broadcast(128), key="c", writes=["vnw"])
        P.dma("sp", vnb[:], gm_vn_b.partition_broadcast(128), key="c", writes=["vnb"])
        P.dma("sp", g1b[:], modv[0, 1, 2 * D:3 * D].partition_broadcast(128), key="c", writes=["g1b"])
        P.op("pool", lambda g: g.memset(epsb[:], EPS), writes=["epsb"])
        cz = {"z": 0}

        def s1a(t):
            s3, s2 = t % 3, t % 2
            r0 = t * 128
            P.dma("sp", xt[s3][:], X1[r0:r0 + 128, :], key=("xt", s3), writes=[("xt", s3)])
            nt.emit(P, xt[s3][:], ("xt", s3), scT, shT, pk, hT[s2][:], ("hT", s2))

        def s1b(t):
            s3, s2 = t % 3, t % 2
            for cg in range(8):
                z = cz["z"] % 2
                cz["z"] += 1
                mm_group(P, pz[z][:], [(hT[s2][:, kc, :], Win[:, kc, cg * 512:(cg + 1) * 512]) for kc in range(8)],
                         reads=[("hT", s2), "Win"], writes=[("pz", z)])
                dst = u_sb[s3][:, cg * 512:(cg + 1) * 512] if cg < 4 else v_sb[s2][:, (cg - 4) * 512:(cg - 3) * 512]
                P.op("act", (lambda z=z, dst=dst: lambda a: a.activation(out=dst, in_=pz[z][:], func=AF.Gelu))(),
                     reads=[("pz", z)], writes=[("u", s3) if cg < 4 else ("v", s2)])

        def s2(t):
            s = t % 2
            for c in range(4):
                P.op("dve", (lambda c=c: lambda v: v.bn_stats(out=stats[s][:, c, :], in_=v_sb[s][:, c * 512:(c + 1) * 512]))(),
                     reads=[("v", s)], writes=[("stats", s)])
            P.op("dve", lambda v: v.bn_aggr(out=mv[s][:], in_=stats[s][:].rearrange("p a b -> p (a b)")), reads=[("stats", s)], writes=[("mv", s)])
            P.op("act", lambda a: a.activation(out=rstd[s][:], in_=mv[s][:, 1:2], func=AF.Sqrt, bias=epsb[:]),
                 reads=[("mv", s), "epsb"], writes=[("rstd", s)])
            P.op("dve", lambda v: v.reciprocal(out=rstd[s][:], in_=rstd[s][:]), reads=[("rstd", s)], writes=[("rstd", s)])
            P.op("dve", lambda v: v.tensor_scalar(out=v_sb[s][:], in0=v_sb[s][:], scalar1=mv[s][:, 0:1], scalar2=rstd[s][:, 0:1],
                                                  op0=ALU.subtract, op1=ALU.mult), reads=[("v", s), ("mv", s), ("rstd", s)], writes=[("v", s)])
            P.op("pool", lambda g: g.tensor_tensor(out=v_sb[s][:], in0=v_sb[s][:], in1=vnw[:], op=ALU.mult), reads=[("v", s), "vnw"], writes=[("v", s)])
            P.op("pool", lambda g: g.tensor_tensor(out=vb[s][:], in0=v_sb[s][:], in1=vnb[:], op=ALU.add), reads=[("v", s), "vnb"], writes=[("vb", s)])

        def s3a(t):
            s, s3 = t % 2, t % 3
            for gp in range(4):
                sp_ = gp % 2
                for k in range(2):
                    g_ = gp * 2 + k
                    P.op("pe", (lambda g_=g_, k=k, sp_=sp_: lambda t_: t_.matmul(psS[sp_][:, k * 256:(k + 1) * 256], lhsT=WsT[:, g_, :],
                                                                               rhs=vb[s][:, g_ * 256:(g_ + 1) * 256], start=(k == 0), stop=True))(),
                         reads=[("vb", s), "WsT"], writes=[("psS", sp_)])
                for k in range(2):
                    g_ = gp * 2 + k
                    P.op("dve", (lambda g_=g_, k=k, sp_=sp_: lambda v: v.scalar_tensor_tensor(
                        out=gt[s][:, g_ * 256:(g_ + 1) * 256], in0=psS[sp_][:, k * 256:(k + 1) * 256], scalar=bsT[:, g_:g_ + 1],
                        in1=u_sb[s3][:, g_ * 256:(g_ + 1) * 256], op0=ALU.add, op1=ALU.mult))(),
                         reads=[("psS", sp_), "bsT", ("u", s3)], writes=[("gt", s)])

        def s3b(t):
            s, s3 = t % 2, t % 3
            r0 = t * 128
            for hb in range(2):
                for k in range(8):
                    fc = hb * 8 + k
                    P.op("pe", (lambda fc=fc, k=k, hb=hb: lambda t_: t_.transpose(out=pgT[hb][:, k, :], in_=gt[s][:, fc * 128:(fc + 1) * 128],
                                                                                 identity=nt.ident[:]))(),
                         reads=[("gt", s), (nt.n, "ident")] if k in (0, 7) else (), writes=[("pgT", hb)] if k in (0, 7) else ())
                P.op("act", (lambda hb=hb: lambda a: a.copy(out=gT[s][:, hb * 8:(hb + 1) * 8, :], in_=pgT[hb][:]))(),
                     reads=[("pgT", hb)], writes=[("gT", s)])
            for half in range(2):
                mm_group(P, py[:], [(gT[s][:, fc, :], Wout[:, fc, half * 512:(half + 1) * 512]) for fc in range(16)],
                         reads=[("gT", s), "Wout"], writes=["py"])
                P.op("dve", (lambda half=half: lambda v: v.tensor_tensor(out=xo[s][:, half * 512:(half + 1) * 512], in0=py[:],
                                                                         in1=g1b[:, half * 512:(half + 1) * 512], op=ALU.mult))(),
                     reads=["py", "g1b"], writes=[("xo", s)])
            P.op("pool", lambda g: g.tensor_tensor(out=xo[s][:], in0=xo[s][:], in1=xt[s3][:], op=ALU.add),
                 reads=[("xo", s), ("xt", s3)], writes=[("xo", s)])
            P.dma("act", X2[r0:r0 + 128, :], xo[s][:], key=("st", s), reads=[("xo", s)])

        for k in range(n_tiles + 2):
            if k < n_tiles:
                s1a(k)
            if 0 <= k - 2 < n_tiles:
                s3a(k - 2)
            if k < n_tiles:
                s1b(k)
            if 0 <= k - 2 < n_tiles:
                s3b(k - 2)
            if 0 <= k - 1 < n_tiles:
                s2(k - 1)
        P.flush()


def phase_moe(nc, dr, gsem, n_groups=TOK // 512, n_exp=NEXP):
    X2 = dr.get("X2", [TOK, D], F32)
    OUT = dr.get("out", [TOK, D], F32)
    modv = dr.get("modv", [2, 2, 6 * D], F32)
    norm2_w = dr.get("norm2_w", [2, D], F32)
    final_w = dr.get("final_norm_w", [D], F32)
    w_router = dr.get("moe_w_router", [D, NEXP], F32)
    WG, WU, WD = wcast_decl(dr, "moe", NEXP, EXP_D)
    P = Prog(nc, "p5")
    with ExitStack() as st:
        sb = lambda nm, shp, dt: st.enter_context(nc.sbuf_tensor(f"p5_{nm}", shp, dt))
        ps = lambda nm, shp, dt: st.enter_context(nc.psum_tensor(f"p5_{nm}", shp, dt))
        nt = NormT(nc, st, "p5n", nbuf=1)
        nt.init(P)
        scT, shT, pk = load_mod_T(P, nc, st, "p5m", modv, 0, 1, 1, norm2_w[1, :])
        sw = SwigluStream(nc, st, "p5s", EXP_D // 128)
        g2b = sb("g2b", [128, D], F32)
        fnb = sb("fnb", [128, D], F32)
        P.dma("sp", g2b[:], modv[0, 1, 5 * D:6 * D].partition_broadcast(128), key="g", writes=["g2b"])
        P.dma("sp", fnb[:], final_w.partition_broadcast(128), key="g", writes=["fnb"])
        xg = sb("xg", [128, 4, D], F32)
        yacc = sb("yacc", [128, 4, D], F32)
        hT = sb("hT", [128, 8, 512], BF16)
        identf = sb("identf", [128, 128], F32)
        wr = sb("wr", [128, 8, NEXP], F32)
        wrs = sb("wrs", [128, 8, NEXP], F32)
        shTb = sb("shTb", [128, 8, 128], F32)
        rbias = sb("rbias", [128, NEXP], F32)
        xT32 = sb("xT32", [128, 8, 128], F32)
        lg = sb("lg", [128, 4, NEXP], F32)
        m8 = sb("m8", [128, 4, 8], F32)
        nm1 = sb("nm1", [128, 4], F32)
        msk = sb("msk", [128, 4, NEXP], F32)
        ex = sb("ex", [128, 4, NEXP], F32)
        den = sb("den", [128, 4], F32)
        gates = sb("gates", [128, 4, NEXP], F32)
        ss = sb("ss", [128, 1], F32)
        rstd = sb("rstd", [128, 1], F32)
        junk = sb("junk", [128, D], BF16)
        epsb = sb("epsb", [128, 1], F32)
        prt = ps("prt", [128, 4, 128], F32)
        P.op("pool", lambda g: g.memset(identf[:], 0.0), writes=["identf"])
        P.op("pool", lambda g: g.affine_select(out=identf[:], in_=identf[:], pattern=[[-1, 128]], compare_op=ALU.not_equal,
                                               fill=1.0, base=0, channel_multiplier=1), reads=["identf"], writes=["identf"])
        P.op("pool", lambda g: g.memset(epsb[:], EPS), writes=["epsb"])
        P.dma("sp", wr[:], w_router.rearrange("(kc p) e -> p kc e", p=128), key="g", writes=["wr"])
        P.op("dve", lambda v: v.tensor_tensor(out=wrs[:], in0=wr[:], in1=bc(scT.unsqueeze(2), [128, 8, NEXP]), op=ALU.mult),
             reads=["wr"] + pk, writes=["wrs"])
        P.op("dve", lambda v: v.tensor_copy(out=shTb[:], in_=bc(shT.unsqueeze(2), [128, 8, 128])), reads=pk, writes=["shTb"])
        mm_group(P, prt[:, 0, 0:NEXP], [(shTb[:, kc, :], wr[:, kc, :]) for kc in range(8)], reads=["shTb", "wr"], writes=["prt"])
        P.op("dve", lambda v: v.tensor_copy(out=rbias[:], in_=prt[:, 0, 0:NEXP]), reads=["prt"], writes=["rbias"])
        if gsem is not None:
            P.op("sp", lambda s: s.wait_ge(gsem[0], gsem[1]))
        for g in range(n_groups):
            for sub in range(4):
                r0 = g * 512 + sub * 128
                P.dma("sp", xg[:, sub, :], X2[r0:r0 + 128, :], key="xg", writes=[("xg", sub)])
                nt.emit(P, xg[:, sub, :], ("xg", sub), scT, shT, pk, hT[:, :, sub * 128:(sub + 1) * 128], "hT")
                rs_ap, rs_key = nt.last_rstd
                for hb in range(2):
                    for k in range(4):
                        kc = hb * 4 + k
                        P.op("pe", (lambda kc=kc, k=k, sub=sub: lambda t: t.matmul(prt[:, k, :], lhsT=xg[:, sub, kc * 128:(kc + 1) * 128],
                                                                                  rhs=identf[:], start=(k == 0), stop=True))(),
                             reads=[("xg", sub), "identf"] if k in (0, 3) else (), writes=["prt"] if k in (0, 3) else ())
                    P.op("dve", (lambda hb=hb: lambda v: v.tensor_copy(out=xT32[:, hb * 4:(hb + 1) * 4, :], in_=prt[:]))(),
                         reads=["prt"], writes=["xT32"])
                mm_group(P, prt[:, 0, 0:NEXP], [(xT32[:, kc, :], wrs[:, kc, :]) for kc in range(8)], reads=["xT32", "wrs"], writes=["prt"])
                P.op("dve", (lambda sub=sub, rs_ap=rs_ap: lambda v: v.scalar_tensor_tensor(
                    out=lg[:, sub, :], in0=prt[:, 0, 0:NEXP], scalar=rs_ap[:, 0:1], in1=rbias[:], op0=ALU.mult, op1=ALU.add))(),
                     reads=["prt", rs_key, "rbias"], writes=["lg"])
            for sub in range(4):
                P.op("dve", (lambda sub=sub: lambda v: v.max(out=m8[:, sub, :], in_=lg[:, sub, :]))(), reads=["lg"], writes=["m8"])
            P.op("dve", lambda v: v.tensor_scalar(out=nm1[:], in0=m8[:, :, 0], scalar1=-1.0, scalar2=None, op0=ALU.mult), reads=["m8"], writes=["nm1"])
            for sub in range(4):
                P.op("dve", (lambda sub=sub: lambda v: v.tensor_scalar(out=msk[:, sub, :], in0=lg[:, sub, :], scalar1=m8[:, sub, 1:2], scalar2=None,
                                                                      op0=ALU.is_ge))(), reads=["lg", "m8"], writes=["msk"])
                P.op("act", (lambda sub=sub: lambda a: a.activation(out=ex[:, sub, :], in_=lg[:, sub, :], func=AF.Exp, bias=nm1[:, sub:sub + 1]))(),
                     reads=["lg", "nm1"], writes=["ex"])
            P.op("dve", lambda v: v.tensor_tensor(out=ex[:], in0=ex[:], in1=msk[:], op=ALU.mult), reads=["ex", "msk"], writes=["ex"])
            P.op("dve", lambda v: v.reduce_sum(out=den[:], in_=ex[:], axis=AX.X), reads=["ex"], writes=["den"])
            P.op("dve", lambda v: v.reciprocal(out=den[:], in_=den[:]), reads=["den"], writes=["den"])
            P.op("dve", lambda v: v.tensor_tensor(out=gates[:], in0=ex[:], in1=bc(den[:, :].unsqueeze(2), [128, 4, NEXP]), op=ALU.mult),
                 reads=["ex", "den"], writes=["gates"])
            for e in range(n_exp):
                def evac(P, sub, half, pap, pkey, e=e):
                    dst = yacc[:, sub, half * 512:(half + 1) * 512]
                    if e == 0:
                        P.op("dve", lambda v: v.tensor_scalar(out=dst, in0=pap, scalar1=gates[:, sub, e:e + 1], scalar2=None, op0=ALU.mult),
                             reads=[pkey, "gates"], writes=[("yacc", sub, half)])
                    else:
                        P.op("dve", lambda v: v.scalar_tensor_tensor(out=dst, in0=pap, scalar=gates[:, sub, e:e + 1], in1=dst,
                                                                     op0=ALU.mult, op1=ALU.add),
                             reads=[pkey, "gates", ("yacc", sub, half)], writes=[("yacc", sub, half)])
                sw.run(P, hT, "hT", WG[e], WU[e], WD[e], EXP_D // 128, evac)
            for sub in range(4):
                r0 = g * 512 + sub * 128
                P.op("pool", (lambda sub=sub: lambda g_: g_.tensor_tensor(out=yacc[:, sub, :], in0=yacc[:, sub, :], in1=g2b[:], op=ALU.mult))(),
                     reads=[("yacc", sub, 0), ("yacc", sub, 1), "g2b"], writes=[("yacc", sub, 0), ("yacc", sub, 1)])
                P.op("pool", (lambda sub=sub: lambda g_: g_.tensor_tensor(out=xg[:, sub, :], in0=yacc[:, sub, :], in1=xg[:, sub, :], op=ALU.add))(),
                     reads=[("yacc", sub, 0), ("yacc", sub, 1), ("xg", sub)], writes=[("xg", sub)])
                P.op("act", (lambda sub=sub: lambda a: a.activation(out=junk[:], in_=xg[:, sub, :], func=AF.Square, accum_out=ss[:]))(),
                     reads=[("xg", sub)], writes=["junk", "ss"])
                P.op("act", lambda a: a.activation(out=ss[:], in_=ss[:], func=AF.Sqrt, scale=1.0 / D, bias=epsb[:]), reads=["ss", "epsb"], writes=["ss"])
                P.op("dve", lambda v: v.reciprocal(out=rstd[:], in_=ss[:]), reads=["ss"], writes=["rstd"])
                P.op("dve", (lambda sub=sub: lambda v: v.scalar_tensor_tensor(out=yacc[:, sub, :], in0=xg[:, sub, :], scalar=rstd[:, 0:1], in1=fnb[:],
                                                                             op0=ALU.mult, op1=ALU.mult))(),
                     reads=[("xg", sub), "rstd", "fnb"], writes=[("yacc", sub, 0), ("yacc", sub, 1)])
                P.dma("act", OUT[r0:r0 + 128, :], yacc[:, sub, :], key="ost", reads=[("yacc", sub, 0), ("yacc", sub, 1)])
        P.flush()


EXT_IN = ["x_b", "x_own", "ctx_b", "cvec", "w_mod", "b_mod", "norm1_w", "norm2_w", "final_norm_w", "wqkv", "wqk_perm",
          "cos_all", "sin_all", "cos_own", "sin_own", "lamv", "subln_w", "w_o", "ffn_wg", "ffn_wu", "ffn_wd",
          "gm_w_in", "gm_vn_w", "gm_vn_b", "gm_wsT", "gm_bsT", "gm_w_out", "moe_w_router", "moe_wg", "moe_wu", "moe_wd"]


def build_program():
    nc = bass.Bass("TRN2", target_bir_lowering=False)
    dr = Dram(nc, ext_in=EXT_IN, ext_out=["out"])
    fwg = dr.get("ffn_wg", [1, D, FFN], F32)
    fwu = dr.get("ffn_wu", [1, D, FFN], F32)
    fwd = dr.get("ffn_wd", [1, FFN, D], F32)
    mwg = dr.get("moe_wg", [NEXP, D, EXP_D], F32)
    mwu = dr.get("moe_wu", [NEXP, D, EXP_D], F32)
    mwd = dr.get("moe_wd", [NEXP, EXP_D, D], F32)
    with nc.semaphore("g_wcast") as gw:
        gs = {"sem": gw, "count": 0}

        def casts(P):
            phase_wcast(nc, dr, P, "ffn", fwg, fwu, fwd, 1, FFN, "wc", gs=gs)
            phase_wcast(nc, dr, P, "moe", mwg, mwu, mwd, NEXP, EXP_D, "wc", gs=gs)

        phase_mod(nc, dr)
        nc.all_engine_barrier()
        phase_qkv(nc, dr)
        nc.all_engine_barrier()
        phase_attn(nc, dr, pre=casts)
        nc.all_engine_barrier()
        gsem = (gw, gs["count"])
        phase_wo(nc, dr)
        nc.all_engine_barrier()
        phase_ffn(nc, dr, gsem)
        nc.all_engine_barrier()
        phase_gmlp(nc, dr)
        nc.all_engine_barrier()
        phase_moe(nc, dr, gsem)
    return nc


def _perm_cols():
    idx = np.arange(D)
    d = idx % 64
    partner = np.where(d % 32 < 16, d + 16, d - 16)
    return (idx // 64) * 64 + partner


def _rope_tables():
    t = np.arange(SEQ)
    row = (t // 64).astype(np.float32)
    col = (t % 64).astype(np.float32)
    inv_freq = (np.float32(10000.0) ** (-np.arange(16, dtype=np.float32) / np.float32(16))).astype(np.float32)
    d = np.arange(128) % 64
    pos = np.where((d >= 32)[:, None], col[None, :], row[None, :]).astype(np.float32)
    ang = (pos * inv_freq[d % 16][:, None]).astype(np.float32)
    sgn = np.where(d % 32 < 16, -1.0, 1.0).astype(np.float32)
    return np.cos(ang).astype(np.float32), (np.sin(ang) * sgn[:, None]).astype(np.float32)


def make_in_maps(inp):
    f32 = lambda a: np.ascontiguousarray(np.asarray(a, dtype=np.float32))
    x, c, ctx = f32(inp["x"]), f32(inp["c"]), f32(inp["ctx"])
    wqkv = f32(inp["da_w_qkv"])[0]
    pc = _perm_cols()
    wqk_perm = np.ascontiguousarray(np.concatenate([wqkv[:, 0:D][:, pc], wqkv[:, D:2 * D][:, pc]], axis=1))
    cos_all, sin_all = _rope_tables()
    lamv = np.stack([f32(inp["da_lambda_q1"])[0], f32(inp["da_lambda_k1"])[0], f32(inp["da_lambda_q2"])[0], f32(inp["da_lambda_k2"])[0]])
    shared = dict(
        w_mod=f32(inp["w_mod"]), b_mod=f32(inp["b_mod"]), norm1_w=f32(inp["norm1_w"]), norm2_w=f32(inp["norm2_w"]),
        final_norm_w=f32(inp["final_norm_w"]), wqkv=wqkv, wqk_perm=wqk_perm, cos_all=cos_all, sin_all=sin_all,
        lamv=f32(lamv), subln_w=f32(inp["da_subln_w"])[0], w_o=f32(inp["da_w_o"])[0],
        ffn_wg=f32(inp["ffn_w_gate"]), ffn_wu=f32(inp["ffn_w_up"]), ffn_wd=f32(inp["ffn_w_down"]),
        gm_w_in=f32(inp["gm_w_in"])[0], gm_vn_w=f32(inp["gm_vnorm_w"])[0], gm_vn_b=f32(inp["gm_vnorm_b"])[0],
        gm_wsT=np.ascontiguousarray(f32(inp["gm_w_s"])[0].transpose(0, 2, 1)), gm_bsT=np.ascontiguousarray(f32(inp["gm_b_s"])[0].T),
        gm_w_out=f32(inp["gm_w_out"])[0], moe_w_router=f32(inp["moe_w_router"])[0],
        moe_wg=f32(inp["moe_w_gate"])[0], moe_wu=f32(inp["moe_w_up"])[0], moe_wd=f32(inp["moe_w_down"])[0],
    )
    maps = []
    for core in range(8):
        b, r = core // 4, core % 4
        t0 = r * TOK
        m = dict(shared)
        m["x_b"] = x[b]
        m["x_own"] = np.ascontiguousarray(x[b, t0:t0 + TOK])
        m["ctx_b"] = ctx[b]
        m["cvec"] = np.ascontiguousarray(np.stack([c[b], f32(inp["c_ctx"])]))
        m["cos_own"] = np.ascontiguousarray(cos_all[:, t0:t0 + TOK] * np.float32(0.125))
        m["sin_own"] = np.ascontiguousarray(sin_all[:, t0:t0 + TOK] * np.float32(0.125))
        maps.append(m)
    return maps


def kernel(**inputs):
    maps = make_in_maps(inputs)
    nc = build_program()
    res = run_bass_kernel_spmd(nc, maps, core_ids=list(range(8)))
    out = np.empty((2, SEQ, D), np.float32)
    for core in range(8):
        b, r = core // 4, core % 4
        out[b, r * TOK:(r + 1) * TOK] = res.results[core]["out"]
    return out
```
